# Optimizing a Trainium2 kernel written in Bass

```python
import math
import jax, jax.numpy as jnp
from jax import lax
import numpy as np

D_MODEL = 1024
BATCH = 16
SEQ = 4096
DEPTH = 2

CHUNK = 64
HEAD_DIM = 64
H_A = 4
KV_RANK = 128
IDX_HEADS = 4
IDX_DIM = 64
TOPK_MAX = 256
QBLK = 128
H_B = 4
DK_B = 64
DV_B = 128
GATE_RANK = 16
GATE_TEMP = 16.0
H_C = 4
KV_C = 2
WINDOW = 128
WIN_CHUNKS = WINDOW // CHUNK
NUM_BUCKETS = 32
MAX_DISTANCE = 128
D_FF = ((8 * D_MODEL // 3 + 255) // 256) * 256
D_MIX = H_A * HEAD_DIM + H_B * DV_B + H_C * HEAD_DIM
IN_SPLITS = (H_A * HEAD_DIM, KV_RANK, IDX_HEADS * IDX_DIM, IDX_DIM, IDX_HEADS,
             H_B * DK_B, H_B * DK_B, H_B * DV_B, GATE_RANK, H_B * DV_B,
             H_C * HEAD_DIM, KV_C * HEAD_DIM, KV_C * HEAD_DIM)
D_IN = sum(IN_SPLITS)
EPS = 1e-6

kernel_name = 'hybrid_dsa_gla_swa_encoder'


def rms_norm(x, g):
    x32 = x.astype(jnp.float32)
    y = x32 * lax.rsqrt(jnp.mean(x32 * x32, axis=-1, keepdims=True) + EPS)
    return (y * g.astype(jnp.float32)).astype(x.dtype)


def rel_bucket(rel):
    nb = NUM_BUCKETS // 2
    max_exact = nb // 2
    n = jnp.abs(rel)
    side = jnp.where(rel > 0, nb, 0)
    nf = jnp.maximum(n, 1).astype(jnp.float32)
    large = max_exact + (jnp.log(nf / max_exact) / math.log(MAX_DISTANCE / max_exact)
                         * (nb - max_exact)).astype(jnp.int32)
    large = jnp.minimum(large, nb - 1)
    return side + jnp.where(n < max_exact, n, large)


def split_columns(p):
    offs, acc = [], 0
    for s in IN_SPLITS[:-1]:
        acc += s
        offs.append(acc)
    return jnp.split(p, offs, axis=-1)


def dsa_mixer(q_a, c_kv, q_idx, k_idx, w_idx, kv_norm, w_uk, w_uv, bias_table):
    bsz, s_len, _ = q_a.shape
    topk = min(TOPK_MAX, s_len // 4)
    nblk = s_len // QBLK
    c_kv = rms_norm(c_kv, kv_norm)
    q = q_a.reshape(bsz, s_len, H_A, HEAD_DIM)
    q_lat = jnp.einsum('bshd,rhd->bshr', q, w_uk) * HEAD_DIM ** -0.5
    q_idx = q_idx.reshape(bsz, s_len, IDX_HEADS, IDX_DIM)
    w_idx = w_idx * (IDX_HEADS ** -0.5 * IDX_DIM ** -0.5)
    key_chunk = jnp.arange(s_len, dtype=jnp.int32) // CHUNK
    table = bias_table[:, :H_A]

    def to_blocks(a):
        return jnp.moveaxis(a.reshape(bsz, nblk, QBLK, *a.shape[2:]), 1, 0)

    def block(args):
        ql, qi, wi, pos = args
        s_idx = jax.nn.relu(jnp.einsum('bqhd,bsd->bqhs', qi, k_idx))
        score = jnp.einsum('bqhs,bqh->bqs', s_idx, wi).astype(jnp.float32)
        q_chunk = pos // CHUNK
        admissible = key_chunk[None, :] <= q_chunk[:, None]
        score = jnp.where(admissible[None], score, -jnp.inf)
        _, sel = lax.top_k(score, topk)
        valid = (sel // CHUNK) <= q_chunk[None, :, None]
        c_sel = jax.vmap(lambda c, i: c[i])(c_kv, sel)
        logits = jnp.einsum('bqhr,bqkr->bhqk', ql, c_sel).astype(jnp.float32)
        bias = table[rel_bucket(sel - pos[None, :, None])]
        logits = logits + jnp.moveaxis(bias, -1, 1).astype(jnp.float32)
        logits = jnp.where(valid[:, None], logits, -jnp.inf)
        p = jax.nn.softmax(logits, axis=-1).astype(c_sel.dtype)
        return jnp.einsum('bhqk,bqkr->bqhr', p, c_sel)

    pos_blocks = jnp.arange(s_len, dtype=jnp.int32).reshape(nblk, QBLK)
    o_lat = lax.map(block, (to_blocks(q_lat), to_blocks(q_idx), to_blocks(w_idx), pos_blocks))
    o_lat = jnp.moveaxis(o_lat, 0, 1).reshape(bsz, s_len, H_A, KV_RANK)
    o = jnp.einsum('bshr,rhd->bshd', o_lat, w_uv)
    return o.reshape(bsz, s_len, H_A * HEAD_DIM)


def gla_mixer(q_b, k_b, v_b, g_lr, r_b, w_g2, b_g, gn):
    bsz, s_len, _ = q_b.shape
    n = s_len // CHUNK
    f32 = jnp.float32
    g = jax.nn.log_sigmoid((g_lr @ w_g2 + b_g).astype(f32)) / GATE_TEMP

    def heads(a, d):
        return a.reshape(bsz, n, CHUNK, H_B, d).transpose(0, 3, 1, 2, 4).astype(f32)

    q = heads(q_b, DK_B) * DK_B ** -0.5
    k = heads(k_b, DK_B)
    v = heads(v_b, DV_B)
    g = heads(g, DK_B)
    b = jnp.cumsum(g, axis=3)
    b_last = b[:, :, :, -1:, :]
    qe = q * jnp.exp(b)
    ke = k * jnp.exp(-b)
    kd = k * jnp.exp(b_last - b)
    causal = jnp.tril(jnp.ones((CHUNK, CHUNK), dtype=bool))
    att = jnp.where(causal, jnp.einsum('bhnid,bhnjd->bhnij', qe, ke), 0.0)
    o_intra = jnp.einsum('bhnij,bhnje->bhnie', att, v)
    kv = jnp.einsum('bhncd,bhnce->bhnde', kd, v)
    decay = jnp.exp(b_last[:, :, :, 0, :])

    def step(state, inp):
        d, kvn = inp
        return d[..., None] * state + kvn, state

    _, s_prev = lax.scan(step, jnp.zeros((bsz, H_B, DK_B, DV_B), f32),
                         (jnp.moveaxis(decay, 2, 0), jnp.moveaxis(kv, 2, 0)))
    s_prev = jnp.moveaxis(s_prev, 0, 2)
    o = o_intra + jnp.einsum('bhncd,bhnde->bhnce', qe, s_prev)
    o = o.transpose(0, 2, 3, 1, 4).reshape(bsz, s_len, H_B, DV_B)
    o = o * lax.rsqrt(jnp.mean(o * o, axis=-1, keepdims=True) + EPS) * gn.reshape(H_B, DV_B).astype(f32)
    o = o.reshape(bsz, s_len, H_B * DV_B) * jax.nn.silu(r_b.astype(f32))
    return o.astype(q_b.dtype)


def swa_mixer(q_c, k_c, v_c, sinks, bias_table):
    bsz, s_len, _ = q_c.shape
    n = s_len // CHUNK
    grp = H_C // KV_C
    band = (WIN_CHUNKS + 1) * CHUNK
    q = q_c.reshape(bsz, n, CHUNK, KV_C, grp, HEAD_DIM)

    def banded(a):
        a = a.reshape(bsz, n, CHUNK, KV_C, HEAD_DIM)
        a = jnp.pad(a, ((0, 0), (WIN_CHUNKS, 0), (0, 0), (0, 0), (0, 0)))
        return jnp.concatenate([a[:, w:w + n] for w in range(WIN_CHUNKS + 1)], axis=2)

    k = banded(k_c)
    v = banded(v_c)
    logits = jnp.einsum('bnqkgd,bnskd->bnkgqs', q, k).astype(jnp.float32) * HEAD_DIM ** -0.5
    qi = jnp.arange(CHUNK, dtype=jnp.int32)[:, None]
    kj = jnp.arange(band, dtype=jnp.int32)[None, :]
    rel = kj - WIN_CHUNKS * CHUNK - qi
    bias = bias_table[:, H_A:][rel_bucket(rel)]
    bias = bias.transpose(2, 0, 1).reshape(KV_C, grp, CHUNK, band).astype(jnp.float32)
    valid = (jnp.arange(n, dtype=jnp.int32)[:, None] * CHUNK + kj - WIN_CHUNKS * CHUNK) >= 0
    logits = jnp.where(valid[None, :, None, None, None, :], logits + bias, -jnp.inf)
    sink = sinks.reshape(KV_C, grp)[:, :, None, None].astype(jnp.float32)
    m = jnp.maximum(jnp.max(logits, axis=-1, keepdims=True), sink)
    e = jnp.exp(logits - m)
    p = (e / (jnp.sum(e, axis=-1, keepdims=True) + jnp.exp(sink - m))).astype(v.dtype)
    o = jnp.einsum('bnkgqs,bnskd->bnqkgd', p, v)
    return o.reshape(bsz, s_len, H_C * HEAD_DIM)


def setup_inputs(seed: int = 0) -> dict:
    key = jax.random.key(seed)
    ks = jax.random.split(key, 20)
    nrm = jax.random.normal
    f32 = jnp.float32
    return {
        'x': nrm(ks[0], (BATCH, SEQ, D_MODEL), f32),
        'w_in': nrm(ks[1], (DEPTH, D_MODEL, D_IN), f32) * D_MODEL ** -0.5,
        'w_out': nrm(ks[2], (DEPTH, D_MIX, D_MODEL), f32) * D_MIX ** -0.5,
        'norm_mix': 1.0 + 0.01 * nrm(ks[3], (DEPTH, D_MODEL), f32),
        'norm_ffn': 1.0 + 0.01 * nrm(ks[4], (DEPTH, D_MODEL), f32),
        'kv_norm': 1.0 + 0.01 * nrm(ks[5], (DEPTH, KV_RANK), f32),
        'w_uk': nrm(ks[6], (DEPTH, KV_RANK, H_A, HEAD_DIM), f32) * KV_RANK ** -0.5,
        'w_uv': nrm(ks[7], (DEPTH, KV_RANK, H_A, HEAD_DIM), f32) * KV_RANK ** -0.5,
        'w_gate2': nrm(ks[8], (DEPTH, GATE_RANK, H_B * DK_B), f32) * GATE_RANK ** -0.5,
        'b_gate': 0.1 * nrm(ks[9], (DEPTH, H_B * DK_B), f32),
        'gla_norm': 1.0 + 0.01 * nrm(ks[10], (DEPTH, H_B * DV_B), f32),
        'sinks': nrm(ks[11], (DEPTH, H_C), f32),
        'rel_bias': 0.5 * nrm(ks[12], (NUM_BUCKETS, H_A + H_C), f32),
        'w_ffn_gate': nrm(ks[13], (DEPTH, D_MODEL, D_FF), f32) * D_MODEL ** -0.5,
        'w_ffn_up': nrm(ks[14], (DEPTH, D_MODEL, D_FF), f32) * D_MODEL ** -0.5,
        'w_ffn_down': nrm(ks[15], (DEPTH, D_FF, D_MODEL), f32) * D_FF ** -0.5,
        'final_norm': 1.0 + 0.01 * nrm(ks[16], (D_MODEL,), f32),
    }


def reference(x, w_in, w_out, norm_mix, norm_ffn, kv_norm, w_uk, w_uv, w_gate2, b_gate,
              gla_norm, sinks, rel_bias, w_ffn_gate, w_ffn_up, w_ffn_down, final_norm):
    for l in range(DEPTH):
        h = rms_norm(x, norm_mix[l])
        p = h @ w_in[l]
        (q_a, c_kv, q_idx, k_idx, w_idx, q_b, k_b, v_b, g_lr, r_b,
         q_c, k_c, v_c) = split_columns(p)
        out_a = dsa_mixer(q_a, c_kv, q_idx, k_idx, w_idx, kv_norm[l], w_uk[l], w_uv[l], rel_bias)
        out_b = gla_mixer(q_b, k_b, v_b, g_lr, r_b, w_gate2[l], b_gate[l], gla_norm[l])
        out_c = swa_mixer(q_c, k_c, v_c, sinks[l], rel_bias)
        x = x + jnp.concatenate([out_a, out_b, out_c], axis=-1) @ w_out[l]
        h = rms_norm(x, norm_ffn[l])
        x = x + (jax.nn.silu(h @ w_ffn_gate[l]) * (h @ w_ffn_up[l])) @ w_ffn_down[l]
    return rms_norm(x, final_norm)
```

```python
import numpy as np
from contextlib import ExitStack
import concourse.bass as bass
import concourse.mybir as mybir
from concourse.bass_utils import run_bass_kernel_spmd

F32 = mybir.dt.float32
BF16 = mybir.dt.bfloat16
ALU = mybir.AluOpType
AF = mybir.ActivationFunctionType
AX = mybir.AxisListType

D = 1024
DFF = 2816
NFF = DFF // 128
EPS = 1e-6
NEG = -30000.0


class Ev:
    __slots__ = ("sem", "val")

    def __init__(self, sem, val):
        self.sem = sem
        self.val = val


class Region:
    __slots__ = ("last_w", "readers", "dsem", "dcnt", "name", "excl")

    def __init__(self, name, excl=False):
        self.name = name
        self.excl = excl
        self.last_w = None
        self.readers = {}
        self.dsem = None
        self.dcnt = 0


class Tile:
    def __init__(self, t, regs):
        self.t = t
        self.r = regs

    def __getitem__(self, idx):
        return self.t[idx]


class Q:
    def __init__(self, kb, eng, name, is_pe=False):
        self.eng = eng
        self.name = name
        self.is_pe = is_pe
        self.sem = kb.newsem("q_" + name)
        self.cnt = 0
        self.known = {}

    def wait(self, ev):
        if ev is None:
            return
        if ev.sem is self.sem and self.is_pe:
            return
        k = id(ev.sem)
        if self.known.get(k, 0) >= ev.val:
            return
        self.eng.wait_ge(ev.sem, ev.val)
        self.known[k] = ev.val


class KB:
    def __init__(self, nc, es):
        self.nc = nc
        self.es = es
        self.nsem = 0
        self.pe = Q(self, nc.tensor, "pe", is_pe=True)
        self.act = Q(self, nc.scalar, "act")
        self.dve = Q(self, nc.vector, "dve")
        self.pool = Q(self, nc.gpsimd, "pool")
        self.sp = Q(self, nc.sync, "sp")
        self.ninst = 0
        self.dregs = []
        self.uid = 0

    def newsem(self, name):
        self.nsem += 1
        return self.es.enter_context(self.nc.semaphore(name))

    def sb(self, name, shape, dt, nreg=1, es=None):
        self.uid += 1
        name = f"{name}_{self.uid}"
        t = (es or self.es).enter_context(self.nc.sbuf_tensor(name, list(shape), dt))
        return Tile(t, [Region(f"{name}.{i}") for i in range(nreg)])

    def barrier(self):
        qs = [self.pe, self.act, self.dve, self.pool, self.sp]
        for q in qs:
            for q2 in qs:
                if q2 is not q and q2.cnt > 0:
                    q.wait(Ev(q2.sem, q2.cnt))
            for r in self.dregs:
                q.wait(Ev(r.dsem, r.dcnt))

    def ps(self, name, shape=(128, 512), dt=F32):
        t = self.es.enter_context(self.nc.psum_tensor(name, list(shape), dt))
        return Tile(t, [Region(name, excl=True)])

    def dram(self, name, shape, dt, nreg=1, kind="Internal"):
        t = self.nc.dram_tensor(name, list(shape), dt, kind=kind)
        return Tile(t.ap(), [Region(f"{name}.{i}") for i in range(nreg)])

    def _deps(self, q, reads, writes):
        for r in reads:
            q.wait(r.last_w)
            if r.excl:
                for ev in r.readers.values():
                    if ev.sem is not q.sem:
                        q.wait(ev)
        for w in writes:
            q.wait(w.last_w)
            for ev in w.readers.values():
                q.wait(ev)

    def _record(self, ev, reads, writes):
        for r in reads:
            r.readers[id(ev.sem)] = ev
        for w in writes:
            w.last_w = ev
            w.readers = {}

    def op(self, q, fn, reads=(), writes=(), inc=True):
        self._deps(q, reads, writes)
        inst = fn(q.eng)
        self.ninst += 1
        ev = Ev(q.sem, q.cnt + 1)
        if inc:
            inst.then_inc(q.sem, 1)
            q.cnt += 1
        self._record(ev, reads, writes)
        return ev

    def dma(self, q, out, in_, reads, writes, semreg, **kw):
        self._deps(q, reads, writes)
        if semreg.dsem is None:
            semreg.dsem = self.newsem("d_" + semreg.name.replace(".", "_"))
            self.dregs.append(semreg)
        inst = q.eng.dma_start(out=out, in_=in_, **kw)
        semreg.dcnt += 16
        inst.then_inc(semreg.dsem, 16)
        self.ninst += 1
        ev = Ev(semreg.dsem, semreg.dcnt)
        self._record(ev, reads, writes)
        return ev


def _bucket(rel):
    rel = np.asarray(rel, dtype=np.int64)
    n = np.abs(rel)
    side = np.where(rel > 0, 16, 0)
    nf = np.maximum(n, 1).astype(np.float32)
    large = 8 + (np.log(nf / np.float32(8)) / np.float32(np.log(16.0)) * np.float32(8)).astype(np.int32)
    large = np.minimum(large, 15)
    return (side + np.where(n < 8, n, large)).astype(np.int64)


def _host_consts(rel_bias):
    rel_bias = np.asarray(rel_bias, dtype=np.float32)
    s = np.arange(128)[:, None]
    q = np.arange(128)[None, :]
    ip = _bucket(s - 128 - q)
    idg = _bucket(s - q)
    g = np.stack([rel_bias[ip], rel_bias[idg]])
    cA = np.ascontiguousarray(g[..., 0:4].transpose(0, 1, 3, 2)).reshape(2, 128, 512)
    cC = np.ascontiguousarray(g[..., 4:8].transpose(0, 1, 3, 2)).reshape(2, 128, 512)
    bfar = np.ascontiguousarray(rel_bias[15:16, 0:4])
    mp = np.where((q >= 64) & (s < 64), NEG, 0.0).astype(np.float32)
    md = np.where((q < 64) & (s >= 64), NEG, 0.0).astype(np.float32)
    mC = np.stack([np.broadcast_to(mp[:, None, :], (128, 4, 128)),
                   np.broadcast_to(md[:, None, :], (128, 4, 128))]).reshape(2, 128, 512)
    return cA, cC, np.ascontiguousarray(mC, dtype=np.float32), bfar


_FCH = [
    ("qa0", [(0, 128)]), ("qa1", [(128, 128)]),
    ("qi0", [(384, 128)]), ("qi1", [(512, 128)]),
    ("ki", [(640, 64), (640, 64)]),
    ("glr", [(1732, 16)]),
    ("qb0", [(708, 128)]), ("qb1", [(836, 128)]),
    ("kb0", [(964, 128)]), ("kb1", [(1092, 128)]),
    ("rb0", [(1748, 128)]), ("rb1", [(1876, 128)]), ("rb2", [(2004, 128)]), ("rb3", [(2132, 128)]),
    ("qc0", [(2260, 64), (2388, 64)]), ("qc1", [(2324, 64), (2452, 64)]),
    ("kc", [(2516, 128)]),
]
_TCH = [("A", [(256, 128), (704, 4)]), ("B", [(964, 256), (2644, 128)]), ("C", [(1220, 512)])]


def build(S=4096, NSEQ=2, DEPTH=2, do_mixer=True, do_ffn=True, xbuf=1, parts=("gla", "swa", "dsa"), stage=99):
    nc = bass.Bass("TRN2", target_bir_lowering=False)
    NT = NSEQ * S
    TT = 512
    NTILE = NT // TT
    TPS = S // TT
    NB = S // 128

    def din(name, shape):
        return nc.dram_tensor(name, list(shape), F32, kind="ExternalInput").ap()

    x_in = din("x", [NT, D])
    w_in = din("w_in", [DEPTH, D, 2772])
    w_out = din("w_out", [DEPTH, D, D])
    n_mix = din("norm_mix", [DEPTH, D])
    n_ffn = din("norm_ffn", [DEPTH, D])
    kv_norm = din("kv_norm", [DEPTH, 128])
    w_uk = din("w_uk", [DEPTH, 128, 256])
    w_uv = din("w_uv", [DEPTH, 128, 256])
    w_g2 = din("w_gate2", [DEPTH, 16, 256])
    b_g = din("b_gate", [DEPTH, 256])
    gla_n = din("gla_norm", [DEPTH, 512])
    sinks = din("sinks", [DEPTH, 4])
    w_g = din("w_ffn_gate", [DEPTH, D, DFF])
    w_u = din("w_ffn_up", [DEPTH, D, DFF])
    w_d = din("w_ffn_down", [DEPTH, DFF, D])
    n_fin = din("final_norm", [1, D])
    cA_d = din("cA", [2, 128, 512])
    cC_d = din("cC", [2, 128, 512])
    mC_d = din("mC", [2, 128, 512])
    bfar_d = din("bfar", [1, 4])
    y_out = nc.dram_tensor("y", [NT, D], F32, kind="ExternalOutput").ap()

    with ExitStack() as es:
        kb = KB(nc, es)
        pe, act, dve, pool, sp = kb.pe, kb.act, kb.dve, kb.pool, kb.sp

        xs = kb.dram("xs", [NT, D], F32, nreg=NTILE)
        xin_regs = [Region(f"xin.{i}") for i in range(NTILE)]
        yout_regs = [Region(f"yout.{i}") for i in range(NTILE)]

        def MM(out, lhsT, rhs, reads, wr, start=True, stop=True, inc=True):
            return kb.op(pe, lambda e: e.matmul(out, lhsT=lhsT, rhs=rhs, start=start, stop=stop),
                         reads=reads, writes=wr, inc=inc)

        def ACT(out, in_, func, reads, wr, **kw):
            return kb.op(act, lambda e: e.activation(out=out, in_=in_, func=func, **kw), reads=reads, writes=wr)

        identf = kb.sb("identf", [128, 128], F32)
        ident = kb.sb("ident", [128, 128], BF16)
        ident4 = kb.sb("ident4", [128, 4, 128], BF16)
        ones_bf = kb.sb("ones_bf", [128, 128], BF16)
        tribd = kb.sb("tribd", [128, 128], F32)
        ones_f = kb.sb("ones_f", [128, 128], F32)
        ramp512 = kb.sb("ramp512", [128, 512], F32)
        cmask = kb.sb("cmask", [128, 2], F32)
        subd = kb.sb("subd", [128, 128], F32)
        epsc = kb.sb("epsc", [128, 1], F32)
        onec = kb.sb("onec", [128, 1], F32)
        thr_all = kb.sb("thr_all", [128, 1], F32)
        kb.op(pool, lambda e: e.memset(identf[:], 0.0), writes=identf.r)
        kb.op(pool, lambda e: e.affine_select(out=identf[:], in_=identf[:], pattern=[[-1, 128]],
                                              compare_op=ALU.not_equal, fill=1.0, base=0, channel_multiplier=1),
              reads=identf.r, writes=identf.r)
        kb.op(dve, lambda e: e.tensor_copy(out=ident[:], in_=identf[:]), reads=identf.r, writes=ident.r)
        for h in range(4):
            kb.op(dve, lambda e: e.tensor_copy(out=ident4[:, h, :], in_=identf[:]), reads=identf.r, writes=ident4.r)
        kb.op(pool, lambda e: e.memset(ones_bf[:], 1.0), writes=ones_bf.r)
        kb.op(pool, lambda e: e.memset(epsc[:], EPS), writes=epsc.r)
        kb.op(pool, lambda e: e.memset(onec[:], 1.0), writes=onec.r)
        kb.op(pool, lambda e: e.memset(thr_all[:], -1e29), writes=thr_all.r)
        kb.op(pool, lambda e: e.memset(tribd[:], 1.0), writes=tribd.r)
        kb.op(pool, lambda e: e.affine_select(out=tribd[:], in_=tribd[:], pattern=[[1, 128]],
                                              compare_op=ALU.is_ge, fill=0.0, base=0, channel_multiplier=-1),
              reads=tribd.r, writes=tribd.r)
        kb.op(pool, lambda e: e.memset(tribd[0:64, 64:128], 0.0), reads=tribd.r, writes=tribd.r)
        kb.op(pool, lambda e: e.memset(cmask[:], 0.0), writes=cmask.r)
        kb.op(pool, lambda e: e.memset(cmask[0:64, 0:1], 1.0), reads=cmask.r, writes=cmask.r)
        kb.op(pool, lambda e: e.memset(cmask[64:128, 1:2], 1.0), reads=cmask.r, writes=cmask.r)
        kb.op(pool, lambda e: e.memset(subd[:], 1.0), writes=subd.r)
        kb.op(pool, lambda e: e.affine_select(out=subd[:], in_=subd[:], pattern=[[-1, 128]],
                                              compare_op=ALU.is_gt, fill=0.0, base=0, channel_multiplier=1),
              reads=subd.r, writes=subd.r)
        kb.op(pool, lambda e: e.memset(subd[64:128, 0:64], 0.0), reads=subd.r, writes=subd.r)

        TIE = 1e-9
        kb.op(pool, lambda e: e.memset(ones_f[:], 1.0), writes=ones_f.r)
        gfin = kb.sb("gfin", [128, D], F32)
        kb.dma(sp, gfin[:], n_fin[0].partition_broadcast(128), reads=(), writes=gfin.r, semreg=gfin.r[0])
        gffn = kb.sb("gffn", [128, DEPTH * 8], F32)
        gmix = kb.sb("gmix", [128, DEPTH * 8], F32)
        with nc.allow_non_contiguous_dma(reason="tiny gain vectors"):
            kb.dma(sp, gffn[:], n_ffn.rearrange("l (k p) -> p (l k)", p=128), reads=(), writes=gffn.r,
                   semreg=gffn.r[0])
            kb.dma(sp, gmix[:], n_mix.rearrange("l (k p) -> p (l k)", p=128), reads=(), writes=gmix.r,
                   semreg=gmix.r[0])

        X = [kb.sb(f"X{i}", [128, 4, D], F32) for i in range(xbuf)]
        ss = kb.sb("ss", [128, 4], F32)
        rstd = kb.sb("rstd", [128, 4], F32)
        hT = kb.sb("hT", [128, 8, TT], BF16)
        PS = [kb.ps(f"ps{i}") for i in range(8)]
        gstate = {"i": 0}

        def gp():
            gstate["i"] += 1
            return PS[gstate["i"] % 4]

        pr = PS[0]
        MM(pr[:, 0:128], ones_f[:], tribd[:], ones_f.r + tribd.r, pr.r)
        for c8 in range(8):
            kb.op(dve, lambda e: e.tensor_scalar(out=ramp512[:, c8 * 64:(c8 + 1) * 64],
                                                 in0=pr[:, 0:64], scalar1=-TIE,
                                                 scalar2=-TIE * 64 * c8, op0=ALU.mult, op1=ALU.add),
                  reads=pr.r, writes=ramp512.r)

        def rstd_from_ss():
            ACT(rstd[:], ss[:], AF.Sqrt, ss.r + epsc.r, rstd.r, bias=epsc[:], scale=1.0 / D)
            kb.op(dve, lambda e: e.reciprocal(out=rstd[:], in_=rstd[:]), reads=rstd.r, writes=rstd.r)

        def rmsnorm_T(Xt, gain_cols, xn, junk):
            for s in range(4):
                ACT(junk[:], Xt[:, s, :], AF.Square, Xt.r, junk.r + ss.r, accum_out=ss[:, s:s + 1])
            rstd_from_ss()
            for s in range(4):
                ACT(xn[:, s, :], Xt[:, s, :], AF.Copy, Xt.r + rstd.r, xn.r, scale=rstd[:, s:s + 1])
            for c in range(8):
                p = gp()
                for s in range(4):
                    MM(p[:, s * 128:(s + 1) * 128], xn[:, s, c * 128:(c + 1) * 128], ident[:],
                       xn.r + ident.r, p.r, inc=(s == 3))
                kb.op(dve, lambda e: e.tensor_scalar_mul(out=hT[:, c, :], in0=p[:], scalar1=gain_cols[c]),
                      reads=p.r, writes=hT.r)

        def ffn_phase(l, src, src_regs, last, pes):
            Wg = kb.sb("Wg", [128, 8, DFF], BF16, es=pes)
            Wu = kb.sb("Wu", [128, 8, DFF], BF16, es=pes)
            Wd = kb.sb("Wd", [128, NFF, D], BF16, es=pes)
            aT = kb.sb("aT", [128, NFF, TT], BF16, es=pes)
            xn = Tile(aT[:, 0:8, :].rearrange("p (s a) t -> p s (a t)", s=4), aT.r)
            sg = [kb.sb(f"sg{i}", [128, TT], F32, es=pes) for i in range(2)]
            junk = kb.sb("junk", [128, D], BF16, es=pes)
            HC = DFF // 2
            for hh in range(2):
                kb.dma(pool, Wg[:, :, hh * HC:(hh + 1) * HC],
                       w_g[l].rearrange("(k p) n -> p k n", p=128)[:, :, hh * HC:(hh + 1) * HC],
                       reads=(), writes=Wg.r, semreg=Wg.r[0])
                kb.dma(pool, Wu[:, :, hh * HC:(hh + 1) * HC],
                       w_u[l].rearrange("(k p) n -> p k n", p=128)[:, :, hh * HC:(hh + 1) * HC],
                       reads=(), writes=Wu.r, semreg=Wu.r[0])
            for hh in range(2):
                kb.dma(pool, Wd[:, hh * 11:(hh + 1) * 11, :],
                       w_d[l].rearrange("(k p) n -> p k n", p=128)[:, hh * 11:(hh + 1) * 11, :],
                       reads=(), writes=Wd.r, semreg=Wd.r[0])
            gcols = [gffn[:, l * 8 + c:l * 8 + c + 1] for c in range(8)]
            for t in range(NTILE):
                Xt = X[t % xbuf]
                kb.dma(sp, Xt[:], src[t * TT:(t + 1) * TT, :].rearrange("(s p) d -> p s d", p=128),
                       reads=[src_regs[t]], writes=Xt.r, semreg=Xt.r[0])
                rmsnorm_T(Xt, gcols, xn, junk)
                for f in range(NFF):
                    pg = PS[4 + (f % 2)]
                    pu = PS[6 + (f % 2)]
                    for k in range(8):
                        MM(pg[:], Wg[:, k, f * 128:(f + 1) * 128], hT[:, k, :], Wg.r + hT.r, pg.r,
                           start=(k == 0), stop=(k == 7), inc=(k == 7))
                    for k in range(8):
                        MM(pu[:], Wu[:, k, f * 128:(f + 1) * 128], hT[:, k, :], Wu.r + hT.r, pu.r,
                           start=(k == 0), stop=(k == 7), inc=(k == 7))
                    sgt = sg[f % 2]
                    ACT(sgt[:], pg[:], AF.Silu, pg.r, sgt.r)
                    kb.op(dve, lambda e: e.tensor_tensor(out=aT[:, f, :], in0=sgt[:], in1=pu[:], op=ALU.mult),
                          reads=sgt.r + pu.r, writes=aT.r)
                for s in range(4):
                    for hh in range(2):
                        po = gp()
                        for f in range(NFF):
                            MM(po[:], aT[:, f, s * 128:(s + 1) * 128], Wd[:, f, hh * 512:(hh + 1) * 512],
                               aT.r + Wd.r, po.r, start=(f == 0), stop=(f == NFF - 1), inc=(f == NFF - 1))
                        kb.op(dve, lambda e: e.tensor_tensor(
                            out=Xt[:, s, hh * 512:(hh + 1) * 512], in0=Xt[:, s, hh * 512:(hh + 1) * 512],
                            in1=po[:], op=ALU.add), reads=Xt.r + po.r, writes=Xt.r)
                if last:
                    for s in range(4):
                        ACT(junk[:], Xt[:, s, :], AF.Square, Xt.r, junk.r + ss.r, accum_out=ss[:, s:s + 1])
                    rstd_from_ss()
                    for s in range(4):
                        kb.op(dve, lambda e: e.scalar_tensor_tensor(
                            out=Xt[:, s, :], in0=Xt[:, s, :], scalar=rstd[:, s:s + 1], in1=gfin[:],
                            op0=ALU.mult, op1=ALU.mult), reads=Xt.r + rstd.r + gfin.r, writes=Xt.r)
                    kb.dma(sp, y_out[t * TT:(t + 1) * TT, :].rearrange("(s p) d -> p s d", p=128), Xt[:],
                           reads=Xt.r, writes=[yout_regs[t]], semreg=Xt.r[0])
                else:
                    kb.dma(sp, xs[t * TT:(t + 1) * TT, :].rearrange("(s p) d -> p s d", p=128), Xt[:],
                           reads=Xt.r, writes=[xs.r[t]], semreg=Xt.r[0])

        def mixer_phase(l, src, src_regs, pes):
            def sb(name, shape, dt, nreg=1):
                return kb.sb(name, shape, dt, nreg=nreg, es=pes)

            foff, fw, off = {}, {}, 0
            for name, pieces in _FCH:
                foff[name] = off
                fw[name] = sum(n for _, n in pieces)
                off += fw[name]
            NFC = off
            toff, tw, off = {}, {}, 0
            for name, pieces in _TCH:
                toff[name] = off
                tw[name] = sum(n for _, n in pieces)
                off += tw[name]
            NTC = off
            Wf = sb("Wf", [128, 8, NFC], BF16)
            Wt = sb("Wt", [128, 8, NTC], BF16)
            Wo = sb("Wo", [128, 8, D], BF16)
            wsrc = w_in[l].rearrange("(k p) n -> p k n", p=128)
            for name, pieces in _FCH:
                o = foff[name]
                for c0, n in pieces:
                    kb.dma(pool, Wf[:, :, o:o + n], wsrc[:, :, c0:c0 + n], reads=(), writes=Wf.r, semreg=Wf.r[0])
                    o += n
            for name, pieces in _TCH:
                o = toff[name]
                for c0, n in pieces:
                    kb.dma(pool, Wt[:, :, o:o + n], wsrc[:, :, c0:c0 + n], reads=(), writes=Wt.r, semreg=Wt.r[0])
                    o += n
            for hh in range(2):
                kb.dma(pool, Wo[:, :, hh * 512:(hh + 1) * 512],
                       w_out[l].rearrange("(k p) n -> p k n", p=128)[:, :, hh * 512:(hh + 1) * 512],
                       reads=(), writes=Wo.r, semreg=Wo.r[0])

            kvn = sb("kvn", [128, 128], F32)
            wukn = sb("wukn", [128, 256], F32)
            wuvn = sb("wuvn", [128, 256], F32)
            wg2a = sb("wg2a", [33, 256], F32)
            gn = sb("gn", [128, 4], F32)
            sinkb = sb("sinkb", [128, 4], F32)
            bfar = sb("bfar", [128, 4], F32)
            cst = sb("cst", [128, 512], F32)
            cst2 = sb("cst2", [128, 512], F32)
            kb.dma(sp, kvn[:], kv_norm[l].partition_broadcast(128), reads=(), writes=kvn.r, semreg=kvn.r[0])
            kb.dma(sp, wukn[:], w_uk[l], reads=(), writes=wukn.r, semreg=wukn.r[0])
            kb.dma(sp, wuvn[:], w_uv[l], reads=(), writes=wuvn.r, semreg=wuvn.r[0])
            kb.op(pool, lambda e: e.memset(wg2a[:], 0.0), writes=wg2a.r)
            kb.dma(sp, wg2a[0:16, :], w_g2[l], reads=(), writes=wg2a.r, semreg=wg2a.r[0])
            kb.dma(sp, wg2a[32:33, :], b_g[l:l + 1, :], reads=(), writes=wg2a.r, semreg=wg2a.r[0])
            with nc.allow_non_contiguous_dma(reason="tiny gain vectors"):
                kb.dma(sp, gn[:], gla_n[l].rearrange("(h e) -> e h", e=128), reads=(), writes=gn.r, semreg=gn.r[0])
            kb.dma(sp, sinkb[:], sinks[l].partition_broadcast(128), reads=(), writes=sinkb.r, semreg=sinkb.r[0])
            kb.dma(sp, bfar[:], bfar_d[0].partition_broadcast(128), reads=(), writes=bfar.r, semreg=bfar.r[0])

            if stage < 1:
                return
            wukT = sb("wukT", [128, 2, 128], BF16)
            wuv = sb("wuv", [128, 256], BF16)
            for i in range(2):
                p = gp()
                MM(p[:, 0:128], wukn[:, i * 128:(i + 1) * 128], identf[:], wukn.r + identf.r, p.r)
                ACT(wukT[:, i, :], p[:, 0:128], AF.Copy, p.r, wukT.r, scale=0.125)
            kb.op(dve, lambda e: e.tensor_copy(out=wuv[:], in_=wuvn[:]), reads=wuvn.r, writes=wuv.r)
            sinkexp = sb("sinkexp", [128, 4, 128], F32)
            ACT(sinkb[:], sinkb[:], AF.Exp, sinkb.r, sinkb.r)
            kb.op(pool, lambda e: e.memset(sinkexp[:], 0.0), writes=sinkexp.r)
            for h in range(4):
                kb.op(dve, lambda e: e.tensor_scalar_add(out=sinkexp[:, h, :], in0=sinkexp[:, h, :],
                                                         scalar1=sinkb[:, h:h + 1]),
                      reads=sinkexp.r + sinkb.r, writes=sinkexp.r)
            biasA = [sb(f"biasA{i}", [128, 4, 128], BF16) for i in range(2)]
            biasC = [sb(f"biasC{i}", [128, 4, 128], BF16) for i in range(2)]
            for i in range(2):
                kb.dma(sp, cst[:], cA_d[i], reads=(), writes=cst.r, semreg=cst.r[0])
                for h in range(4):
                    kb.op(dve, lambda e: e.tensor_scalar_sub(out=biasA[i][:, h, :], in0=cst[:, h * 128:(h + 1) * 128],
                                                             scalar1=bfar[:, h:h + 1]),
                          reads=cst.r + bfar.r, writes=biasA[i].r)
                kb.dma(sp, cst[:], cC_d[i], reads=(), writes=cst.r, semreg=cst.r[0])
                kb.dma(sp, cst2[:], mC_d[i], reads=(), writes=cst2.r, semreg=cst2.r[0])
                kb.op(dve, lambda e: e.tensor_tensor(out=biasC[i][:].rearrange("p h q -> p (h q)"), in0=cst[:],
                                                     in1=cst2[:], op=ALU.add),
                      reads=cst.r + cst2.r, writes=biasC[i].r)

            if stage < 2:
                return
            kiT = sb("kiT", [128, S], BF16, nreg=NB)
            cT = sb("cT", [128, S], BF16, nreg=NB)
            ctok = sb("ctok", [128, NB, 128], BF16, nreg=NB)
            kcT = sb("kcT", [128, 8, 128], BF16, nreg=8)
            vctok = sb("vctok", [128, 8, 128], BF16, nreg=8)
            St = sb("St", [128, 2, 128], F32)
            Sbf = [sb(f"Sbf{i}", [128, 2, 128], BF16) for i in range(2)]
            qaT = sb("qaT", [128, 2, TT], BF16)
            qlT = sb("qlT", [128, 4, TT], BF16)
            qiT = sb("qiT", [128, 2, TT], BF16)
            glra = sb("glra", [33, TT], F32)
            sptok1 = sb("sptok", [128, 256], F32)
            eDk = sb("eDk", [128, 256], F32)
            EbT = sb("EbT", [128, 2, TT], F32)
            EnT = sb("EnT", [128, 2, TT], BF16)
            qeT = sb("qeT", [128, 2, TT], BF16)
            keT = sb("keT", [128, 2, TT], BF16)
            SrgT = sb("SrgT", [128, 4, TT], BF16)
            qcT = sb("qcT", [128, 2, TT], BF16)
            kdtok = sb("kdtok", [128, 4, 256], BF16, nreg=4)
            vbtok = sb("vbtok", [128, 4, 512], BF16, nreg=4)
            widx = sb("widx", [128, 4, 4], F32, nreg=4)
            ssc = sb("ssc", [128, 1], F32)
            rc = sb("rc", [128, 1], F32)
            mixT = hT
            attm = sb("attm", [128, 4, 128], BF16)
            kdm = sb("kdm", [128, 2, 256], BF16)
            sqb = sb("sqb", [128, 512], BF16)
            PT = [sb(f"PT{i}", [128, 512], BF16) for i in range(2)]
            assert xbuf == 1
            score = Tile(X[0][:].rearrange("p s d -> p (s d)"), X[0].r)
            Wk = sb("Wk", [128, max(S, 2048)], F32)
            xn = Tile(Wk[:, 0:2048].bitcast(BF16).rearrange("p (s d) -> p s d", s=4), Wk.r)
            negm = sb("negm", [128, max(S, 1024)], BF16)
            junk = Tile(negm[:, 0:1024], negm.r)
            tmpR = [cst, cst2]
            t1, rsb = cst, cst2
            m8 = sb("m8", [128, 8], F32)
            olT = sb("olT", [128, 4, 128], BF16)
            pstate = {"i": 0}

            def nextPT():
                pstate["i"] += 1
                return PT[pstate["i"] % 2]

            kb.op(pool, lambda e: e.memset(glra[:], 0.0), writes=glra.r)
            kb.op(pool, lambda e: e.memset(glra[32:33, :], 1.0), reads=glra.r, writes=glra.r)
            gcols = [gmix[:, l * 8 + c:l * 8 + c + 1] for c in range(8)]

            def fproj(name):
                o, n = foff[name], fw[name]
                p = gp()
                for k in range(8):
                    MM(p[0:n, :], Wf[:, k, o:o + n], hT[:, k, :], Wf.r + hT.r, p.r,
                       start=(k == 0), stop=(k == 7), inc=(k == 7))
                return p

            def tproj(name, blk):
                o, n = toff[name], tw[name]
                p = gp()
                for k in range(8):
                    MM(p[:, 0:n], hT[:, k, blk * 128:(blk + 1) * 128], Wt[:, k, o:o + n], Wt.r + hT.r, p.r,
                       start=(k == 0), stop=(k == 7), inc=(k == 7))
                return p

            for b in range(NSEQ):
                kb.op(pool, lambda e: e.memset(St[:], 0.0), reads=St.r, writes=St.r)
                for t in range(TPS):
                    gt = b * TPS + t
                    Xt = X[gt % xbuf]
                    kb.dma(sp, Xt[:], src[gt * TT:(gt + 1) * TT, :].rearrange("(s p) d -> p s d", p=128),
                           reads=[src_regs[gt]], writes=Xt.r, semreg=Xt.r[0])
                    rmsnorm_T(Xt, gcols, xn, junk)
                    if stage < 3:
                        continue

                    for i in range(2):
                        p = fproj(f"qa{i}")
                        ACT(qaT[:, i, :], p[:], AF.Copy, p.r, qaT.r)
                    for h in range(4):
                        p = gp()
                        hp = (h % 2) * 64
                        MM(p[:], wukT[hp:hp + 64, h // 2, :], qaT[hp:hp + 64, h // 2, :], wukT.r + qaT.r, p.r)
                        kb.op(dve, lambda e: e.tensor_copy(out=qlT[:, h, :], in_=p[:]), reads=p.r, writes=qlT.r)
                    if stage == 31:
                        continue
                    for i in range(2):
                        p = fproj(f"qi{i}")
                        ACT(qiT[:, i, :], p[:], AF.Copy, p.r, qiT.r)
                    p = fproj("ki")
                    kb.op(dve, lambda e: e.tensor_copy(out=kiT[:, t * TT:(t + 1) * TT], in_=p[:]), reads=p.r,
                          writes=kiT.r[t * 4:t * 4 + 4])
                    p = fproj("glr")
                    kb.op(dve, lambda e: e.tensor_copy(out=glra[0:16, :], in_=p[0:16, :]), reads=p.r, writes=glra.r)
                    if stage == 32:
                        continue
                    for i in range(2):
                        p = fproj(f"qc{i}")
                        ACT(qcT[:, i, :], p[:], AF.Copy, p.r, qcT.r, scale=0.125)
                    p = fproj("kc")
                    for blk in range(4):
                        sl = (t * 4 + blk) % 8
                        kb.op(dve, lambda e: e.tensor_copy(out=kcT[:, sl, :], in_=p[:, blk * 128:(blk + 1) * 128]),
                              reads=p.r, writes=[kcT.r[sl]])
                    if stage == 33:
                        continue
                    for i in range(4):
                        p = fproj(f"rb{i}")
                        ACT(SrgT[:, i, :], p[:], AF.Silu, p.r, SrgT.r)
                        kb.op(dve, lambda e: e.tensor_scalar_mul(out=SrgT[:, i, :], in0=SrgT[:, i, :],
                                                                   scalar1=gn[:, i:i + 1]),
                              reads=SrgT.r + gn.r, writes=SrgT.r)

                    if stage < 4 or stage == 41:
                        continue
                    pG = [PS[4], PS[5]]
                    for blk in range(4):
                        jb = t * 4 + blk
                        sl = jb % 8
                        p = tproj("A", blk)
                        ACT(junk[:, 0:128], p[:, 0:128], AF.Square, p.r, junk.r + ssc.r, accum_out=ssc[:])
                        ACT(rc[:], ssc[:], AF.Sqrt, ssc.r + epsc.r, rc.r, bias=epsc[:], scale=1.0 / 128)
                        kb.op(dve, lambda e: e.reciprocal(out=rc[:], in_=rc[:]), reads=rc.r, writes=rc.r)
                        kb.op(dve, lambda e: e.scalar_tensor_tensor(out=ctok[:, jb, :], in0=p[:, 0:128], scalar=rc[:],
                                                                    in1=kvn[:], op0=ALU.mult, op1=ALU.mult),
                              reads=p.r + rc.r + kvn.r, writes=[ctok.r[jb]])
                        kb.op(dve, lambda e: e.tensor_copy(out=widx[:, blk, :], in_=p[:, 128:132]), reads=p.r,
                              writes=[widx.r[blk]])
                        p2 = gp()
                        MM(p2[:, 0:128], ctok[:, jb, :], ident[:], [ctok.r[jb]] + ident.r, p2.r)
                        ACT(cT[:, jb * 128:(jb + 1) * 128], p2[:, 0:128], AF.Copy, p2.r, [cT.r[jb]])
                        if stage == 42:
                            continue
                        pz = gp()
                        MM(pz[:, 0:256], glra[0:33, blk * 128:(blk + 1) * 128], wg2a[0:33, :], glra.r + wg2a.r, pz.r)
                        ACT(sptok1[:], pz[:, 0:256], AF.Exp, pz.r, sptok1.r, scale=-1.0)
                        ACT(sptok1[:], sptok1[:], AF.Ln, sptok1.r + onec.r, sptok1.r,
                            bias=onec[:], scale=1.0)
                        if stage == 43:
                            continue
                        for i in range(2):
                            MM(pG[i][:, blk * 128:(blk + 1) * 128], sptok1[:, i * 128:(i + 1) * 128], tribd[:],
                               sptok1.r + tribd.r, pG[i].r)
                        pd = gp()
                        MM(pd[:, 0:256], subd[:], sptok1[:], sptok1.r + subd.r, pd.r)
                        ACT(eDk[:], pd[:, 0:256], AF.Exp, pd.r, eDk.r, scale=-1.0 / 16)
                        if stage == 44:
                            continue
                        p = tproj("B", blk)
                        if stage == 46:
                            continue
                        if stage != 49:
                            kb.op(dve, lambda e: e.tensor_tensor(out=kdtok[:, blk, :], in0=p[:, 0:256], in1=eDk[:],
                                                                 op=ALU.mult),
                                  reads=p.r + eDk.r, writes=[kdtok.r[blk]])
                        if stage == 48:
                            continue
                        ACT(vctok[:, sl, :], p[:, 256:384], AF.Copy, p.r, [vctok.r[sl]])
                        if stage in (47, 49):
                            continue
                        p = tproj("C", blk)
                        ACT(vbtok[:, blk, :], p[:], AF.Copy, p.r, [vbtok.r[blk]])
                    if stage in (42, 43, 44, 45, 46, 47, 48, 49):
                        continue
                    for i in range(2):
                        ACT(EbT[:, i, :], pG[i][:], AF.Exp, pG[i].r, EbT.r, scale=-1.0 / 16)
                        ACT(EnT[:, i, :], pG[i][:], AF.Exp, pG[i].r, EnT.r, scale=1.0 / 16)
                    for i in range(2):
                        p = fproj(f"qb{i}")
                        kb.op(dve, lambda e: e.scalar_tensor_tensor(out=qeT[:, i, :], in0=p[:], scalar=0.125,
                                                                    in1=EbT[:, i, :], op0=ALU.mult, op1=ALU.mult),
                              reads=p.r + EbT.r, writes=qeT.r)
                        p = fproj(f"kb{i}")
                        kb.op(dve, lambda e: e.tensor_tensor(out=keT[:, i, :], in0=p[:], in1=EnT[:, i, :], op=ALU.mult),
                              reads=p.r + EnT.r, writes=keT.r)

                    if stage < 5:
                        continue
                    if len(parts) < 3:
                        kb.op(pool, lambda e: e.memset(mixT[:], 0.0), reads=mixT.r, writes=mixT.r)
                    for blk in range(4 if "gla" in parts else 0):
                        c0 = blk * 128
                        pA2 = [gp(), gp()]
                        for h in range(4):
                            hp = (h % 2) * 64
                            MM(pA2[h % 2][:, (h // 2) * 128:(h // 2 + 1) * 128], keT[hp:hp + 64, h // 2, c0:c0 + 128],
                               qeT[hp:hp + 64, h // 2, c0:c0 + 128], keT.r + qeT.r, pA2[h % 2].r)
                        for h in range(4):
                            kb.op(dve, lambda e: e.tensor_tensor(out=attm[:, h, :],
                                                                 in0=pA2[h % 2][:, (h // 2) * 128:(h // 2 + 1) * 128],
                                                                 in1=tribd[:], op=ALU.mult),
                                  reads=pA2[h % 2].r + tribd.r, writes=attm.r)
                        for cc in range(2):
                            kb.op(dve, lambda e: e.tensor_copy(out=Sbf[cc][:], in_=St[:]), reads=St.r, writes=Sbf[cc].r)
                            kb.op(dve, lambda e: e.tensor_scalar_mul(out=kdm[:, cc, :], in0=kdtok[:, blk, :],
                                                                     scalar1=cmask[:, cc:cc + 1]),
                                  reads=[kdtok.r[blk]] + cmask.r, writes=kdm.r)
                            pK = gp()
                            for h in range(4):
                                hp = (h % 2) * 64
                                MM(pK[hp:hp + 64, (h // 2) * 128:(h // 2 + 1) * 128],
                                   kdm[:, cc, h * 64:(h + 1) * 64], vbtok[:, blk, h * 128:(h + 1) * 128],
                                   kdm.r + [vbtok.r[blk]], pK.r, inc=(h == 3))
                            col = c0 + cc * 64 + 63
                            for i in range(2):
                                kb.op(dve, lambda e: e.scalar_tensor_tensor(
                                    out=St[:, i, :], in0=St[:, i, :], scalar=EbT[:, i, col:col + 1],
                                    in1=pK[:, i * 128:(i + 1) * 128], op0=ALU.mult, op1=ALU.add),
                                    reads=St.r + EbT.r + pK.r, writes=St.r)
                        import os as _os
                        if _os.environ.get("GSKIP") == "1":
                            continue
                        pO = gp()
                        for h in range(4):
                            hp = (h % 2) * 64
                            for cc in range(2):
                                oc = h * 128 + cc * 64
                                MM(pO[:, oc:oc + 64], vbtok[:, blk, h * 128:(h + 1) * 128],
                                   attm[:, h, cc * 64:cc * 64 + 64], [vbtok.r[blk]] + attm.r, pO.r,
                                   start=True, stop=False, inc=False)
                                MM(pO[:, oc:oc + 64], Sbf[cc][hp:hp + 64, h // 2, :],
                                   qeT[hp:hp + 64, h // 2, c0 + cc * 64:c0 + cc * 64 + 64], Sbf[cc].r + qeT.r, pO.r,
                                   start=False, stop=True, inc=(h == 3 and cc == 1))
                        if _os.environ.get("GSKIP") == "2":
                            continue
                        ACT(sqb[:], pO[:], AF.Square, pO.r, sqb.r)
                        pN = gp()
                        MM(pN[:], ones_bf[:], sqb[:], ones_bf.r + sqb.r, pN.r)
                        ACT(rsb[:], pN[:], AF.Sqrt, pN.r + epsc.r, rsb.r, bias=epsc[:], scale=1.0 / 128)
                        kb.op(dve, lambda e: e.reciprocal(out=rsb[:], in_=rsb[:]), reads=rsb.r, writes=rsb.r)
                        kb.op(dve, lambda e: e.tensor_tensor(out=t1[:], in0=pO[:], in1=rsb[:], op=ALU.mult),
                              reads=pO.r + rsb.r, writes=t1.r)
                        kb.op(dve, lambda e: e.tensor_tensor(out=mixT[:, 2:6, c0:c0 + 128],
                                                              in0=t1[:].rearrange("p (h q) -> p h q", h=4),
                                                              in1=SrgT[:, :, c0:c0 + 128], op=ALU.mult),
                              reads=t1.r + SrgT.r, writes=mixT.r)

                    for blk in range(4 if "swa" in parts else 0):
                        jb = t * 4 + blk
                        c0 = blk * 128
                        pO, pD = PS[6], PS[7]
                        kbs = [jb - 1, jb] if jb > 0 else [jb]
                        for n_i, kbk in enumerate(kbs):
                            sl = kbk % 8
                            which = 1 if kbk == jb else 0
                            pL = PS[4 + (n_i % 2)]
                            for kv in range(2):
                                MM(pL[:, kv * 256:(kv + 1) * 256], kcT[kv * 64:kv * 64 + 64, sl, :],
                                   qcT[kv * 64:kv * 64 + 64, :, c0:c0 + 128], [kcT.r[sl]] + qcT.r, pL.r,
                                   start=True, stop=False, inc=False)
                                MM(pL[:, kv * 256:(kv + 1) * 256], ident[:],
                                   biasC[which][:, kv * 2:kv * 2 + 2, :].rearrange("p h q -> p (h q)"),
                                   ident.r + biasC[which].r, pL.r, start=False, stop=True, inc=(kv == 1))
                            P = PT[n_i]
                            ACT(P[:], pL[:], AF.Exp, pL.r, P.r)
                            first, lastk = (n_i == 0), (n_i == len(kbs) - 1)
                            MM(pD[:], ones_bf[:], P[:], ones_bf.r + P.r, pD.r, start=first, stop=lastk)
                        for kv in range(2):
                            for g in range(2):
                                hd = kv * 2 + g
                                for n_i, kbk in enumerate(kbs):
                                    sl = kbk % 8
                                    P = PT[n_i]
                                    MM(pO[g * 64:g * 64 + 64, kv * 128:(kv + 1) * 128], vctok[:, sl, kv * 64:kv * 64 + 64],
                                       P[:, hd * 128:(hd + 1) * 128], [vctok.r[sl]] + P.r, pO.r,
                                       start=(n_i == 0), stop=(n_i == len(kbs) - 1))
                        kb.op(dve, lambda e: e.tensor_tensor(out=rsb[:], in0=pD[:],
                                                             in1=sinkexp[:].rearrange("p h q -> p (h q)"), op=ALU.add),
                              reads=pD.r + sinkexp.r, writes=rsb.r)
                        kb.op(dve, lambda e: e.reciprocal(out=rsb[:], in_=rsb[:]), reads=rsb.r, writes=rsb.r)
                        for kv in range(2):
                            for g in range(2):
                                hd = kv * 2 + g
                                kb.op(dve, lambda e: e.tensor_tensor(
                                    out=mixT[g * 64:g * 64 + 64, 6 + kv, c0:c0 + 128],
                                    in0=pO[g * 64:g * 64 + 64, kv * 128:(kv + 1) * 128],
                                    in1=rsb[g * 64:g * 64 + 64, hd * 128:(hd + 1) * 128], op=ALU.mult),
                                    reads=pO.r + rsb.r, writes=mixT.r)

                    for blk in range(4 if "dsa" in parts else 0):
                        jb = t * 4 + blk
                        c0 = blk * 128
                        N = (jb + 1) * 128
                        for kt in range((N + 511) // 512):
                            k0 = kt * 512
                            n = min(512, N - k0)
                            kregs = kiT.r[k0 // 128:(k0 + n) // 128]
                            for h in range(4):
                                hp = (h % 2) * 64
                                p = gp()
                                MM(p[:, 0:n], qiT[hp:hp + 64, h // 2, c0:c0 + 128], kiT[hp:hp + 64, k0:k0 + n],
                                   qiT.r + kregs, p.r)
                                if h == 0:
                                    kb.op(dve, lambda e: e.tensor_scalar(
                                        out=score[:, k0:k0 + n], in0=p[:, 0:n], scalar1=0.0,
                                        scalar2=widx[:, blk, 0:1], op0=ALU.max, op1=ALU.mult),
                                        reads=p.r + [widx.r[blk]], writes=score.r)
                                else:
                                    tr = tmpR[h % 2]
                                    ACT(tr[:, 0:n], p[:, 0:n], AF.Relu, p.r, tr.r)
                                    kb.op(dve, lambda e: e.scalar_tensor_tensor(
                                        out=score[:, k0:k0 + n], in0=tr[:, 0:n], scalar=widx[:, blk, h:h + 1],
                                        in1=score[:, k0:k0 + n], op0=ALU.mult, op1=ALU.add),
                                        reads=tr.r + [widx.r[blk]] + score.r, writes=score.r)
                            kb.op(dve, lambda e: e.scalar_tensor_tensor(
                                out=score[:, k0:k0 + n], in0=ramp512[:, 0:n], scalar=-TIE * k0,
                                in1=score[:, k0:k0 + n], op0=ALU.add, op1=ALU.add),
                                reads=ramp512.r + score.r, writes=score.r)
                        kb.op(pool, lambda e: e.memset(score[0:64, N - 64:N], -1e30), reads=score.r, writes=score.r)
                        if N > 256:
                            ACT(Wk[:, 0:N], score[:, 0:N], AF.Copy, score.r, Wk.r)
                            for r in range(32):
                                kb.op(dve, lambda e: e.max(out=m8[:], in_=Wk[:, 0:N]), reads=Wk.r, writes=m8.r)
                                if r < 31:
                                    kb.op(dve, lambda e: e.match_replace(out=Wk[:, 0:N], in_to_replace=m8[:],
                                                                         in_values=Wk[:, 0:N], imm_value=-1e30),
                                          reads=Wk.r + m8.r, writes=Wk.r)
                            thr, thr_r = m8[:, 7:8], m8.r
                        else:
                            thr, thr_r = thr_all[:], thr_all.r
                        kb.op(dve, lambda e: e.tensor_scalar(out=negm[:, 0:N], in0=score[:, 0:N], scalar1=thr,
                                                             scalar2=NEG, op0=ALU.is_lt, op1=ALU.mult),
                              reads=score.r + thr_r, writes=negm.r)
                        pO, pD = PS[6], PS[7]
                        for kbk in range(jb + 1):
                            pL = PS[4 + (kbk % 2)]
                            near = kbk >= jb - 1
                            MM(pL[:], cT[:, kbk * 128:(kbk + 1) * 128], qlT[:, :, c0:c0 + 128], [cT.r[kbk]] + qlT.r, pL.r,
                               start=True, stop=False, inc=False)
                            MM(pL[:], negm[:, kbk * 128:(kbk + 1) * 128], ident4[:].rearrange("p h q -> p (h q)"),
                               negm.r + ident4.r, pL.r, start=False, stop=not near, inc=not near)
                            if near:
                                which = 1 if kbk == jb else 0
                                MM(pL[:], ident[:], biasA[which][:].rearrange("p h q -> p (h q)"),
                                   ident.r + biasA[which].r, pL.r, start=False, stop=True)
                            P = nextPT()
                            ACT(P[:], pL[:], AF.Exp, pL.r, P.r)
                            MM(pO[:], ctok[:, kbk, :], P[:], [ctok.r[kbk]] + P.r, pO.r, start=(kbk == 0), stop=(kbk == jb),
                               inc=False)
                            MM(pD[:], ones_bf[:], P[:], ones_bf.r + P.r, pD.r + pO.r, start=(kbk == 0), stop=(kbk == jb))
                        kb.op(dve, lambda e: e.reciprocal(out=rsb[:], in_=pD[:]), reads=pD.r, writes=rsb.r)
                        kb.op(dve, lambda e: e.tensor_tensor(out=olT[:].rearrange("p h q -> p (h q)"), in0=pO[:],
                                                             in1=rsb[:], op=ALU.mult),
                              reads=pO.r + rsb.r, writes=olT.r)
                        pF = gp()
                        for h in range(4):
                            hp = (h % 2) * 64
                            MM(pF[hp:hp + 64, (h // 2) * 128:(h // 2 + 1) * 128], wuv[:, h * 64:(h + 1) * 64], olT[:, h, :],
                               wuv.r + olT.r, pF.r, inc=(h == 3))
                        ACT(mixT[:, 0:2, c0:c0 + 128], pF[:, 0:256].rearrange("p (i q) -> p i q", i=2), AF.Copy, pF.r,
                            mixT.r)

                    kb.dma(sp, Xt[:], src[gt * TT:(gt + 1) * TT, :].rearrange("(s p) d -> p s d", p=128),
                           reads=[src_regs[gt]], writes=Xt.r, semreg=Xt.r[0])
                    for s in range(4):
                        for hh in range(2):
                            po = gp()
                            for c in range(8):
                                MM(po[:], mixT[:, c, s * 128:(s + 1) * 128], Wo[:, c, hh * 512:(hh + 1) * 512],
                                   mixT.r + Wo.r, po.r, start=(c == 0), stop=(c == 7), inc=(c == 7))
                            kb.op(dve, lambda e: e.tensor_tensor(
                                out=Xt[:, s, hh * 512:(hh + 1) * 512], in0=Xt[:, s, hh * 512:(hh + 1) * 512],
                                in1=po[:], op=ALU.add), reads=Xt.r + po.r, writes=Xt.r)
                    kb.dma(sp, xs[gt * TT:(gt + 1) * TT, :].rearrange("(s p) d -> p s d", p=128), Xt[:],
                           reads=Xt.r, writes=[xs.r[gt]], semreg=Xt.r[0])

        cur, cur_regs = x_in, xin_regs
        for l in range(DEPTH):
            if do_mixer:
                with ExitStack() as pes:
                    mixer_phase(l, cur, cur_regs, pes)
                    kb.barrier()
                cur, cur_regs = xs.t, xs.r
            if do_ffn:
                with ExitStack() as pes:
                    ffn_phase(l, cur, cur_regs, last=(l == DEPTH - 1), pes=pes)
                    kb.barrier()
                cur, cur_regs = xs.t, xs.r

        for r in yout_regs:
            sp.wait(r.last_w)
        print(f"[build] instructions={kb.ninst} sems={kb.nsem}", flush=True)
    return nc


_W_NAMES = ["w_in", "w_out", "norm_mix", "norm_ffn", "kv_norm", "w_uk", "w_uv", "w_gate2", "b_gate", "gla_norm",
            "sinks", "w_ffn_gate", "w_ffn_up", "w_ffn_down", "final_norm"]


def make_in_maps(inputs, n_cores, nseq, S):
    x = np.ascontiguousarray(inputs["x"], dtype=np.float32)
    L = inputs["w_in"].shape[0]
    shared = {}
    for k in _W_NAMES:
        a = np.ascontiguousarray(inputs[k], dtype=np.float32)
        if k == "final_norm":
            a = a.reshape(1, D)
        if k in ("w_uk", "w_uv"):
            a = a.reshape(L, 128, 256)
        shared[k] = a
    cA, cC, mC, bfar = _host_consts(inputs["rel_bias"])
    shared.update(cA=cA, cC=cC, mC=mC, bfar=bfar)
    in_maps = []
    for c in range(n_cores):
        m = dict(shared)
        m["x"] = x[c * nseq:(c + 1) * nseq].reshape(nseq * S, D)
        in_maps.append(m)
    return in_maps


def kernel(**inputs):
    n = 8
    B, S, _ = inputs["x"].shape
    nseq = B // n
    nc = build(S=S, NSEQ=nseq, DEPTH=inputs["w_in"].shape[0])
    in_maps = make_in_maps(inputs, n, nseq, S)
    res = run_bass_kernel_spmd(nc, in_maps, core_ids=list(range(n)))
    out = np.concatenate([r["y"].reshape(nseq, S, D) for r in res.results], axis=0)
    return out.astype(np.float32)
```

```python
import numpy as np
from contextlib import ExitStack
import concourse.bass as bass
import concourse.mybir as mybir
from concourse.bass_utils import run_bass_kernel_spmd

F32 = mybir.dt.float32
BF16 = mybir.dt.bfloat16
ALU = mybir.AluOpType
AF = mybir.ActivationFunctionType
AX = mybir.AxisListType

D = 1024
DFF = 2816
NFF = DFF // 128
EPS = 1e-6
NEG = -30000.0


class Ev:
    __slots__ = ("sem", "val")

    def __init__(self, sem, val):
        self.sem = sem
        self.val = val


class Region:
    __slots__ = ("last_w", "readers", "dsem", "dcnt", "name", "excl")

    def __init__(self, name, excl=False):
        self.name = name
        self.excl = excl
        self.last_w = None
        self.readers = {}
        self.dsem = None
        self.dcnt = 0


class Tile:
    def __init__(self, t, regs):
        self.t = t
        self.r = regs

    def __getitem__(self, idx):
        return self.t[idx]


class Q:
    def __init__(self, kb, eng, name, is_pe=False):
        self.eng = eng
        self.name = name
        self.is_pe = is_pe
        self.sem = kb.newsem("q_" + name)
        self.cnt = 0
        self.known = {}

    def wait(self, ev):
        if ev is None:
            return
        if ev.sem is self.sem and self.is_pe:
            return
        k = id(ev.sem)
        if self.known.get(k, 0) >= ev.val:
            return
        self.eng.wait_ge(ev.sem, ev.val)
        self.known[k] = ev.val


class KB:
    def __init__(self, nc, es):
        self.nc = nc
        self.es = es
        self.nsem = 0
        self.pe = Q(self, nc.tensor, "pe", is_pe=True)
        self.act = Q(self, nc.scalar, "act")
        self.dve = Q(self, nc.vector, "dve")
        self.pool = Q(self, nc.gpsimd, "pool")
        self.sp = Q(self, nc.sync, "sp")
        self.ninst = 0
        self.dregs = []
        self.uid = 0

    def newsem(self, name):
        self.nsem += 1
        return self.es.enter_context(self.nc.semaphore(name))

    def sb(self, name, shape, dt, nreg=1, es=None):
        self.uid += 1
        name = f"{name}_{self.uid}"
        t = (es or self.es).enter_context(self.nc.sbuf_tensor(name, list(shape), dt))
        return Tile(t, [Region(f"{name}.{i}") for i in range(nreg)])

    def barrier(self):
        qs = [self.pe, self.act, self.dve, self.pool, self.sp]
        for q in qs:
            for q2 in qs:
                if q2 is not q and q2.cnt > 0:
                    q.wait(Ev(q2.sem, q2.cnt))
            for r in self.dregs:
                q.wait(Ev(r.dsem, r.dcnt))

    def ps(self, name, shape=(128, 512), dt=F32):
        t = self.es.enter_context(self.nc.psum_tensor(name, list(shape), dt))
        return Tile(t, [Region(name, excl=True)])

    def dram(self, name, shape, dt, nreg=1, kind="Internal"):
        t = self.nc.dram_tensor(name, list(shape), dt, kind=kind)
        return Tile(t.ap(), [Region(f"{name}.{i}") for i in range(nreg)])

    def _deps(self, q, reads, writes):
        for r in reads:
            q.wait(r.last_w)
            if r.excl:
                for ev in r.readers.values():
                    if ev.sem is not q.sem:
                        q.wait(ev)
        for w in writes:
            q.wait(w.last_w)
            for ev in w.readers.values():
                q.wait(ev)

    def _record(self, ev, reads, writes):
        for r in reads:
            r.readers[id(ev.sem)] = ev
        for w in writes:
            w.last_w = ev
            w.readers = {}

    def op(self, q, fn, reads=(), writes=(), inc=True):
        self._deps(q, reads, writes)
        inst = fn(q.eng)
        self.ninst += 1
        ev = Ev(q.sem, q.cnt + 1)
        if inc:
            inst.then_inc(q.sem, 1)
            q.cnt += 1
        self._record(ev, reads, writes)
        return ev

    def dma(self, q, out, in_, reads, writes, semreg, **kw):
        self._deps(q, reads, writes)
        if semreg.dsem is None:
            semreg.dsem = self.newsem("d_" + semreg.name.replace(".", "_"))
            self.dregs.append(semreg)
        inst = q.eng.dma_start(out=out, in_=in_, **kw)
        semreg.dcnt += 16
        inst.then_inc(semreg.dsem, 16)
        self.ninst += 1
        ev = Ev(semreg.dsem, semreg.dcnt)
        self._record(ev, reads, writes)
        return ev


def _bucket(rel):
    rel = np.asarray(rel, dtype=np.int64)
    n = np.abs(rel)
    side = np.where(rel > 0, 16, 0)
    nf = np.maximum(n, 1).astype(np.float32)
    large = 8 + (np.log(nf / np.float32(8)) / np.float32(np.log(16.0)) * np.float32(8)).astype(np.int32)
    large = np.minimum(large, 15)
    return (side + np.where(n < 8, n, large)).astype(np.int64)


def _host_consts(rel_bias):
    rel_bias = np.asarray(rel_bias, dtype=np.float32)
    s = np.arange(128)[:, None]
    q = np.arange(128)[None, :]
    ip = _bucket(s - 128 - q)
    idg = _bucket(s - q)
    g = np.stack([rel_bias[ip], rel_bias[idg]])
    cA = np.ascontiguousarray(g[..., 0:4].transpose(0, 1, 3, 2)).reshape(2, 128, 512)
    cC = np.ascontiguousarray(g[..., 4:8].transpose(0, 1, 3, 2)).reshape(2, 128, 512)
    bfar = np.ascontiguousarray(rel_bias[15:16, 0:4])
    mp = np.where((q >= 64) & (s < 64), NEG, 0.0).astype(np.float32)
    md = np.where((q < 64) & (s >= 64), NEG, 0.0).astype(np.float32)
    mC = np.stack([np.broadcast_to(mp[:, None, :], (128, 4, 128)),
                   np.broadcast_to(md[:, None, :], (128, 4, 128))]).reshape(2, 128, 512)
    return cA, cC, np.ascontiguousarray(mC, dtype=np.float32), bfar


_FCH = [
    ("qa0", [(0, 128)]), ("qa1", [(128, 128)]),
    ("qi0", [(384, 128)]), ("qi1", [(512, 128)]),
    ("ki", [(640, 64), (640, 64)]),
    ("glr", [(1732, 16)]),
    ("qb0", [(708, 128)]), ("qb1", [(836, 128)]),
    ("kb0", [(964, 128)]), ("kb1", [(1092, 128)]),
    ("rb0", [(1748, 128)]), ("rb1", [(1876, 128)]), ("rb2", [(2004, 128)]), ("rb3", [(2132, 128)]),
    ("qc0", [(2260, 64), (2388, 64)]), ("qc1", [(2324, 64), (2452, 64)]),
    ("kc", [(2516, 128)]),
]
_TCH = [("A", [(256, 128), (704, 4)]), ("B", [(964, 256), (2644, 128)]), ("C", [(1220, 512)])]


def build(S=4096, NSEQ=2, DEPTH=2, do_mixer=True, do_ffn=True, xbuf=1, parts=("gla", "swa", "dsa"), stage=99):
    nc = bass.Bass("TRN2", target_bir_lowering=False)
    NT = NSEQ * S
    TT = 512
    NTILE = NT // TT
    TPS = S // TT
    NB = S // 128

    def din(name, shape):
        return nc.dram_tensor(name, list(shape), F32, kind="ExternalInput").ap()

    x_in = din("x", [NT, D])
    w_in = din("w_in", [DEPTH, D, 2772])
    w_out = din("w_out", [DEPTH, D, D])
    n_mix = din("norm_mix", [DEPTH, D])
    n_ffn = din("norm_ffn", [DEPTH, D])
    kv_norm = din("kv_norm", [DEPTH, 128])
    w_uk = din("w_uk", [DEPTH, 128, 256])
    w_uv = din("w_uv", [DEPTH, 128, 256])
    w_g2 = din("w_gate2", [DEPTH, 16, 256])
    b_g = din("b_gate", [DEPTH, 256])
    gla_n = din("gla_norm", [DEPTH, 512])
    sinks = din("sinks", [DEPTH, 4])
    w_g = din("w_ffn_gate", [DEPTH, D, DFF])
    w_u = din("w_ffn_up", [DEPTH, D, DFF])
    w_d = din("w_ffn_down", [DEPTH, DFF, D])
    n_fin = din("final_norm", [1, D])
    cA_d = din("cA", [2, 128, 512])
    cC_d = din("cC", [2, 128, 512])
    mC_d = din("mC", [2, 128, 512])
    bfar_d = din("bfar", [1, 4])
    y_out = nc.dram_tensor("y", [NT, D], F32, kind="ExternalOutput").ap()

    with ExitStack() as es:
        kb = KB(nc, es)
        pe, act, dve, pool, sp = kb.pe, kb.act, kb.dve, kb.pool, kb.sp

        xs = kb.dram("xs", [NT, D], F32, nreg=NTILE)
        xin_regs = [Region(f"xin.{i}") for i in range(NTILE)]
        yout_regs = [Region(f"yout.{i}") for i in range(NTILE)]

        def MM(out, lhsT, rhs, reads, wr, start=True, stop=True, inc=True):
            return kb.op(pe, lambda e: e.matmul(out, lhsT=lhsT, rhs=rhs, start=start, stop=stop),
                         reads=reads, writes=wr, inc=inc)

        def ACT(out, in_, func, reads, wr, **kw):
            return kb.op(act, lambda e: e.activation(out=out, in_=in_, func=func, **kw), reads=reads, writes=wr)

        identf = kb.sb("identf", [128, 128], F32)
        ident = kb.sb("ident", [128, 128], BF16)
        ident4 = kb.sb("ident4", [128, 4, 128], BF16)
        ones_bf = kb.sb("ones_bf", [128, 128], BF16)
        tribd = kb.sb("tribd", [128, 128], F32)
        ones_f = kb.sb("ones_f", [128, 128], F32)
        ramp512 = kb.sb("ramp512", [128, 512], F32)
        cmask = kb.sb("cmask", [128, 2], F32)
        subd = kb.sb("subd", [128, 128], F32)
        epsc = kb.sb("epsc", [128, 1], F32)
        onec = kb.sb("onec", [128, 1], F32)
        thr_all = kb.sb("thr_all", [128, 1], F32)
        kb.op(pool, lambda e: e.memset(identf[:], 0.0), writes=identf.r)
        kb.op(pool, lambda e: e.affine_select(out=identf[:], in_=identf[:], pattern=[[-1, 128]],
                                              compare_op=ALU.not_equal, fill=1.0, base=0, channel_multiplier=1),
              reads=identf.r, writes=identf.r)
        kb.op(dve, lambda e: e.tensor_copy(out=ident[:], in_=identf[:]), reads=identf.r, writes=ident.r)
        for h in range(4):
            kb.op(dve, lambda e: e.tensor_copy(out=ident4[:, h, :], in_=identf[:]), reads=identf.r, writes=ident4.r)
        kb.op(pool, lambda e: e.memset(ones_bf[:], 1.0), writes=ones_bf.r)
        kb.op(pool, lambda e: e.memset(epsc[:], EPS), writes=epsc.r)
        kb.op(pool, lambda e: e.memset(onec[:], 1.0), writes=onec.r)
        kb.op(pool, lambda e: e.memset(thr_all[:], -1e29), writes=thr_all.r)
        kb.op(pool, lambda e: e.memset(tribd[:], 1.0), writes=tribd.r)
        kb.op(pool, lambda e: e.affine_select(out=tribd[:], in_=tribd[:], pattern=[[1, 128]],
                                              compare_op=ALU.is_ge, fill=0.0, base=0, channel_multiplier=-1),
              reads=tribd.r, writes=tribd.r)
        kb.op(pool, lambda e: e.memset(tribd[0:64, 64:128], 0.0), reads=tribd.r, writes=tribd.r)
        kb.op(pool, lambda e: e.memset(cmask[:], 0.0), writes=cmask.r)
        kb.op(pool, lambda e: e.memset(cmask[0:64, 0:1], 1.0), reads=cmask.r, writes=cmask.r)
        kb.op(pool, lambda e: e.memset(cmask[64:128, 1:2], 1.0), reads=cmask.r, writes=cmask.r)
        kb.op(pool, lambda e: e.memset(subd[:], 1.0), writes=subd.r)
        kb.op(pool, lambda e: e.affine_select(out=subd[:], in_=subd[:], pattern=[[-1, 128]],
                                              compare_op=ALU.is_gt, fill=0.0, base=0, channel_multiplier=1),
              reads=subd.r, writes=subd.r)
        kb.op(pool, lambda e: e.memset(subd[64:128, 0:64], 0.0), reads=subd.r, writes=subd.r)

        TIE = 1e-9
        kb.op(pool, lambda e: e.memset(ones_f[:], 1.0), writes=ones_f.r)
        gfin = kb.sb("gfin", [128, D], F32)
        kb.dma(sp, gfin[:], n_fin[0].partition_broadcast(128), reads=(), writes=gfin.r, semreg=gfin.r[0])
        gffn = kb.sb("gffn", [128, DEPTH * 8], F32)
        gmix = kb.sb("gmix", [128, DEPTH * 8], F32)
        with nc.allow_non_contiguous_dma(reason="tiny gain vectors"):
            kb.dma(sp, gffn[:], n_ffn.rearrange("l (k p) -> p (l k)", p=128), reads=(), writes=gffn.r,
                   semreg=gffn.r[0])
            kb.dma(sp, gmix[:], n_mix.rearrange("l (k p) -> p (l k)", p=128), reads=(), writes=gmix.r,
                   semreg=gmix.r[0])

        X = [kb.sb(f"X{i}", [128, 4, D], F32) for i in range(xbuf)]
        ss = kb.sb("ss", [128, 4], F32)
        rstd = kb.sb("rstd", [128, 4], F32)
        hT = kb.sb("hT", [128, 8, TT], BF16)
        PS = [kb.ps(f"ps{i}") for i in range(8)]
        gstate = {"i": 0}

        def gp():
            gstate["i"] += 1
            return PS[gstate["i"] % 4]

        pr = PS[0]
        MM(pr[:, 0:128], ones_f[:], tribd[:], ones_f.r + tribd.r, pr.r)
        for c8 in range(8):
            kb.op(dve, lambda e: e.tensor_scalar(out=ramp512[:, c8 * 64:(c8 + 1) * 64],
                                                 in0=pr[:, 0:64], scalar1=-TIE,
                                                 scalar2=-TIE * 64 * c8, op0=ALU.mult, op1=ALU.add),
                  reads=pr.r, writes=ramp512.r)

        def rstd_from_ss():
            ACT(rstd[:], ss[:], AF.Sqrt, ss.r + epsc.r, rstd.r, bias=epsc[:], scale=1.0 / D)
            kb.op(dve, lambda e: e.reciprocal(out=rstd[:], in_=rstd[:]), reads=rstd.r, writes=rstd.r)

        def rmsnorm_T(Xt, gain_cols, xn, junk):
            for s in range(4):
                ACT(junk[:], Xt[:, s, :], AF.Square, Xt.r, junk.r + ss.r, accum_out=ss[:, s:s + 1])
            rstd_from_ss()
            for s in range(4):
                ACT(xn[:, s, :], Xt[:, s, :], AF.Copy, Xt.r + rstd.r, xn.r, scale=rstd[:, s:s + 1])
            for c in range(8):
                p = gp()
                for s in range(4):
                    MM(p[:, s * 128:(s + 1) * 128], xn[:, s, c * 128:(c + 1) * 128], ident[:],
                       xn.r + ident.r, p.r, inc=(s == 3))
                kb.op(dve, lambda e: e.tensor_scalar_mul(out=hT[:, c, :], in0=p[:], scalar1=gain_cols[c]),
                      reads=p.r, writes=hT.r)

        def ffn_phase(l, src, src_regs, last, pes):
            Wg = kb.sb("Wg", [128, 8, DFF], BF16, es=pes)
            Wu = kb.sb("Wu", [128, 8, DFF], BF16, es=pes)
            Wd = kb.sb("Wd", [128, NFF, D], BF16, es=pes)
            aT = kb.sb("aT", [128, NFF, TT], BF16, es=pes)
            xn = Tile(aT[:, 0:8, :].rearrange("p (s a) t -> p s (a t)", s=4), aT.r)
            sg = [kb.sb(f"sg{i}", [128, TT], F32, es=pes) for i in range(2)]
            junk = kb.sb("junk", [128, D], BF16, es=pes)
            HC = DFF // 2
            for hh in range(2):
                kb.dma(pool, Wg[:, :, hh * HC:(hh + 1) * HC],
                       w_g[l].rearrange("(k p) n -> p k n", p=128)[:, :, hh * HC:(hh + 1) * HC],
                       reads=(), writes=Wg.r, semreg=Wg.r[0])
                kb.dma(pool, Wu[:, :, hh * HC:(hh + 1) * HC],
                       w_u[l].rearrange("(k p) n -> p k n", p=128)[:, :, hh * HC:(hh + 1) * HC],
                       reads=(), writes=Wu.r, semreg=Wu.r[0])
            for hh in range(2):
                kb.dma(pool, Wd[:, hh * 11:(hh + 1) * 11, :],
                       w_d[l].rearrange("(k p) n -> p k n", p=128)[:, hh * 11:(hh + 1) * 11, :],
                       reads=(), writes=Wd.r, semreg=Wd.r[0])
            gcols = [gffn[:, l * 8 + c:l * 8 + c + 1] for c in range(8)]
            for t in range(NTILE):
                Xt = X[t % xbuf]
                kb.dma(sp, Xt[:], src[t * TT:(t + 1) * TT, :].rearrange("(s p) d -> p s d", p=128),
                       reads=[src_regs[t]], writes=Xt.r, semreg=Xt.r[0])
                rmsnorm_T(Xt, gcols, xn, junk)
                for f in range(NFF):
                    pg = PS[4 + (f % 2)]
                    pu = PS[6 + (f % 2)]
                    for k in range(8):
                        MM(pg[:], Wg[:, k, f * 128:(f + 1) * 128], hT[:, k, :], Wg.r + hT.r, pg.r,
                           start=(k == 0), stop=(k == 7), inc=(k == 7))
                    for k in range(8):
                        MM(pu[:], Wu[:, k, f * 128:(f + 1) * 128], hT[:, k, :], Wu.r + hT.r, pu.r,
                           start=(k == 0), stop=(k == 7), inc=(k == 7))
                    sgt = sg[f % 2]
                    ACT(sgt[:], pg[:], AF.Silu, pg.r, sgt.r)
                    kb.op(dve, lambda e: e.tensor_tensor(out=aT[:, f, :], in0=sgt[:], in1=pu[:], op=ALU.mult),
                          reads=sgt.r + pu.r, writes=aT.r)
                for s in range(4):
                    for hh in range(2):
                        po = gp()
                        for f in range(NFF):
                            MM(po[:], aT[:, f, s * 128:(s + 1) * 128], Wd[:, f, hh * 512:(hh + 1) * 512],
                               aT.r + Wd.r, po.r, start=(f == 0), stop=(f == NFF - 1), inc=(f == NFF - 1))
                        kb.op(dve, lambda e: e.tensor_tensor(
                            out=Xt[:, s, hh * 512:(hh + 1) * 512], in0=Xt[:, s, hh * 512:(hh + 1) * 512],
                            in1=po[:], op=ALU.add), reads=Xt.r + po.r, writes=Xt.r)
                if last:
                    for s in range(4):
                        ACT(junk[:], Xt[:, s, :], AF.Square, Xt.r, junk.r + ss.r, accum_out=ss[:, s:s + 1])
                    rstd_from_ss()
                    for s in range(4):
                        kb.op(dve, lambda e: e.scalar_tensor_tensor(
                            out=Xt[:, s, :], in0=Xt[:, s, :], scalar=rstd[:, s:s + 1], in1=gfin[:],
                            op0=ALU.mult, op1=ALU.mult), reads=Xt.r + rstd.r + gfin.r, writes=Xt.r)
                    kb.dma(sp, y_out[t * TT:(t + 1) * TT, :].rearrange("(s p) d -> p s d", p=128), Xt[:],
                           reads=Xt.r, writes=[yout_regs[t]], semreg=Xt.r[0])
                else:
                    kb.dma(sp, xs[t * TT:(t + 1) * TT, :].rearrange("(s p) d -> p s d", p=128), Xt[:],
                           reads=Xt.r, writes=[xs.r[t]], semreg=Xt.r[0])

        def mixer_phase(l, src, src_regs, pes):
            def sb(name, shape, dt, nreg=1):
                return kb.sb(name, shape, dt, nreg=nreg, es=pes)

            foff, fw, off = {}, {}, 0
            for name, pieces in _FCH:
                foff[name] = off
                fw[name] = sum(n for _, n in pieces)
                off += fw[name]
            NFC = off
            toff, tw, off = {}, {}, 0
            for name, pieces in _TCH:
                toff[name] = off
                tw[name] = sum(n for _, n in pieces)
                off += tw[name]
            NTC = off
            Wf = sb("Wf", [128, 8, NFC], BF16)
            Wt = sb("Wt", [128, 8, NTC], BF16)
            Wo = sb("Wo", [128, 8, D], BF16)
            wsrc = w_in[l].rearrange("(k p) n -> p k n", p=128)
            for name, pieces in _FCH:
                o = foff[name]
                for c0, n in pieces:
                    kb.dma(pool, Wf[:, :, o:o + n], wsrc[:, :, c0:c0 + n], reads=(), writes=Wf.r, semreg=Wf.r[0])
                    o += n
            for name, pieces in _TCH:
                o = toff[name]
                for c0, n in pieces:
                    kb.dma(pool, Wt[:, :, o:o + n], wsrc[:, :, c0:c0 + n], reads=(), writes=Wt.r, semreg=Wt.r[0])
                    o += n
            for hh in range(2):
                kb.dma(pool, Wo[:, :, hh * 512:(hh + 1) * 512],
                       w_out[l].rearrange("(k p) n -> p k n", p=128)[:, :, hh * 512:(hh + 1) * 512],
                       reads=(), writes=Wo.r, semreg=Wo.r[0])

            kvn = sb("kvn", [128, 128], F32)
            wukn = sb("wukn", [128, 256], F32)
            wuvn = sb("wuvn", [128, 256], F32)
            wg2a = sb("wg2a", [33, 256], F32)
            gn = sb("gn", [128, 4], F32)
            sinkb = sb("sinkb", [128, 4], F32)
            bfar = sb("bfar", [128, 4], F32)
            cst = sb("cst", [128, 512], F32)
            cst2 = sb("cst2", [128, 512], F32)
            kb.dma(sp, kvn[:], kv_norm[l].partition_broadcast(128), reads=(), writes=kvn.r, semreg=kvn.r[0])
            kb.dma(sp, wukn[:], w_uk[l], reads=(), writes=wukn.r, semreg=wukn.r[0])
            kb.dma(sp, wuvn[:], w_uv[l], reads=(), writes=wuvn.r, semreg=wuvn.r[0])
            kb.op(pool, lambda e: e.memset(wg2a[:], 0.0), writes=wg2a.r)
            kb.dma(sp, wg2a[0:16, :], w_g2[l], reads=(), writes=wg2a.r, semreg=wg2a.r[0])
            kb.dma(sp, wg2a[32:33, :], b_g[l:l + 1, :], reads=(), writes=wg2a.r, semreg=wg2a.r[0])
            with nc.allow_non_contiguous_dma(reason="tiny gain vectors"):
                kb.dma(sp, gn[:], gla_n[l].rearrange("(h e) -> e h", e=128), reads=(), writes=gn.r, semreg=gn.r[0])
            kb.dma(sp, sinkb[:], sinks[l].partition_broadcast(128), reads=(), writes=sinkb.r, semreg=sinkb.r[0])
            kb.dma(sp, bfar[:], bfar_d[0].partition_broadcast(128), reads=(), writes=bfar.r, semreg=bfar.r[0])

            if stage < 1:
                return
            wukT = sb("wukT", [128, 2, 128], BF16)
            wuv = sb("wuv", [128, 256], BF16)
            for i in range(2):
                p = gp()
                MM(p[:, 0:128], wukn[:, i * 128:(i + 1) * 128], identf[:], wukn.r + identf.r, p.r)
                ACT(wukT[:, i, :], p[:, 0:128], AF.Copy, p.r, wukT.r, scale=0.125)
            kb.op(dve, lambda e: e.tensor_copy(out=wuv[:], in_=wuvn[:]), reads=wuvn.r, writes=wuv.r)
            sinkexp = sb("sinkexp", [128, 4, 128], F32)
            ACT(sinkb[:], sinkb[:], AF.Exp, sinkb.r, sinkb.r)
            kb.op(pool, lambda e: e.memset(sinkexp[:], 0.0), writes=sinkexp.r)
            for h in range(4):
                kb.op(dve, lambda e: e.tensor_scalar_add(out=sinkexp[:, h, :], in0=sinkexp[:, h, :],
                                                         scalar1=sinkb[:, h:h + 1]),
                      reads=sinkexp.r + sinkb.r, writes=sinkexp.r)
            biasA = [sb(f"biasA{i}", [128, 4, 128], BF16) for i in range(2)]
            biasC = [sb(f"biasC{i}", [128, 4, 128], BF16) for i in range(2)]
            for i in range(2):
                kb.dma(sp, cst[:], cA_d[i], reads=(), writes=cst.r, semreg=cst.r[0])
                for h in range(4):
                    kb.op(dve, lambda e: e.tensor_scalar_sub(out=biasA[i][:, h, :], in0=cst[:, h * 128:(h + 1) * 128],
                                                             scalar1=bfar[:, h:h + 1]),
                          reads=cst.r + bfar.r, writes=biasA[i].r)
                kb.dma(sp, cst[:], cC_d[i], reads=(), writes=cst.r, semreg=cst.r[0])
                kb.dma(sp, cst2[:], mC_d[i], reads=(), writes=cst2.r, semreg=cst2.r[0])
                kb.op(dve, lambda e: e.tensor_tensor(out=biasC[i][:].rearrange("p h q -> p (h q)"), in0=cst[:],
                                                     in1=cst2[:], op=ALU.add),
                      reads=cst.r + cst2.r, writes=biasC[i].r)

            if stage < 2:
                return
            kiT = sb("kiT", [128, S], BF16, nreg=NB)
            cT = sb("cT", [128, S], BF16, nreg=NB)
            ctok = sb("ctok", [128, NB, 128], BF16, nreg=NB)
            kcT = sb("kcT", [128, 8, 128], BF16, nreg=8)
            vctok = sb("vctok", [128, 8, 128], BF16, nreg=8)
            St = sb("St", [128, 2, 128], F32)
            Sbf = [sb(f"Sbf{i}", [128, 2, 128], BF16) for i in range(2)]
            qaT = sb("qaT", [128, 2, TT], BF16)
            qlT = sb("qlT", [128, 4, TT], BF16)
            qiT = sb("qiT", [128, 2, TT], BF16)
            glra = sb("glra", [33, TT], F32)
            sptok1 = sb("sptok", [128, 256], F32)
            eDk = sb("eDk", [128, 256], F32)
            EbT = sb("EbT", [128, 2, TT], F32)
            EnT = sb("EnT", [128, 2, TT], BF16)
            qeT = sb("qeT", [128, 2, TT], BF16)
            keT = sb("keT", [128, 2, TT], BF16)
            SrgT = sb("SrgT", [128, 4, TT], BF16)
            qcT = sb("qcT", [128, 2, TT], BF16)
            kdtok = sb("kdtok", [128, 4, 256], BF16, nreg=4)
            vbtok = sb("vbtok", [128, 4, 512], BF16, nreg=4)
            widx = sb("widx", [128, 4, 4], F32, nreg=4)
            ssc = sb("ssc", [128, 1], F32)
            rc = sb("rc", [128, 1], F32)
            mixT = hT
            attm = sb("attm", [128, 4, 128], BF16)
            kdm = sb("kdm", [128, 2, 256], BF16)
            sqb = sb("sqb", [128, 512], BF16)
            PT = [sb(f"PT{i}", [128, 512], BF16) for i in range(2)]
            assert xbuf == 1
            score = Tile(X[0][:].rearrange("p s d -> p (s d)"), X[0].r)
            negm0 = sb("negm0", [128, 4096], BF16)
            negm1 = sb("negm1", [128, S], BF16)
            negmB = [negm0, negm1]
            xn = Tile(negm0[:, 0:4096].rearrange("p (s d) -> p s d", s=4), negm0.r)
            junk = Tile(negm0[:, 0:1024], negm0.r)
            rsbB = sb("rsbB", [128, 512], F32)
            tmpR = [cst, cst2]
            t1, rsb = cst, cst2
            m8 = sb("m8", [128, 8], F32)
            olT = sb("olT", [128, 4, 128], BF16)
            pstate = {"i": 0}

            def nextPT():
                pstate["i"] += 1
                return PT[pstate["i"] % 2]

            kb.op(pool, lambda e: e.memset(glra[:], 0.0), writes=glra.r)
            kb.op(pool, lambda e: e.memset(glra[32:33, :], 1.0), reads=glra.r, writes=glra.r)
            gcols = [gmix[:, l * 8 + c:l * 8 + c + 1] for c in range(8)]

            def fproj(name):
                o, n = foff[name], fw[name]
                p = gp()
                for k in range(8):
                    MM(p[0:n, :], Wf[:, k, o:o + n], hT[:, k, :], Wf.r + hT.r, p.r,
                       start=(k == 0), stop=(k == 7), inc=(k == 7))
                return p

            def tproj(name, blk):
                o, n = toff[name], tw[name]
                p = gp()
                for k in range(8):
                    MM(p[:, 0:n], hT[:, k, blk * 128:(blk + 1) * 128], Wt[:, k, o:o + n], Wt.r + hT.r, p.r,
                       start=(k == 0), stop=(k == 7), inc=(k == 7))
                return p

            for b in range(NSEQ):
                kb.op(pool, lambda e: e.memset(St[:], 0.0), reads=St.r, writes=St.r)
                for t in range(TPS):
                    gt = b * TPS + t
                    Xt = X[gt % xbuf]
                    kb.dma(sp, Xt[:], src[gt * TT:(gt + 1) * TT, :].rearrange("(s p) d -> p s d", p=128),
                           reads=[src_regs[gt]], writes=Xt.r, semreg=Xt.r[0])
                    rmsnorm_T(Xt, gcols, xn, junk)
                    if stage < 3:
                        continue

                    for i in range(2):
                        p = fproj(f"qa{i}")
                        ACT(qaT[:, i, :], p[:], AF.Copy, p.r, qaT.r)
                    for h in range(4):
                        p = gp()
                        hp = (h % 2) * 64
                        MM(p[:], wukT[hp:hp + 64, h // 2, :], qaT[hp:hp + 64, h // 2, :], wukT.r + qaT.r, p.r)
                        kb.op(dve, lambda e: e.tensor_copy(out=qlT[:, h, :], in_=p[:]), reads=p.r, writes=qlT.r)
                    if stage == 31:
                        continue
                    for i in range(2):
                        p = fproj(f"qi{i}")
                        ACT(qiT[:, i, :], p[:], AF.Copy, p.r, qiT.r)
                    p = fproj("ki")
                    kb.op(dve, lambda e: e.tensor_copy(out=kiT[:, t * TT:(t + 1) * TT], in_=p[:]), reads=p.r,
                          writes=kiT.r[t * 4:t * 4 + 4])
                    p = fproj("glr")
                    kb.op(dve, lambda e: e.tensor_copy(out=glra[0:16, :], in_=p[0:16, :]), reads=p.r, writes=glra.r)
                    if stage == 32:
                        continue
                    for i in range(2):
                        p = fproj(f"qc{i}")
                        ACT(qcT[:, i, :], p[:], AF.Copy, p.r, qcT.r, scale=0.125)
                    p = fproj("kc")
                    for blk in range(4):
                        sl = (t * 4 + blk) % 8
                        kb.op(dve, lambda e: e.tensor_copy(out=kcT[:, sl, :], in_=p[:, blk * 128:(blk + 1) * 128]),
                              reads=p.r, writes=[kcT.r[sl]])
                    if stage == 33:
                        continue
                    for i in range(4):
                        p = fproj(f"rb{i}")
                        ACT(SrgT[:, i, :], p[:], AF.Silu, p.r, SrgT.r)
                        kb.op(dve, lambda e: e.tensor_scalar_mul(out=SrgT[:, i, :], in0=SrgT[:, i, :],
                                                                   scalar1=gn[:, i:i + 1]),
                              reads=SrgT.r + gn.r, writes=SrgT.r)

                    if stage < 4 or stage == 41:
                        continue
                    pG = [PS[4], PS[5]]
                    for blk in range(4):
                        jb = t * 4 + blk
                        sl = jb % 8
                        p = tproj("A", blk)
                        ACT(junk[:, 0:128], p[:, 0:128], AF.Square, p.r, junk.r + ssc.r, accum_out=ssc[:])
                        ACT(rc[:], ssc[:], AF.Sqrt, ssc.r + epsc.r, rc.r, bias=epsc[:], scale=1.0 / 128)
                        kb.op(dve, lambda e: e.reciprocal(out=rc[:], in_=rc[:]), reads=rc.r, writes=rc.r)
                        kb.op(dve, lambda e: e.scalar_tensor_tensor(out=ctok[:, jb, :], in0=p[:, 0:128], scalar=rc[:],
                                                                    in1=kvn[:], op0=ALU.mult, op1=ALU.mult),
                              reads=p.r + rc.r + kvn.r, writes=[ctok.r[jb]])
                        kb.op(dve, lambda e: e.tensor_copy(out=widx[:, blk, :], in_=p[:, 128:132]), reads=p.r,
                              writes=[widx.r[blk]])
                        p2 = gp()
                        MM(p2[:, 0:128], ctok[:, jb, :], ident[:], [ctok.r[jb]] + ident.r, p2.r)
                        ACT(cT[:, jb * 128:(jb + 1) * 128], p2[:, 0:128], AF.Copy, p2.r, [cT.r[jb]])
                        if stage == 42:
                            continue
                        pz = gp()
                        MM(pz[:, 0:256], glra[0:33, blk * 128:(blk + 1) * 128], wg2a[0:33, :], glra.r + wg2a.r, pz.r)
                        ACT(sptok1[:], pz[:, 0:256], AF.Exp, pz.r, sptok1.r, scale=-1.0)
                        ACT(sptok1[:], sptok1[:], AF.Ln, sptok1.r + onec.r, sptok1.r,
                            bias=onec[:], scale=1.0)
                        if stage == 43:
                            continue
                        for i in range(2):
                            MM(pG[i][:, blk * 128:(blk + 1) * 128], sptok1[:, i * 128:(i + 1) * 128], tribd[:],
                               sptok1.r + tribd.r, pG[i].r)
                        pd = gp()
                        MM(pd[:, 0:256], subd[:], sptok1[:], sptok1.r + subd.r, pd.r)
                        ACT(eDk[:], pd[:, 0:256], AF.Exp, pd.r, eDk.r, scale=-1.0 / 16)
                        if stage == 44:
                            continue
                        p = tproj("B", blk)
                        if stage == 46:
                            continue
                        if stage != 49:
                            kb.op(dve, lambda e: e.tensor_tensor(out=kdtok[:, blk, :], in0=p[:, 0:256], in1=eDk[:],
                                                                 op=ALU.mult),
                                  reads=p.r + eDk.r, writes=[kdtok.r[blk]])
                        if stage == 48:
                            continue
                        ACT(vctok[:, sl, :], p[:, 256:384], AF.Copy, p.r, [vctok.r[sl]])
                        if stage in (47, 49):
                            continue
                        p = tproj("C", blk)
                        ACT(vbtok[:, blk, :], p[:], AF.Copy, p.r, [vbtok.r[blk]])
                    if stage in (42, 43, 44, 45, 46, 47, 48, 49):
                        continue
                    for i in range(2):
                        ACT(EbT[:, i, :], pG[i][:], AF.Exp, pG[i].r, EbT.r, scale=-1.0 / 16)
                        ACT(EnT[:, i, :], pG[i][:], AF.Exp, pG[i].r, EnT.r, scale=1.0 / 16)
                    for i in range(2):
                        p = fproj(f"qb{i}")
                        kb.op(dve, lambda e: e.scalar_tensor_tensor(out=qeT[:, i, :], in0=p[:], scalar=0.125,
                                                                    in1=EbT[:, i, :], op0=ALU.mult, op1=ALU.mult),
                              reads=p.r + EbT.r, writes=qeT.r)
                        p = fproj(f"kb{i}")
                        kb.op(dve, lambda e: e.tensor_tensor(out=keT[:, i, :], in0=p[:], in1=EnT[:, i, :], op=ALU.mult),
                              reads=p.r + EnT.r, writes=keT.r)

                    if stage < 5:
                        continue
                    if len(parts) < 3:
                        kb.op(pool, lambda e: e.memset(mixT[:], 0.0), reads=mixT.r, writes=mixT.r)
                    for blk in range(4 if "gla" in parts else 0):
                        c0 = blk * 128
                        pA2 = [gp(), gp()]
                        for h in range(4):
                            hp = (h % 2) * 64
                            MM(pA2[h % 2][:, (h // 2) * 128:(h // 2 + 1) * 128], keT[hp:hp + 64, h // 2, c0:c0 + 128],
                               qeT[hp:hp + 64, h // 2, c0:c0 + 128], keT.r + qeT.r, pA2[h % 2].r)
                        for h in range(4):
                            kb.op(dve, lambda e: e.tensor_tensor(out=attm[:, h, :],
                                                                 in0=pA2[h % 2][:, (h // 2) * 128:(h // 2 + 1) * 128],
                                                                 in1=tribd[:], op=ALU.mult),
                                  reads=pA2[h % 2].r + tribd.r, writes=attm.r)
                        for cc in range(2):
                            kb.op(dve, lambda e: e.tensor_copy(out=Sbf[cc][:], in_=St[:]), reads=St.r, writes=Sbf[cc].r)
                            kb.op(dve, lambda e: e.tensor_scalar_mul(out=kdm[:, cc, :], in0=kdtok[:, blk, :],
                                                                     scalar1=cmask[:, cc:cc + 1]),
                                  reads=[kdtok.r[blk]] + cmask.r, writes=kdm.r)
                            pK = gp()
                            for h in range(4):
                                hp = (h % 2) * 64
                                MM(pK[hp:hp + 64, (h // 2) * 128:(h // 2 + 1) * 128],
                                   kdm[:, cc, h * 64:(h + 1) * 64], vbtok[:, blk, h * 128:(h + 1) * 128],
                                   kdm.r + [vbtok.r[blk]], pK.r, inc=(h == 3))
                            col = c0 + cc * 64 + 63
                            for i in range(2):
                                kb.op(dve, lambda e: e.scalar_tensor_tensor(
                                    out=St[:, i, :], in0=St[:, i, :], scalar=EbT[:, i, col:col + 1],
                                    in1=pK[:, i * 128:(i + 1) * 128], op0=ALU.mult, op1=ALU.add),
                                    reads=St.r + EbT.r + pK.r, writes=St.r)
                        import os as _os
                        if _os.environ.get("GSKIP") == "1":
                            continue
                        pO = gp()
                        for h in range(4):
                            hp = (h % 2) * 64
                            for cc in range(2):
                                oc = h * 128 + cc * 64
                                MM(pO[:, oc:oc + 64], vbtok[:, blk, h * 128:(h + 1) * 128],
                                   attm[:, h, cc * 64:cc * 64 + 64], [vbtok.r[blk]] + attm.r, pO.r,
                                   start=True, stop=False, inc=False)
                                MM(pO[:, oc:oc + 64], Sbf[cc][hp:hp + 64, h // 2, :],
                                   qeT[hp:hp + 64, h // 2, c0 + cc * 64:c0 + cc * 64 + 64], Sbf[cc].r + qeT.r, pO.r,
                                   start=False, stop=True, inc=(h == 3 and cc == 1))
                        if _os.environ.get("GSKIP") == "2":
                            continue
                        ACT(sqb[:], pO[:], AF.Square, pO.r, sqb.r)
                        pN = gp()
                        MM(pN[:], ones_bf[:], sqb[:], ones_bf.r + sqb.r, pN.r)
                        ACT(rsb[:], pN[:], AF.Sqrt, pN.r + epsc.r, rsb.r, bias=epsc[:], scale=1.0 / 128)
                        kb.op(dve, lambda e: e.reciprocal(out=rsb[:], in_=rsb[:]), reads=rsb.r, writes=rsb.r)
                        kb.op(dve, lambda e: e.tensor_tensor(out=t1[:], in0=pO[:], in1=rsb[:], op=ALU.mult),
                              reads=pO.r + rsb.r, writes=t1.r)
                        kb.op(dve, lambda e: e.tensor_tensor(out=mixT[:, 2:6, c0:c0 + 128],
                                                              in0=t1[:].rearrange("p (h q) -> p h q", h=4),
                                                              in1=SrgT[:, :, c0:c0 + 128], op=ALU.mult),
                              reads=t1.r + SrgT.r, writes=mixT.r)

                    for blk in range(4 if "swa" in parts else 0):
                        jb = t * 4 + blk
                        c0 = blk * 128
                        pO, pD = PS[6], PS[7]
                        kbs = [jb - 1, jb] if jb > 0 else [jb]
                        for n_i, kbk in enumerate(kbs):
                            sl = kbk % 8
                            which = 1 if kbk == jb else 0
                            pL = PS[4 + (n_i % 2)]
                            for kv in range(2):
                                MM(pL[:, kv * 256:(kv + 1) * 256], kcT[kv * 64:kv * 64 + 64, sl, :],
                                   qcT[kv * 64:kv * 64 + 64, :, c0:c0 + 128], [kcT.r[sl]] + qcT.r, pL.r,
                                   start=True, stop=False, inc=False)
                                MM(pL[:, kv * 256:(kv + 1) * 256], ident[:],
                                   biasC[which][:, kv * 2:kv * 2 + 2, :].rearrange("p h q -> p (h q)"),
                                   ident.r + biasC[which].r, pL.r, start=False, stop=True, inc=(kv == 1))
                            P = PT[n_i]
                            ACT(P[:], pL[:], AF.Exp, pL.r, P.r)
                            first, lastk = (n_i == 0), (n_i == len(kbs) - 1)
                            MM(pD[:], ones_bf[:], P[:], ones_bf.r + P.r, pD.r, start=first, stop=lastk)
                        for kv in range(2):
                            for g in range(2):
                                hd = kv * 2 + g
                                for n_i, kbk in enumerate(kbs):
                                    sl = kbk % 8
                                    P = PT[n_i]
                                    MM(pO[g * 64:g * 64 + 64, kv * 128:(kv + 1) * 128], vctok[:, sl, kv * 64:kv * 64 + 64],
                                       P[:, hd * 128:(hd + 1) * 128], [vctok.r[sl]] + P.r, pO.r,
                                       start=(n_i == 0), stop=(n_i == len(kbs) - 1))
                        kb.op(dve, lambda e: e.tensor_tensor(out=rsb[:], in0=pD[:],
                                                             in1=sinkexp[:].rearrange("p h q -> p (h q)"), op=ALU.add),
                              reads=pD.r + sinkexp.r, writes=rsb.r)
                        kb.op(dve, lambda e: e.reciprocal(out=rsb[:], in_=rsb[:]), reads=rsb.r, writes=rsb.r)
                        for kv in range(2):
                            for g in range(2):
                                hd = kv * 2 + g
                                kb.op(dve, lambda e: e.tensor_tensor(
                                    out=mixT[g * 64:g * 64 + 64, 6 + kv, c0:c0 + 128],
                                    in0=pO[g * 64:g * 64 + 64, kv * 128:(kv + 1) * 128],
                                    in1=rsb[g * 64:g * 64 + 64, hd * 128:(hd + 1) * 128], op=ALU.mult),
                                    reads=pO.r + rsb.r, writes=mixT.r)

                    def dsa_A(blk):
                        jb = t * 4 + blk
                        c0 = blk * 128
                        N = (jb + 1) * 128
                        negm = negmB[blk % 2]
                        for kt in range((N + 511) // 512):
                            k0 = kt * 512
                            n = min(512, N - k0)
                            kregs = kiT.r[k0 // 128:(k0 + n) // 128]
                            for h in range(4):
                                hp = (h % 2) * 64
                                p = gp()
                                MM(p[:, 0:n], qiT[hp:hp + 64, h // 2, c0:c0 + 128], kiT[hp:hp + 64, k0:k0 + n],
                                   qiT.r + kregs, p.r)
                                if h == 0:
                                    kb.op(dve, lambda e: e.tensor_scalar(
                                        out=score[:, k0:k0 + n], in0=p[:, 0:n], scalar1=0.0,
                                        scalar2=widx[:, blk, 0:1], op0=ALU.max, op1=ALU.mult),
                                        reads=p.r + [widx.r[blk]], writes=score.r)
                                else:
                                    tr = tmpR[h % 2]
                                    ACT(tr[:, 0:n], p[:, 0:n], AF.Relu, p.r, tr.r)
                                    kb.op(dve, lambda e: e.scalar_tensor_tensor(
                                        out=score[:, k0:k0 + n], in0=tr[:, 0:n], scalar=widx[:, blk, h:h + 1],
                                        in1=score[:, k0:k0 + n], op0=ALU.mult, op1=ALU.add),
                                        reads=tr.r + [widx.r[blk]] + score.r, writes=score.r)
                            kb.op(dve, lambda e: e.scalar_tensor_tensor(
                                out=score[:, k0:k0 + n], in0=ramp512[:, 0:n], scalar=-TIE * k0,
                                in1=score[:, k0:k0 + n], op0=ALU.add, op1=ALU.add),
                                reads=ramp512.r + score.r, writes=score.r)
                        kb.op(pool, lambda e: e.memset(score[0:64, N - 64:N], -3e38), reads=score.r, writes=score.r)
                        if N > 256:
                            for r in range(32):
                                kb.op(dve, lambda e: e.max(out=m8[:], in_=score[:, 0:N]), reads=score.r, writes=m8.r)
                                kb.op(dve, lambda e: e.match_replace(out=score[:, 0:N], in_to_replace=m8[:],
                                                                     in_values=score[:, 0:N], imm_value=-1e30),
                                      reads=score.r + m8.r, writes=score.r)
                            kb.op(dve, lambda e: e.tensor_scalar(out=negm[:, 0:N], in0=score[:, 0:N], scalar1=-1e30,
                                                                 scalar2=NEG, op0=ALU.not_equal, op1=ALU.mult),
                                  reads=score.r, writes=negm.r)
                        else:
                            kb.op(dve, lambda e: e.tensor_scalar(out=negm[:, 0:N], in0=score[:, 0:N], scalar1=-1e29,
                                                                 scalar2=NEG, op0=ALU.is_lt, op1=ALU.mult),
                                  reads=score.r, writes=negm.r)

                    def dsa_B(blk):
                        jb = t * 4 + blk
                        c0 = blk * 128
                        negm = negmB[blk % 2]
                        pO, pD = PS[6], PS[7]
                        for kbk in range(jb + 1):
                            pL = PS[4 + (kbk % 2)]
                            near = kbk >= jb - 1
                            MM(pL[:], cT[:, kbk * 128:(kbk + 1) * 128], qlT[:, :, c0:c0 + 128], [cT.r[kbk]] + qlT.r, pL.r,
                               start=True, stop=False, inc=False)
                            MM(pL[:], negm[:, kbk * 128:(kbk + 1) * 128], ident4[:].rearrange("p h q -> p (h q)"),
                               negm.r + ident4.r, pL.r, start=False, stop=not near, inc=not near)
                            if near:
                                which = 1 if kbk == jb else 0
                                MM(pL[:], ident[:], biasA[which][:].rearrange("p h q -> p (h q)"),
                                   ident.r + biasA[which].r, pL.r, start=False, stop=True)
                            P = nextPT()
                            ACT(P[:], pL[:], AF.Exp, pL.r, P.r)
                            MM(pO[:], ctok[:, kbk, :], P[:], [ctok.r[kbk]] + P.r, pO.r, start=(kbk == 0), stop=(kbk == jb),
                               inc=False)
                            MM(pD[:], ones_bf[:], P[:], ones_bf.r + P.r, pD.r + pO.r, start=(kbk == 0), stop=(kbk == jb))
                        kb.op(dve, lambda e: e.reciprocal(out=rsbB[:], in_=pD[:]), reads=pD.r, writes=rsbB.r)
                        kb.op(dve, lambda e: e.tensor_tensor(out=olT[:].rearrange("p h q -> p (h q)"), in0=pO[:],
                                                             in1=rsbB[:], op=ALU.mult),
                              reads=pO.r + rsbB.r, writes=olT.r)
                        pF = gp()
                        for h in range(4):
                            hp = (h % 2) * 64
                            MM(pF[hp:hp + 64, (h // 2) * 128:(h // 2 + 1) * 128], wuv[:, h * 64:(h + 1) * 64], olT[:, h, :],
                               wuv.r + olT.r, pF.r, inc=(h == 3))
                        ACT(mixT[:, 0:2, c0:c0 + 128], pF[:, 0:256].rearrange("p (i q) -> p i q", i=2), AF.Copy, pF.r,
                            mixT.r)

                    if "dsa" in parts:
                        dsa_A(0)
                        for blk in range(4):
                            if blk + 1 < 4:
                                dsa_A(blk + 1)
                            dsa_B(blk)

                    kb.dma(sp, Xt[:], src[gt * TT:(gt + 1) * TT, :].rearrange("(s p) d -> p s d", p=128),
                           reads=[src_regs[gt]], writes=Xt.r, semreg=Xt.r[0])
                    for s in range(4):
                        for hh in range(2):
                            po = gp()
                            for c in range(8):
                                MM(po[:], mixT[:, c, s * 128:(s + 1) * 128], Wo[:, c, hh * 512:(hh + 1) * 512],
                                   mixT.r + Wo.r, po.r, start=(c == 0), stop=(c == 7), inc=(c == 7))
                            kb.op(dve, lambda e: e.tensor_tensor(
                                out=Xt[:, s, hh * 512:(hh + 1) * 512], in0=Xt[:, s, hh * 512:(hh + 1) * 512],
                                in1=po[:], op=ALU.add), reads=Xt.r + po.r, writes=Xt.r)
                    kb.dma(sp, xs[gt * TT:(gt + 1) * TT, :].rearrange("(s p) d -> p s d", p=128), Xt[:],
                           reads=Xt.r, writes=[xs.r[gt]], semreg=Xt.r[0])

        cur, cur_regs = x_in, xin_regs
        for l in range(DEPTH):
            if do_mixer:
                with ExitStack() as pes:
                    mixer_phase(l, cur, cur_regs, pes)
                    kb.barrier()
                cur, cur_regs = xs.t, xs.r
            if do_ffn:
                with ExitStack() as pes:
                    ffn_phase(l, cur, cur_regs, last=(l == DEPTH - 1), pes=pes)
                    kb.barrier()
                cur, cur_regs = xs.t, xs.r

        for r in yout_regs:
            sp.wait(r.last_w)
        print(f"[build] instructions={kb.ninst} sems={kb.nsem}", flush=True)
    return nc


_W_NAMES = ["w_in", "w_out", "norm_mix", "norm_ffn", "kv_norm", "w_uk", "w_uv", "w_gate2", "b_gate", "gla_norm",
            "sinks", "w_ffn_gate", "w_ffn_up", "w_ffn_down", "final_norm"]


def make_in_maps(inputs, n_cores, nseq, S):
    x = np.ascontiguousarray(inputs["x"], dtype=np.float32)
    L = inputs["w_in"].shape[0]
    shared = {}
    for k in _W_NAMES:
        a = np.ascontiguousarray(inputs[k], dtype=np.float32)
        if k == "final_norm":
            a = a.reshape(1, D)
        if k in ("w_uk", "w_uv"):
            a = a.reshape(L, 128, 256)
        shared[k] = a
    cA, cC, mC, bfar = _host_consts(inputs["rel_bias"])
    shared.update(cA=cA, cC=cC, mC=mC, bfar=bfar)
    in_maps = []
    for c in range(n_cores):
        m = dict(shared)
        m["x"] = x[c * nseq:(c + 1) * nseq].reshape(nseq * S, D)
        in_maps.append(m)
    return in_maps


def kernel(**inputs):
    n = 8
    B, S, _ = inputs["x"].shape
    nseq = B // n
    nc = build(S=S, NSEQ=nseq, DEPTH=inputs["w_in"].shape[0])
    in_maps = make_in_maps(inputs, n, nseq, S)
    res = run_bass_kernel_spmd(nc, in_maps, core_ids=list(range(n)))
    out = np.concatenate([r["y"].reshape(nseq, S, D) for r in res.results], axis=0)
    return out.astype(np.float32)
```

```python
import numpy as np
from contextlib import ExitStack
import concourse.bass as bass
import concourse.mybir as mybir
from concourse.bass_utils import run_bass_kernel_spmd

F32 = mybir.dt.float32
BF16 = mybir.dt.bfloat16
ALU = mybir.AluOpType
AF = mybir.ActivationFunctionType
AX = mybir.AxisListType

D = 1024
DFF = 2816
NFF = DFF // 128
EPS = 1e-6
NEG = -30000.0


class Ev:
    __slots__ = ("sem", "val")

    def __init__(self, sem, val):
        self.sem = sem
        self.val = val


class Region:
    __slots__ = ("last_w", "readers", "dsem", "dcnt", "name", "excl")

    def __init__(self, name, excl=False):
        self.name = name
        self.excl = excl
        self.last_w = None
        self.readers = {}
        self.dsem = None
        self.dcnt = 0


class Tile:
    def __init__(self, t, regs):
        self.t = t
        self.r = regs

    def __getitem__(self, idx):
        return self.t[idx]


class Q:
    def __init__(self, kb, eng, name, is_pe=False):
        self.eng = eng
        self.name = name
        self.is_pe = is_pe
        self.sem = kb.newsem("q_" + name)
        self.cnt = 0
        self.known = {}

    def wait(self, ev):
        if ev is None:
            return
        if ev.sem is self.sem and self.is_pe:
            return
        k = id(ev.sem)
        if self.known.get(k, 0) >= ev.val:
            return
        self.eng.wait_ge(ev.sem, ev.val)
        self.known[k] = ev.val


class KB:
    def __init__(self, nc, es):
        self.nc = nc
        self.es = es
        self.nsem = 0
        self.pe = Q(self, nc.tensor, "pe", is_pe=True)
        self.act = Q(self, nc.scalar, "act")
        self.dve = Q(self, nc.vector, "dve")
        self.pool = Q(self, nc.gpsimd, "pool")
        self.sp = Q(self, nc.sync, "sp")
        self.ninst = 0
        self.dregs = []
        self.uid = 0

    def newsem(self, name):
        self.nsem += 1
        return self.es.enter_context(self.nc.semaphore(name))

    def sb(self, name, shape, dt, nreg=1, es=None):
        self.uid += 1
        name = f"{name}_{self.uid}"
        t = (es or self.es).enter_context(self.nc.sbuf_tensor(name, list(shape), dt))
        return Tile(t, [Region(f"{name}.{i}") for i in range(nreg)])

    def barrier(self):
        qs = [self.pe, self.act, self.dve, self.pool, self.sp]
        for q in qs:
            for q2 in qs:
                if q2 is not q and q2.cnt > 0:
                    q.wait(Ev(q2.sem, q2.cnt))
            for r in self.dregs:
                q.wait(Ev(r.dsem, r.dcnt))

    def ps(self, name, shape=(128, 512), dt=F32):
        t = self.es.enter_context(self.nc.psum_tensor(name, list(shape), dt))
        return Tile(t, [Region(name, excl=True)])

    def dram(self, name, shape, dt, nreg=1, kind="Internal"):
        t = self.nc.dram_tensor(name, list(shape), dt, kind=kind)
        return Tile(t.ap(), [Region(f"{name}.{i}") for i in range(nreg)])

    def _deps(self, q, reads, writes):
        for r in reads:
            q.wait(r.last_w)
            if r.excl:
                for ev in r.readers.values():
                    if ev.sem is not q.sem:
                        q.wait(ev)
        for w in writes:
            q.wait(w.last_w)
            for ev in w.readers.values():
                q.wait(ev)

    def _record(self, ev, reads, writes):
        for r in reads:
            r.readers[id(ev.sem)] = ev
        for w in writes:
            w.last_w = ev
            w.readers = {}

    def op(self, q, fn, reads=(), writes=(), inc=True):
        self._deps(q, reads, writes)
        inst = fn(q.eng)
        self.ninst += 1
        ev = Ev(q.sem, q.cnt + 1)
        if inc:
            inst.then_inc(q.sem, 1)
            q.cnt += 1
        self._record(ev, reads, writes)
        return ev

    def dma(self, q, out, in_, reads, writes, semreg, **kw):
        self._deps(q, reads, writes)
        if semreg.dsem is None:
            semreg.dsem = self.newsem("d_" + semreg.name.replace(".", "_"))
            self.dregs.append(semreg)
        inst = q.eng.dma_start(out=out, in_=in_, **kw)
        semreg.dcnt += 16
        inst.then_inc(semreg.dsem, 16)
        self.ninst += 1
        ev = Ev(semreg.dsem, semreg.dcnt)
        self._record(ev, reads, writes)
        return ev


def _bucket(rel):
    rel = np.asarray(rel, dtype=np.int64)
    n = np.abs(rel)
    side = np.where(rel > 0, 16, 0)
    nf = np.maximum(n, 1).astype(np.float32)
    large = 8 + (np.log(nf / np.float32(8)) / np.float32(np.log(16.0)) * np.float32(8)).astype(np.int32)
    large = np.minimum(large, 15)
    return (side + np.where(n < 8, n, large)).astype(np.int64)


def _host_consts(rel_bias):
    rel_bias = np.asarray(rel_bias, dtype=np.float32)
    s = np.arange(128)[:, None]
    q = np.arange(128)[None, :]
    ip = _bucket(s - 128 - q)
    idg = _bucket(s - q)
    g = np.stack([rel_bias[ip], rel_bias[idg]])
    cA = np.ascontiguousarray(g[..., 0:4].transpose(0, 1, 3, 2)).reshape(2, 128, 512)
    cC = np.ascontiguousarray(g[..., 4:8].transpose(0, 1, 3, 2)).reshape(2, 128, 512)
    bfar = np.ascontiguousarray(rel_bias[15:16, 0:4])
    mp = np.where((q >= 64) & (s < 64), NEG, 0.0).astype(np.float32)
    md = np.where((q < 64) & (s >= 64), NEG, 0.0).astype(np.float32)
    mC = np.stack([np.broadcast_to(mp[:, None, :], (128, 4, 128)),
                   np.broadcast_to(md[:, None, :], (128, 4, 128))]).reshape(2, 128, 512)
    return cA, cC, np.ascontiguousarray(mC, dtype=np.float32), bfar


_FCH = [
    ("qa0", [(0, 128)]), ("qa1", [(128, 128)]),
    ("qi0", [(384, 128)]), ("qi1", [(512, 128)]),
    ("ki", [(640, 64), (640, 64)]),
    ("glr", [(1732, 16)]),
    ("qb0", [(708, 128)]), ("qb1", [(836, 128)]),
    ("kb0", [(964, 128)]), ("kb1", [(1092, 128)]),
    ("rb0", [(1748, 128)]), ("rb1", [(1876, 128)]), ("rb2", [(2004, 128)]), ("rb3", [(2132, 128)]),
    ("qc0", [(2260, 64), (2388, 64)]), ("qc1", [(2324, 64), (2452, 64)]),
    ("kc", [(2516, 128)]),
]
_TCH = [("A", [(256, 128), (704, 4)]), ("B", [(964, 256), (2644, 128)]), ("C", [(1220, 512)])]


def build(S=4096, NSEQ=2, DEPTH=2, do_mixer=True, do_ffn=True, xbuf=1, parts=("gla", "swa", "dsa"), stage=99):
    nc = bass.Bass("TRN2", target_bir_lowering=False)
    NT = NSEQ * S
    TT = 512
    NTILE = NT // TT
    TPS = S // TT
    NB = S // 128

    def din(name, shape):
        return nc.dram_tensor(name, list(shape), F32, kind="ExternalInput").ap()

    x_in = din("x", [NT, D])
    w_in = din("w_in", [DEPTH, D, 2772])
    w_out = din("w_out", [DEPTH, D, D])
    n_mix = din("norm_mix", [DEPTH, D])
    n_ffn = din("norm_ffn", [DEPTH, D])
    kv_norm = din("kv_norm", [DEPTH, 128])
    w_uk = din("w_uk", [DEPTH, 128, 256])
    w_uv = din("w_uv", [DEPTH, 128, 256])
    w_g2 = din("w_gate2", [DEPTH, 16, 256])
    b_g = din("b_gate", [DEPTH, 256])
    gla_n = din("gla_norm", [DEPTH, 512])
    sinks = din("sinks", [DEPTH, 4])
    w_g = din("w_ffn_gate", [DEPTH, D, DFF])
    w_u = din("w_ffn_up", [DEPTH, D, DFF])
    w_d = din("w_ffn_down", [DEPTH, DFF, D])
    n_fin = din("final_norm", [1, D])
    cA_d = din("cA", [2, 128, 512])
    cC_d = din("cC", [2, 128, 512])
    mC_d = din("mC", [2, 128, 512])
    bfar_d = din("bfar", [1, 4])
    y_out = nc.dram_tensor("y", [NT, D], F32, kind="ExternalOutput").ap()

    with ExitStack() as es:
        kb = KB(nc, es)
        pe, act, dve, pool, sp = kb.pe, kb.act, kb.dve, kb.pool, kb.sp

        xs = kb.dram("xs", [NT, D], F32, nreg=NTILE)
        xin_regs = [Region(f"xin.{i}") for i in range(NTILE)]
        yout_regs = [Region(f"yout.{i}") for i in range(NTILE)]

        def MM(out, lhsT, rhs, reads, wr, start=True, stop=True, inc=True):
            return kb.op(pe, lambda e: e.matmul(out, lhsT=lhsT, rhs=rhs, start=start, stop=stop),
                         reads=reads, writes=wr, inc=inc)

        def ACT(out, in_, func, reads, wr, **kw):
            return kb.op(act, lambda e: e.activation(out=out, in_=in_, func=func, **kw), reads=reads, writes=wr)

        identf = kb.sb("identf", [128, 128], F32)
        ident = kb.sb("ident", [128, 128], BF16)
        ident4 = kb.sb("ident4", [128, 4, 128], BF16)
        ones_bf = kb.sb("ones_bf", [128, 128], BF16)
        tribd = kb.sb("tribd", [128, 128], F32)
        ones_f = kb.sb("ones_f", [128, 128], F32)
        ramp512 = kb.sb("ramp512", [128, 512], F32)
        cmask = kb.sb("cmask", [128, 2], F32)
        subd = kb.sb("subd", [128, 128], F32)
        epsc = kb.sb("epsc", [128, 1], F32)
        onec = kb.sb("onec", [128, 1], F32)
        thr_all = kb.sb("thr_all", [128, 1], F32)
        kb.op(pool, lambda e: e.memset(identf[:], 0.0), writes=identf.r)
        kb.op(pool, lambda e: e.affine_select(out=identf[:], in_=identf[:], pattern=[[-1, 128]],
                                              compare_op=ALU.not_equal, fill=1.0, base=0, channel_multiplier=1),
              reads=identf.r, writes=identf.r)
        kb.op(dve, lambda e: e.tensor_copy(out=ident[:], in_=identf[:]), reads=identf.r, writes=ident.r)
        for h in range(4):
            kb.op(dve, lambda e: e.tensor_copy(out=ident4[:, h, :], in_=identf[:]), reads=identf.r, writes=ident4.r)
        kb.op(pool, lambda e: e.memset(ones_bf[:], 1.0), writes=ones_bf.r)
        kb.op(pool, lambda e: e.memset(epsc[:], EPS), writes=epsc.r)
        kb.op(pool, lambda e: e.memset(onec[:], 1.0), writes=onec.r)
        kb.op(pool, lambda e: e.memset(thr_all[:], -1e29), writes=thr_all.r)
        kb.op(pool, lambda e: e.memset(tribd[:], 1.0), writes=tribd.r)
        kb.op(pool, lambda e: e.affine_select(out=tribd[:], in_=tribd[:], pattern=[[1, 128]],
                                              compare_op=ALU.is_ge, fill=0.0, base=0, channel_multiplier=-1),
              reads=tribd.r, writes=tribd.r)
        kb.op(pool, lambda e: e.memset(tribd[0:64, 64:128], 0.0), reads=tribd.r, writes=tribd.r)
        kb.op(pool, lambda e: e.memset(cmask[:], 0.0), writes=cmask.r)
        kb.op(pool, lambda e: e.memset(cmask[0:64, 0:1], 1.0), reads=cmask.r, writes=cmask.r)
        kb.op(pool, lambda e: e.memset(cmask[64:128, 1:2], 1.0), reads=cmask.r, writes=cmask.r)
        kb.op(pool, lambda e: e.memset(subd[:], 1.0), writes=subd.r)
        kb.op(pool, lambda e: e.affine_select(out=subd[:], in_=subd[:], pattern=[[-1, 128]],
                                              compare_op=ALU.is_gt, fill=0.0, base=0, channel_multiplier=1),
              reads=subd.r, writes=subd.r)
        kb.op(pool, lambda e: e.memset(subd[64:128, 0:64], 0.0), reads=subd.r, writes=subd.r)

        TIE = 1e-9
        KBIS = 24
        pw = kb.sb("pw", [128, KBIS + 1], F32)
        for k in range(KBIS + 1):
            kb.op(pool, lambda e: e.memset(pw[:, k:k + 1], 2.0 ** -(k + 1)), reads=pw.r, writes=pw.r)
        c256 = kb.sb("c256", [128, 1], F32)
        kb.op(pool, lambda e: e.memset(c256[:], 256.0), writes=c256.r)
        kb.op(pool, lambda e: e.memset(ones_f[:], 1.0), writes=ones_f.r)
        gfin = kb.sb("gfin", [128, D], F32)
        kb.dma(sp, gfin[:], n_fin[0].partition_broadcast(128), reads=(), writes=gfin.r, semreg=gfin.r[0])
        gffn = kb.sb("gffn", [128, DEPTH * 8], F32)
        gmix = kb.sb("gmix", [128, DEPTH * 8], F32)
        with nc.allow_non_contiguous_dma(reason="tiny gain vectors"):
            kb.dma(sp, gffn[:], n_ffn.rearrange("l (k p) -> p (l k)", p=128), reads=(), writes=gffn.r,
                   semreg=gffn.r[0])
            kb.dma(sp, gmix[:], n_mix.rearrange("l (k p) -> p (l k)", p=128), reads=(), writes=gmix.r,
                   semreg=gmix.r[0])

        X = [kb.sb(f"X{i}", [128, 4, D], F32) for i in range(xbuf)]
        ss = kb.sb("ss", [128, 4], F32)
        rstd = kb.sb("rstd", [128, 4], F32)
        hT = kb.sb("hT", [128, 8, TT], BF16)
        PS = [kb.ps(f"ps{i}") for i in range(8)]
        gstate = {"i": 0}

        def gp():
            gstate["i"] += 1
            return PS[gstate["i"] % 4]

        pr = PS[0]
        MM(pr[:, 0:128], ones_f[:], tribd[:], ones_f.r + tribd.r, pr.r)
        for c8 in range(8):
            kb.op(dve, lambda e: e.tensor_scalar(out=ramp512[:, c8 * 64:(c8 + 1) * 64],
                                                 in0=pr[:, 0:64], scalar1=-TIE,
                                                 scalar2=-TIE * 64 * c8, op0=ALU.mult, op1=ALU.add),
                  reads=pr.r, writes=ramp512.r)

        def rstd_from_ss():
            ACT(rstd[:], ss[:], AF.Sqrt, ss.r + epsc.r, rstd.r, bias=epsc[:], scale=1.0 / D)
            kb.op(dve, lambda e: e.reciprocal(out=rstd[:], in_=rstd[:]), reads=rstd.r, writes=rstd.r)

        def rmsnorm_T(Xt, gain_cols, xn, junk):
            for s in range(4):
                ACT(junk[:], Xt[:, s, :], AF.Square, Xt.r, junk.r + ss.r, accum_out=ss[:, s:s + 1])
            rstd_from_ss()
            for s in range(4):
                ACT(xn[:, s, :], Xt[:, s, :], AF.Copy, Xt.r + rstd.r, xn.r, scale=rstd[:, s:s + 1])
            for c in range(8):
                p = gp()
                for s in range(4):
                    MM(p[:, s * 128:(s + 1) * 128], xn[:, s, c * 128:(c + 1) * 128], ident[:],
                       xn.r + ident.r, p.r, inc=(s == 3))
                kb.op(dve, lambda e: e.tensor_scalar_mul(out=hT[:, c, :], in0=p[:], scalar1=gain_cols[c]),
                      reads=p.r, writes=hT.r)

        def ffn_phase(l, src, src_regs, last, pes):
            Wg = kb.sb("Wg", [128, 8, DFF], BF16, es=pes)
            Wu = kb.sb("Wu", [128, 8, DFF], BF16, es=pes)
            Wd = kb.sb("Wd", [128, NFF, D], BF16, es=pes)
            aT = kb.sb("aT", [128, NFF, TT], BF16, es=pes)
            xn = Tile(aT[:, 0:8, :].rearrange("p (s a) t -> p s (a t)", s=4), aT.r)
            sg = [kb.sb(f"sg{i}", [128, TT], F32, es=pes) for i in range(2)]
            junk = kb.sb("junk", [128, D], BF16, es=pes)
            HC = DFF // 2
            for hh in range(2):
                kb.dma(pool, Wg[:, :, hh * HC:(hh + 1) * HC],
                       w_g[l].rearrange("(k p) n -> p k n", p=128)[:, :, hh * HC:(hh + 1) * HC],
                       reads=(), writes=Wg.r, semreg=Wg.r[0])
                kb.dma(pool, Wu[:, :, hh * HC:(hh + 1) * HC],
                       w_u[l].rearrange("(k p) n -> p k n", p=128)[:, :, hh * HC:(hh + 1) * HC],
                       reads=(), writes=Wu.r, semreg=Wu.r[0])
            for hh in range(2):
                kb.dma(pool, Wd[:, hh * 11:(hh + 1) * 11, :],
                       w_d[l].rearrange("(k p) n -> p k n", p=128)[:, hh * 11:(hh + 1) * 11, :],
                       reads=(), writes=Wd.r, semreg=Wd.r[0])
            gcols = [gffn[:, l * 8 + c:l * 8 + c + 1] for c in range(8)]
            for t in range(NTILE):
                Xt = X[t % xbuf]
                kb.dma(sp, Xt[:], src[t * TT:(t + 1) * TT, :].rearrange("(s p) d -> p s d", p=128),
                       reads=[src_regs[t]], writes=Xt.r, semreg=Xt.r[0])
                rmsnorm_T(Xt, gcols, xn, junk)
                for f in range(NFF):
                    pg = PS[4 + (f % 2)]
                    pu = PS[6 + (f % 2)]
                    for k in range(8):
                        MM(pg[:], Wg[:, k, f * 128:(f + 1) * 128], hT[:, k, :], Wg.r + hT.r, pg.r,
                           start=(k == 0), stop=(k == 7), inc=(k == 7))
                    for k in range(8):
                        MM(pu[:], Wu[:, k, f * 128:(f + 1) * 128], hT[:, k, :], Wu.r + hT.r, pu.r,
                           start=(k == 0), stop=(k == 7), inc=(k == 7))
                    sgt = sg[f % 2]
                    ACT(sgt[:], pg[:], AF.Silu, pg.r, sgt.r)
                    kb.op(dve, lambda e: e.tensor_tensor(out=aT[:, f, :], in0=sgt[:], in1=pu[:], op=ALU.mult),
                          reads=sgt.r + pu.r, writes=aT.r)
                for s in range(4):
                    for hh in range(2):
                        po = gp()
                        for f in range(NFF):
                            MM(po[:], aT[:, f, s * 128:(s + 1) * 128], Wd[:, f, hh * 512:(hh + 1) * 512],
                               aT.r + Wd.r, po.r, start=(f == 0), stop=(f == NFF - 1), inc=(f == NFF - 1))
                        kb.op(dve, lambda e: e.tensor_tensor(
                            out=Xt[:, s, hh * 512:(hh + 1) * 512], in0=Xt[:, s, hh * 512:(hh + 1) * 512],
                            in1=po[:], op=ALU.add), reads=Xt.r + po.r, writes=Xt.r)
                if last:
                    for s in range(4):
                        ACT(junk[:], Xt[:, s, :], AF.Square, Xt.r, junk.r + ss.r, accum_out=ss[:, s:s + 1])
                    rstd_from_ss()
                    for s in range(4):
                        kb.op(dve, lambda e: e.scalar_tensor_tensor(
                            out=Xt[:, s, :], in0=Xt[:, s, :], scalar=rstd[:, s:s + 1], in1=gfin[:],
                            op0=ALU.mult, op1=ALU.mult), reads=Xt.r + rstd.r + gfin.r, writes=Xt.r)
                    kb.dma(sp, y_out[t * TT:(t + 1) * TT, :].rearrange("(s p) d -> p s d", p=128), Xt[:],
                           reads=Xt.r, writes=[yout_regs[t]], semreg=Xt.r[0])
                else:
                    kb.dma(sp, xs[t * TT:(t + 1) * TT, :].rearrange("(s p) d -> p s d", p=128), Xt[:],
                           reads=Xt.r, writes=[xs.r[t]], semreg=Xt.r[0])

        def mixer_phase(l, src, src_regs, pes):
            def sb(name, shape, dt, nreg=1):
                return kb.sb(name, shape, dt, nreg=nreg, es=pes)

            foff, fw, off = {}, {}, 0
            for name, pieces in _FCH:
                foff[name] = off
                fw[name] = sum(n for _, n in pieces)
                off += fw[name]
            NFC = off
            toff, tw, off = {}, {}, 0
            for name, pieces in _TCH:
                toff[name] = off
                tw[name] = sum(n for _, n in pieces)
                off += tw[name]
            NTC = off
            Wf = sb("Wf", [128, 8, NFC], BF16)
            Wt = sb("Wt", [128, 8, NTC], BF16)
            Wo = sb("Wo", [128, 8, D], BF16)
            wsrc = w_in[l].rearrange("(k p) n -> p k n", p=128)
            for name, pieces in _FCH:
                o = foff[name]
                for c0, n in pieces:
                    kb.dma(pool, Wf[:, :, o:o + n], wsrc[:, :, c0:c0 + n], reads=(), writes=Wf.r, semreg=Wf.r[0])
                    o += n
            for name, pieces in _TCH:
                o = toff[name]
                for c0, n in pieces:
                    kb.dma(pool, Wt[:, :, o:o + n], wsrc[:, :, c0:c0 + n], reads=(), writes=Wt.r, semreg=Wt.r[0])
                    o += n
            for hh in range(2):
                kb.dma(pool, Wo[:, :, hh * 512:(hh + 1) * 512],
                       w_out[l].rearrange("(k p) n -> p k n", p=128)[:, :, hh * 512:(hh + 1) * 512],
                       reads=(), writes=Wo.r, semreg=Wo.r[0])

            kvn = sb("kvn", [128, 128], F32)
            wukn = sb("wukn", [128, 256], F32)
            wuvn = sb("wuvn", [128, 256], F32)
            wg2a = sb("wg2a", [33, 256], F32)
            gn = sb("gn", [128, 4], F32)
            sinkb = sb("sinkb", [128, 4], F32)
            bfar = sb("bfar", [128, 4], F32)
            cst = sb("cst", [128, 512], F32)
            cst2 = sb("cst2", [128, 512], F32)
            kb.dma(sp, kvn[:], kv_norm[l].partition_broadcast(128), reads=(), writes=kvn.r, semreg=kvn.r[0])
            kb.dma(sp, wukn[:], w_uk[l], reads=(), writes=wukn.r, semreg=wukn.r[0])
            kb.dma(sp, wuvn[:], w_uv[l], reads=(), writes=wuvn.r, semreg=wuvn.r[0])
            kb.op(pool, lambda e: e.memset(wg2a[:], 0.0), writes=wg2a.r)
            kb.dma(sp, wg2a[0:16, :], w_g2[l], reads=(), writes=wg2a.r, semreg=wg2a.r[0])
            kb.dma(sp, wg2a[32:33, :], b_g[l:l + 1, :], reads=(), writes=wg2a.r, semreg=wg2a.r[0])
            with nc.allow_non_contiguous_dma(reason="tiny gain vectors"):
                kb.dma(sp, gn[:], gla_n[l].rearrange("(h e) -> e h", e=128), reads=(), writes=gn.r, semreg=gn.r[0])
            kb.dma(sp, sinkb[:], sinks[l].partition_broadcast(128), reads=(), writes=sinkb.r, semreg=sinkb.r[0])
            kb.dma(sp, bfar[:], bfar_d[0].partition_broadcast(128), reads=(), writes=bfar.r, semreg=bfar.r[0])

            if stage < 1:
                return
            wukT = sb("wukT", [128, 2, 128], BF16)
            wuv = sb("wuv", [128, 256], BF16)
            for i in range(2):
                p = gp()
                MM(p[:, 0:128], wukn[:, i * 128:(i + 1) * 128], identf[:], wukn.r + identf.r, p.r)
                ACT(wukT[:, i, :], p[:, 0:128], AF.Copy, p.r, wukT.r, scale=0.125)
            kb.op(dve, lambda e: e.tensor_copy(out=wuv[:], in_=wuvn[:]), reads=wuvn.r, writes=wuv.r)
            sinkexp = sb("sinkexp", [128, 4, 128], F32)
            ACT(sinkb[:], sinkb[:], AF.Exp, sinkb.r, sinkb.r)
            kb.op(pool, lambda e: e.memset(sinkexp[:], 0.0), writes=sinkexp.r)
            for h in range(4):
                kb.op(dve, lambda e: e.tensor_scalar_add(out=sinkexp[:, h, :], in0=sinkexp[:, h, :],
                                                         scalar1=sinkb[:, h:h + 1]),
                      reads=sinkexp.r + sinkb.r, writes=sinkexp.r)
            biasA = [sb(f"biasA{i}", [128, 4, 128], BF16) for i in range(2)]
            biasC = [sb(f"biasC{i}", [128, 4, 128], BF16) for i in range(2)]
            for i in range(2):
                kb.dma(sp, cst[:], cA_d[i], reads=(), writes=cst.r, semreg=cst.r[0])
                for h in range(4):
                    kb.op(dve, lambda e: e.tensor_scalar_sub(out=biasA[i][:, h, :], in0=cst[:, h * 128:(h + 1) * 128],
                                                             scalar1=bfar[:, h:h + 1]),
                          reads=cst.r + bfar.r, writes=biasA[i].r)
                kb.dma(sp, cst[:], cC_d[i], reads=(), writes=cst.r, semreg=cst.r[0])
                kb.dma(sp, cst2[:], mC_d[i], reads=(), writes=cst2.r, semreg=cst2.r[0])
                kb.op(dve, lambda e: e.tensor_tensor(out=biasC[i][:].rearrange("p h q -> p (h q)"), in0=cst[:],
                                                     in1=cst2[:], op=ALU.add),
                      reads=cst.r + cst2.r, writes=biasC[i].r)

            if stage < 2:
                return
            kiT = sb("kiT", [128, S], BF16, nreg=NB)
            cT = sb("cT", [128, S], BF16, nreg=NB)
            ctok = sb("ctok", [128, NB, 128], BF16, nreg=NB)
            kcT = sb("kcT", [128, 8, 128], BF16, nreg=8)
            vctok = sb("vctok", [128, 8, 128], BF16, nreg=8)
            St = sb("St", [128, 2, 128], F32)
            Sbf = [sb(f"Sbf{i}", [128, 2, 128], BF16) for i in range(2)]
            qaT = sb("qaT", [128, 2, TT], BF16)
            qlT = sb("qlT", [128, 4, TT], BF16)
            qiT = sb("qiT", [128, 2, TT], BF16)
            glra = sb("glra", [33, TT], F32)
            sptok1 = sb("sptok", [128, 256], F32)
            eDk = sb("eDk", [128, 256], F32)
            EbT = sb("EbT", [128, 2, TT], F32)
            EnT = sb("EnT", [128, 2, TT], BF16)
            qeT = sb("qeT", [128, 2, TT], BF16)
            keT = sb("keT", [128, 2, TT], BF16)
            SrgT = sb("SrgT", [128, 4, TT], BF16)
            qcT = sb("qcT", [128, 2, TT], BF16)
            kdtok = sb("kdtok", [128, 4, 256], BF16, nreg=4)
            vbtok = sb("vbtok", [128, 4, 512], BF16, nreg=4)
            widx = sb("widx", [128, 4, 4], F32, nreg=4)
            ssc = sb("ssc", [128, 1], F32)
            rc = sb("rc", [128, 1], F32)
            mixT = hT
            attm = sb("attm", [128, 4, 128], BF16)
            kdm = sb("kdm", [128, 2, 256], BF16)
            sqb = sb("sqb", [128, 512], BF16)
            PT = [sb(f"PT{i}", [128, 512], BF16) for i in range(2)]
            assert xbuf == 1
            score = Tile(X[0][:].rearrange("p s d -> p (s d)"), X[0].r)
            negm0 = sb("negm0", [128, 4096], BF16)
            negm1 = sb("negm1", [128, S], BF16)
            negmB = [negm0, negm1]
            xn = Tile(negm0[:, 0:4096].rearrange("p (s d) -> p s d", s=4), negm0.r)
            junk = Tile(negm0[:, 0:1024], negm0.r)
            rsbB = sb("rsbB", [128, 512], F32)
            tmpR = [cst, cst2]
            t1, rsb = cst, cst2
            m8 = sb("m8", [128, 8], F32)
            bs_lo = sb("bs_lo", [128, 1], F32)
            bs_R = sb("bs_R", [128, 1], F32)
            bs_hh = sb("bs_hh", [128, KBIS + 1], F32)
            bs_mid = sb("bs_mid", [128, 1], F32)
            bs_cnt = sb("bs_cnt", [128, 1], F32)
            bs_fh = sb("bs_fh", [128, 1], F32)
            olT = sb("olT", [128, 4, 128], BF16)
            pstate = {"i": 0}

            def nextPT():
                pstate["i"] += 1
                return PT[pstate["i"] % 2]

            kb.op(pool, lambda e: e.memset(glra[:], 0.0), writes=glra.r)
            kb.op(pool, lambda e: e.memset(glra[32:33, :], 1.0), reads=glra.r, writes=glra.r)
            gcols = [gmix[:, l * 8 + c:l * 8 + c + 1] for c in range(8)]

            def fproj(name):
                o, n = foff[name], fw[name]
                p = gp()
                for k in range(8):
                    MM(p[0:n, :], Wf[:, k, o:o + n], hT[:, k, :], Wf.r + hT.r, p.r,
                       start=(k == 0), stop=(k == 7), inc=(k == 7))
                return p

            def tproj(name, blk):
                o, n = toff[name], tw[name]
                p = gp()
                for k in range(8):
                    MM(p[:, 0:n], hT[:, k, blk * 128:(blk + 1) * 128], Wt[:, k, o:o + n], Wt.r + hT.r, p.r,
                       start=(k == 0), stop=(k == 7), inc=(k == 7))
                return p

            for b in range(NSEQ):
                kb.op(pool, lambda e: e.memset(St[:], 0.0), reads=St.r, writes=St.r)
                for t in range(TPS):
                    gt = b * TPS + t
                    Xt = X[gt % xbuf]
                    kb.dma(sp, Xt[:], src[gt * TT:(gt + 1) * TT, :].rearrange("(s p) d -> p s d", p=128),
                           reads=[src_regs[gt]], writes=Xt.r, semreg=Xt.r[0])
                    rmsnorm_T(Xt, gcols, xn, junk)
                    if stage < 3:
                        continue

                    for i in range(2):
                        p = fproj(f"qa{i}")
                        ACT(qaT[:, i, :], p[:], AF.Copy, p.r, qaT.r)
                    for h in range(4):
                        p = gp()
                        hp = (h % 2) * 64
                        MM(p[:], wukT[hp:hp + 64, h // 2, :], qaT[hp:hp + 64, h // 2, :], wukT.r + qaT.r, p.r)
                        kb.op(dve, lambda e: e.tensor_copy(out=qlT[:, h, :], in_=p[:]), reads=p.r, writes=qlT.r)
                    if stage == 31:
                        continue
                    for i in range(2):
                        p = fproj(f"qi{i}")
                        ACT(qiT[:, i, :], p[:], AF.Copy, p.r, qiT.r)
                    p = fproj("ki")
                    kb.op(dve, lambda e: e.tensor_copy(out=kiT[:, t * TT:(t + 1) * TT], in_=p[:]), reads=p.r,
                          writes=kiT.r[t * 4:t * 4 + 4])
                    p = fproj("glr")
                    kb.op(dve, lambda e: e.tensor_copy(out=glra[0:16, :], in_=p[0:16, :]), reads=p.r, writes=glra.r)
                    if stage == 32:
                        continue
                    for i in range(2):
                        p = fproj(f"qc{i}")
                        ACT(qcT[:, i, :], p[:], AF.Copy, p.r, qcT.r, scale=0.125)
                    p = fproj("kc")
                    for blk in range(4):
                        sl = (t * 4 + blk) % 8
                        kb.op(dve, lambda e: e.tensor_copy(out=kcT[:, sl, :], in_=p[:, blk * 128:(blk + 1) * 128]),
                              reads=p.r, writes=[kcT.r[sl]])
                    if stage == 33:
                        continue
                    for i in range(4):
                        p = fproj(f"rb{i}")
                        ACT(SrgT[:, i, :], p[:], AF.Silu, p.r, SrgT.r)
                        kb.op(dve, lambda e: e.tensor_scalar_mul(out=SrgT[:, i, :], in0=SrgT[:, i, :],
                                                                   scalar1=gn[:, i:i + 1]),
                              reads=SrgT.r + gn.r, writes=SrgT.r)

                    if stage < 4 or stage == 41:
                        continue
                    pG = [PS[4], PS[5]]
                    for blk in range(4):
                        jb = t * 4 + blk
                        sl = jb % 8
                        p = tproj("A", blk)
                        ACT(junk[:, 0:128], p[:, 0:128], AF.Square, p.r, junk.r + ssc.r, accum_out=ssc[:])
                        ACT(rc[:], ssc[:], AF.Sqrt, ssc.r + epsc.r, rc.r, bias=epsc[:], scale=1.0 / 128)
                        kb.op(dve, lambda e: e.reciprocal(out=rc[:], in_=rc[:]), reads=rc.r, writes=rc.r)
                        kb.op(dve, lambda e: e.scalar_tensor_tensor(out=ctok[:, jb, :], in0=p[:, 0:128], scalar=rc[:],
                                                                    in1=kvn[:], op0=ALU.mult, op1=ALU.mult),
                              reads=p.r + rc.r + kvn.r, writes=[ctok.r[jb]])
                        kb.op(dve, lambda e: e.tensor_copy(out=widx[:, blk, :], in_=p[:, 128:132]), reads=p.r,
                              writes=[widx.r[blk]])
                        p2 = gp()
                        MM(p2[:, 0:128], ctok[:, jb, :], ident[:], [ctok.r[jb]] + ident.r, p2.r)
                        ACT(cT[:, jb * 128:(jb + 1) * 128], p2[:, 0:128], AF.Copy, p2.r, [cT.r[jb]])
                        if stage == 42:
                            continue
                        pz = gp()
                        MM(pz[:, 0:256], glra[0:33, blk * 128:(blk + 1) * 128], wg2a[0:33, :], glra.r + wg2a.r, pz.r)
                        ACT(sptok1[:], pz[:, 0:256], AF.Exp, pz.r, sptok1.r, scale=-1.0)
                        ACT(sptok1[:], sptok1[:], AF.Ln, sptok1.r + onec.r, sptok1.r,
                            bias=onec[:], scale=1.0)
                        if stage == 43:
                            continue
                        for i in range(2):
                            MM(pG[i][:, blk * 128:(blk + 1) * 128], sptok1[:, i * 128:(i + 1) * 128], tribd[:],
                               sptok1.r + tribd.r, pG[i].r)
                        pd = gp()
                        MM(pd[:, 0:256], subd[:], sptok1[:], sptok1.r + subd.r, pd.r)
                        ACT(eDk[:], pd[:, 0:256], AF.Exp, pd.r, eDk.r, scale=-1.0 / 16)
                        if stage == 44:
                            continue
                        p = tproj("B", blk)
                        if stage == 46:
                            continue
                        if stage != 49:
                            kb.op(dve, lambda e: e.tensor_tensor(out=kdtok[:, blk, :], in0=p[:, 0:256], in1=eDk[:],
                                                                 op=ALU.mult),
                                  reads=p.r + eDk.r, writes=[kdtok.r[blk]])
                        if stage == 48:
                            continue
                        ACT(vctok[:, sl, :], p[:, 256:384], AF.Copy, p.r, [vctok.r[sl]])
                        if stage in (47, 49):
                            continue
                        p = tproj("C", blk)
                        ACT(vbtok[:, blk, :], p[:], AF.Copy, p.r, [vbtok.r[blk]])
                    if stage in (42, 43, 44, 45, 46, 47, 48, 49):
                        continue
                    for i in range(2):
                        ACT(EbT[:, i, :], pG[i][:], AF.Exp, pG[i].r, EbT.r, scale=-1.0 / 16)
                        ACT(EnT[:, i, :], pG[i][:], AF.Exp, pG[i].r, EnT.r, scale=1.0 / 16)
                    for i in range(2):
                        p = fproj(f"qb{i}")
                        kb.op(dve, lambda e: e.scalar_tensor_tensor(out=qeT[:, i, :], in0=p[:], scalar=0.125,
                                                                    in1=EbT[:, i, :], op0=ALU.mult, op1=ALU.mult),
                              reads=p.r + EbT.r, writes=qeT.r)
                        p = fproj(f"kb{i}")
                        kb.op(dve, lambda e: e.tensor_tensor(out=keT[:, i, :], in0=p[:], in1=EnT[:, i, :], op=ALU.mult),
                              reads=p.r + EnT.r, writes=keT.r)

                    if stage < 5:
                        continue
                    if len(parts) < 3:
                        kb.op(pool, lambda e: e.memset(mixT[:], 0.0), reads=mixT.r, writes=mixT.r)
                    for blk in range(4 if "gla" in parts else 0):
                        c0 = blk * 128
                        pA2 = [gp(), gp()]
                        for h in range(4):
                            hp = (h % 2) * 64
                            MM(pA2[h % 2][:, (h // 2) * 128:(h // 2 + 1) * 128], keT[hp:hp + 64, h // 2, c0:c0 + 128],
                               qeT[hp:hp + 64, h // 2, c0:c0 + 128], keT.r + qeT.r, pA2[h % 2].r)
                        for h in range(4):
                            kb.op(dve, lambda e: e.tensor_tensor(out=attm[:, h, :],
                                                                 in0=pA2[h % 2][:, (h // 2) * 128:(h // 2 + 1) * 128],
                                                                 in1=tribd[:], op=ALU.mult),
                                  reads=pA2[h % 2].r + tribd.r, writes=attm.r)
                        for cc in range(2):
                            kb.op(dve, lambda e: e.tensor_copy(out=Sbf[cc][:], in_=St[:]), reads=St.r, writes=Sbf[cc].r)
                            kb.op(dve, lambda e: e.tensor_scalar_mul(out=kdm[:, cc, :], in0=kdtok[:, blk, :],
                                                                     scalar1=cmask[:, cc:cc + 1]),
                                  reads=[kdtok.r[blk]] + cmask.r, writes=kdm.r)
                            pK = gp()
                            for h in range(4):
                                hp = (h % 2) * 64
                                MM(pK[hp:hp + 64, (h // 2) * 128:(h // 2 + 1) * 128],
                                   kdm[:, cc, h * 64:(h + 1) * 64], vbtok[:, blk, h * 128:(h + 1) * 128],
                                   kdm.r + [vbtok.r[blk]], pK.r, inc=(h == 3))
                            col = c0 + cc * 64 + 63
                            for i in range(2):
                                kb.op(dve, lambda e: e.scalar_tensor_tensor(
                                    out=St[:, i, :], in0=St[:, i, :], scalar=EbT[:, i, col:col + 1],
                                    in1=pK[:, i * 128:(i + 1) * 128], op0=ALU.mult, op1=ALU.add),
                                    reads=St.r + EbT.r + pK.r, writes=St.r)
                        import os as _os
                        if _os.environ.get("GSKIP") == "1":
                            continue
                        pO = gp()
                        for h in range(4):
                            hp = (h % 2) * 64
                            for cc in range(2):
                                oc = h * 128 + cc * 64
                                MM(pO[:, oc:oc + 64], vbtok[:, blk, h * 128:(h + 1) * 128],
                                   attm[:, h, cc * 64:cc * 64 + 64], [vbtok.r[blk]] + attm.r, pO.r,
                                   start=True, stop=False, inc=False)
                                MM(pO[:, oc:oc + 64], Sbf[cc][hp:hp + 64, h // 2, :],
                                   qeT[hp:hp + 64, h // 2, c0 + cc * 64:c0 + cc * 64 + 64], Sbf[cc].r + qeT.r, pO.r,
                                   start=False, stop=True, inc=(h == 3 and cc == 1))
                        if _os.environ.get("GSKIP") == "2":
                            continue
                        ACT(sqb[:], pO[:], AF.Square, pO.r, sqb.r)
                        pN = gp()
                        MM(pN[:], ones_bf[:], sqb[:], ones_bf.r + sqb.r, pN.r)
                        ACT(rsb[:], pN[:], AF.Sqrt, pN.r + epsc.r, rsb.r, bias=epsc[:], scale=1.0 / 128)
                        kb.op(dve, lambda e: e.reciprocal(out=rsb[:], in_=rsb[:]), reads=rsb.r, writes=rsb.r)
                        kb.op(dve, lambda e: e.tensor_tensor(out=t1[:], in0=pO[:], in1=rsb[:], op=ALU.mult),
                              reads=pO.r + rsb.r, writes=t1.r)
                        kb.op(dve, lambda e: e.tensor_tensor(out=mixT[:, 2:6, c0:c0 + 128],
                                                              in0=t1[:].rearrange("p (h q) -> p h q", h=4),
                                                              in1=SrgT[:, :, c0:c0 + 128], op=ALU.mult),
                              reads=t1.r + SrgT.r, writes=mixT.r)

                    for blk in range(4 if "swa" in parts else 0):
                        jb = t * 4 + blk
                        c0 = blk * 128
                        pO, pD = PS[6], PS[7]
                        kbs = [jb - 1, jb] if jb > 0 else [jb]
                        for n_i, kbk in enumerate(kbs):
                            sl = kbk % 8
                            which = 1 if kbk == jb else 0
                            pL = PS[4 + (n_i % 2)]
                            for kv in range(2):
                                MM(pL[:, kv * 256:(kv + 1) * 256], kcT[kv * 64:kv * 64 + 64, sl, :],
                                   qcT[kv * 64:kv * 64 + 64, :, c0:c0 + 128], [kcT.r[sl]] + qcT.r, pL.r,
                                   start=True, stop=False, inc=False)
                                MM(pL[:, kv * 256:(kv + 1) * 256], ident[:],
                                   biasC[which][:, kv * 2:kv * 2 + 2, :].rearrange("p h q -> p (h q)"),
                                   ident.r + biasC[which].r, pL.r, start=False, stop=True, inc=(kv == 1))
                            P = PT[n_i]
                            ACT(P[:], pL[:], AF.Exp, pL.r, P.r)
                            first, lastk = (n_i == 0), (n_i == len(kbs) - 1)
                            MM(pD[:], ones_bf[:], P[:], ones_bf.r + P.r, pD.r, start=first, stop=lastk)
                        for kv in range(2):
                            for g in range(2):
                                hd = kv * 2 + g
                                for n_i, kbk in enumerate(kbs):
                                    sl = kbk % 8
                                    P = PT[n_i]
                                    MM(pO[g * 64:g * 64 + 64, kv * 128:(kv + 1) * 128], vctok[:, sl, kv * 64:kv * 64 + 64],
                                       P[:, hd * 128:(hd + 1) * 128], [vctok.r[sl]] + P.r, pO.r,
                                       start=(n_i == 0), stop=(n_i == len(kbs) - 1))
                        kb.op(dve, lambda e: e.tensor_tensor(out=rsb[:], in0=pD[:],
                                                             in1=sinkexp[:].rearrange("p h q -> p (h q)"), op=ALU.add),
                              reads=pD.r + sinkexp.r, writes=rsb.r)
                        kb.op(dve, lambda e: e.reciprocal(out=rsb[:], in_=rsb[:]), reads=rsb.r, writes=rsb.r)
                        for kv in range(2):
                            for g in range(2):
                                hd = kv * 2 + g
                                kb.op(dve, lambda e: e.tensor_tensor(
                                    out=mixT[g * 64:g * 64 + 64, 6 + kv, c0:c0 + 128],
                                    in0=pO[g * 64:g * 64 + 64, kv * 128:(kv + 1) * 128],
                                    in1=rsb[g * 64:g * 64 + 64, hd * 128:(hd + 1) * 128], op=ALU.mult),
                                    reads=pO.r + rsb.r, writes=mixT.r)

                    def dsa_A(blk):
                        jb = t * 4 + blk
                        c0 = blk * 128
                        N = (jb + 1) * 128
                        negm = negmB[blk % 2]
                        for kt in range((N + 511) // 512):
                            k0 = kt * 512
                            n = min(512, N - k0)
                            kregs = kiT.r[k0 // 128:(k0 + n) // 128]
                            for h in range(4):
                                hp = (h % 2) * 64
                                p = gp()
                                MM(p[:, 0:n], qiT[hp:hp + 64, h // 2, c0:c0 + 128], kiT[hp:hp + 64, k0:k0 + n],
                                   qiT.r + kregs, p.r)
                                if h == 0:
                                    kb.op(dve, lambda e: e.tensor_scalar(
                                        out=score[:, k0:k0 + n], in0=p[:, 0:n], scalar1=0.0,
                                        scalar2=widx[:, blk, 0:1], op0=ALU.max, op1=ALU.mult),
                                        reads=p.r + [widx.r[blk]], writes=score.r)
                                else:
                                    tr = tmpR[h % 2]
                                    ACT(tr[:, 0:n], p[:, 0:n], AF.Relu, p.r, tr.r)
                                    kb.op(dve, lambda e: e.scalar_tensor_tensor(
                                        out=score[:, k0:k0 + n], in0=tr[:, 0:n], scalar=widx[:, blk, h:h + 1],
                                        in1=score[:, k0:k0 + n], op0=ALU.mult, op1=ALU.add),
                                        reads=tr.r + [widx.r[blk]] + score.r, writes=score.r)
                            kb.op(dve, lambda e: e.scalar_tensor_tensor(
                                out=score[:, k0:k0 + n], in0=ramp512[:, 0:n], scalar=-TIE * k0,
                                in1=score[:, k0:k0 + n], op0=ALU.add, op1=ALU.add),
                                reads=ramp512.r + score.r, writes=score.r)
                        if N > 256:
                            kb.op(dve, lambda e: e.max(out=m8[:], in_=score[:, 0:N]), reads=score.r, writes=m8.r)
                            kb.op(dve, lambda e: e.tensor_reduce(out=bs_lo[:], in_=score[:, 0:N], axis=AX.X, op=ALU.min),
                                  reads=score.r, writes=bs_lo.r)
                            kb.op(dve, lambda e: e.tensor_scalar_add(out=bs_lo[:], in0=bs_lo[:], scalar1=-1.0),
                                  reads=bs_lo.r, writes=bs_lo.r)
                            kb.op(dve, lambda e: e.tensor_tensor(out=bs_R[:], in0=m8[:, 0:1], in1=bs_lo[:], op=ALU.subtract),
                                  reads=m8.r + bs_lo.r, writes=bs_R.r)
                            kb.op(dve, lambda e: e.tensor_scalar_mul(out=bs_hh[:], in0=pw[:], scalar1=bs_R[:, 0:1]),
                                  reads=pw.r + bs_R.r, writes=bs_hh.r)
                            kb.op(dve, lambda e: e.tensor_tensor(out=bs_mid[:], in0=bs_lo[:], in1=bs_hh[:, 0:1], op=ALU.add),
                                  reads=bs_lo.r + bs_hh.r, writes=bs_mid.r)
                        kb.op(pool, lambda e: e.memset(score[0:64, N - 64:N], -3e38), reads=score.r, writes=score.r)
                        if N > 256:
                            for k in range(KBIS):
                                kb.op(dve, lambda e: e.tensor_scalar(out=negm[:, 0:N], in0=score[:, 0:N],
                                                                     scalar1=bs_mid[:, 0:1], scalar2=None, op0=ALU.is_gt,
                                                                     op1=ALU.add, accum_out=bs_cnt[:]),
                                      reads=score.r + bs_mid.r, writes=negm.r + bs_cnt.r)
                                kb.op(dve, lambda e: e.tensor_scalar(out=bs_fh[:], in0=bs_cnt[:], scalar1=c256[:, 0:1],
                                                                     scalar2=bs_hh[:, k:k + 1], op0=ALU.is_ge, op1=ALU.mult),
                                      reads=bs_cnt.r + c256.r + bs_hh.r, writes=bs_fh.r)
                                kb.op(dve, lambda e: e.scalar_tensor_tensor(out=bs_mid[:], in0=bs_mid[:],
                                                                            scalar=bs_hh[:, k + 1:k + 2], in1=bs_fh[:],
                                                                            op0=ALU.subtract, op1=ALU.add),
                                      reads=bs_mid.r + bs_hh.r + bs_fh.r, writes=bs_mid.r)
                            kb.op(dve, lambda e: e.tensor_tensor(out=bs_lo[:], in0=bs_mid[:], in1=bs_hh[:, KBIS:KBIS + 1],
                                                                 op=ALU.subtract),
                                  reads=bs_mid.r + bs_hh.r, writes=bs_lo.r)
                            kb.op(dve, lambda e: e.tensor_scalar(out=negm[:, 0:N], in0=score[:, 0:N], scalar1=bs_lo[:, 0:1],
                                                                 scalar2=NEG, op0=ALU.is_le, op1=ALU.mult),
                                  reads=score.r + bs_lo.r, writes=negm.r)
                        else:
                            kb.op(dve, lambda e: e.tensor_scalar(out=negm[:, 0:N], in0=score[:, 0:N], scalar1=-1e29,
                                                                 scalar2=NEG, op0=ALU.is_lt, op1=ALU.mult),
                                  reads=score.r, writes=negm.r)

                    def dsa_B(blk):
                        jb = t * 4 + blk
                        c0 = blk * 128
                        negm = negmB[blk % 2]
                        pO, pD = PS[6], PS[7]
                        for kbk in range(jb + 1):
                            pL = PS[4 + (kbk % 2)]
                            near = kbk >= jb - 1
                            MM(pL[:], cT[:, kbk * 128:(kbk + 1) * 128], qlT[:, :, c0:c0 + 128], [cT.r[kbk]] + qlT.r, pL.r,
                               start=True, stop=False, inc=False)
                            MM(pL[:], negm[:, kbk * 128:(kbk + 1) * 128], ident4[:].rearrange("p h q -> p (h q)"),
                               negm.r + ident4.r, pL.r, start=False, stop=not near, inc=not near)
                            if near:
                                which = 1 if kbk == jb else 0
                                MM(pL[:], ident[:], biasA[which][:].rearrange("p h q -> p (h q)"),
                                   ident.r + biasA[which].r, pL.r, start=False, stop=True)
                            P = nextPT()
                            ACT(P[:], pL[:], AF.Exp, pL.r, P.r)
                            MM(pO[:], ctok[:, kbk, :], P[:], [ctok.r[kbk]] + P.r, pO.r, start=(kbk == 0), stop=(kbk == jb),
                               inc=False)
                            MM(pD[:], ones_bf[:], P[:], ones_bf.r + P.r, pD.r + pO.r, start=(kbk == 0), stop=(kbk == jb))
                        kb.op(dve, lambda e: e.reciprocal(out=rsbB[:], in_=pD[:]), reads=pD.r, writes=rsbB.r)
                        kb.op(dve, lambda e: e.tensor_tensor(out=olT[:].rearrange("p h q -> p (h q)"), in0=pO[:],
                                                             in1=rsbB[:], op=ALU.mult),
                              reads=pO.r + rsbB.r, writes=olT.r)
                        pF = gp()
                        for h in range(4):
                            hp = (h % 2) * 64
                            MM(pF[hp:hp + 64, (h // 2) * 128:(h // 2 + 1) * 128], wuv[:, h * 64:(h + 1) * 64], olT[:, h, :],
                               wuv.r + olT.r, pF.r, inc=(h == 3))
                        ACT(mixT[:, 0:2, c0:c0 + 128], pF[:, 0:256].rearrange("p (i q) -> p i q", i=2), AF.Copy, pF.r,
                            mixT.r)

                    if "dsa" in parts:
                        dsa_A(0)
                        for blk in range(4):
                            if blk + 1 < 4:
                                dsa_A(blk + 1)
                            dsa_B(blk)

                    kb.dma(sp, Xt[:], src[gt * TT:(gt + 1) * TT, :].rearrange("(s p) d -> p s d", p=128),
                           reads=[src_regs[gt]], writes=Xt.r, semreg=Xt.r[0])
                    for s in range(4):
                        for hh in range(2):
                            po = gp()
                            for c in range(8):
                                MM(po[:], mixT[:, c, s * 128:(s + 1) * 128], Wo[:, c, hh * 512:(hh + 1) * 512],
                                   mixT.r + Wo.r, po.r, start=(c == 0), stop=(c == 7), inc=(c == 7))
                            kb.op(dve, lambda e: e.tensor_tensor(
                                out=Xt[:, s, hh * 512:(hh + 1) * 512], in0=Xt[:, s, hh * 512:(hh + 1) * 512],
                                in1=po[:], op=ALU.add), reads=Xt.r + po.r, writes=Xt.r)
                    kb.dma(sp, xs[gt * TT:(gt + 1) * TT, :].rearrange("(s p) d -> p s d", p=128), Xt[:],
                           reads=Xt.r, writes=[xs.r[gt]], semreg=Xt.r[0])

        cur, cur_regs = x_in, xin_regs
        for l in range(DEPTH):
            if do_mixer:
                with ExitStack() as pes:
                    mixer_phase(l, cur, cur_regs, pes)
                    kb.barrier()
                cur, cur_regs = xs.t, xs.r
            if do_ffn:
                with ExitStack() as pes:
                    ffn_phase(l, cur, cur_regs, last=(l == DEPTH - 1), pes=pes)
                    kb.barrier()
                cur, cur_regs = xs.t, xs.r

        for r in yout_regs:
            sp.wait(r.last_w)
        print(f"[build] instructions={kb.ninst} sems={kb.nsem}", flush=True)
    return nc


_W_NAMES = ["w_in", "w_out", "norm_mix", "norm_ffn", "kv_norm", "w_uk", "w_uv", "w_gate2", "b_gate", "gla_norm",
            "sinks", "w_ffn_gate", "w_ffn_up", "w_ffn_down", "final_norm"]


def make_in_maps(inputs, n_cores, nseq, S):
    x = np.ascontiguousarray(inputs["x"], dtype=np.float32)
    L = inputs["w_in"].shape[0]
    shared = {}
    for k in _W_NAMES:
        a = np.ascontiguousarray(inputs[k], dtype=np.float32)
        if k == "final_norm":
            a = a.reshape(1, D)
        if k in ("w_uk", "w_uv"):
            a = a.reshape(L, 128, 256)
        shared[k] = a
    cA, cC, mC, bfar = _host_consts(inputs["rel_bias"])
    shared.update(cA=cA, cC=cC, mC=mC, bfar=bfar)
    in_maps = []
    for c in range(n_cores):
        m = dict(shared)
        m["x"] = x[c * nseq:(c + 1) * nseq].reshape(nseq * S, D)
        in_maps.append(m)
    return in_maps


def kernel(**inputs):
    n = 8
    B, S, _ = inputs["x"].shape
    nseq = B // n
    nc = build(S=S, NSEQ=nseq, DEPTH=inputs["w_in"].shape[0])
    in_maps = make_in_maps(inputs, n, nseq, S)
    res = run_bass_kernel_spmd(nc, in_maps, core_ids=list(range(n)))
    out = np.concatenate([r["y"].reshape(nseq, S, D) for r in res.results], axis=0)
    return out.astype(np.float32)
```

```python
import numpy as np
from contextlib import ExitStack
import concourse.bass as bass
import concourse.mybir as mybir
from concourse.bass_utils import run_bass_kernel_spmd

F32 = mybir.dt.float32
BF16 = mybir.dt.bfloat16
ALU = mybir.AluOpType
AF = mybir.ActivationFunctionType
AX = mybir.AxisListType

D = 1024
DFF = 2816
NFF = DFF // 128
EPS = 1e-6
NEG = -30000.0


class Ev:
    __slots__ = ("sem", "val")

    def __init__(self, sem, val):
        self.sem = sem
        self.val = val


class Region:
    __slots__ = ("last_w", "readers", "dsem", "dcnt", "name", "excl")

    def __init__(self, name, excl=False):
        self.name = name
        self.excl = excl
        self.last_w = None
        self.readers = {}
        self.dsem = None
        self.dcnt = 0


class Tile:
    def __init__(self, t, regs):
        self.t = t
        self.r = regs

    def __getitem__(self, idx):
        return self.t[idx]


class Q:
    def __init__(self, kb, eng, name, is_pe=False):
        self.eng = eng
        self.name = name
        self.is_pe = is_pe
        self.sem = kb.newsem("q_" + name)
        self.cnt = 0
        self.known = {}

    def wait(self, ev):
        if ev is None:
            return
        if ev.sem is self.sem and self.is_pe:
            return
        k = id(ev.sem)
        if self.known.get(k, 0) >= ev.val:
            return
        self.eng.wait_ge(ev.sem, ev.val)
        self.known[k] = ev.val


class KB:
    def __init__(self, nc, es):
        self.nc = nc
        self.es = es
        self.nsem = 0
        self.pe = Q(self, nc.tensor, "pe", is_pe=True)
        self.act = Q(self, nc.scalar, "act")
        self.dve = Q(self, nc.vector, "dve")
        self.pool = Q(self, nc.gpsimd, "pool")
        self.sp = Q(self, nc.sync, "sp")
        self.ninst = 0
        self.dregs = []
        self.uid = 0

    def newsem(self, name):
        self.nsem += 1
        return self.es.enter_context(self.nc.semaphore(name))

    def sb(self, name, shape, dt, nreg=1, es=None):
        self.uid += 1
        name = f"{name}_{self.uid}"
        t = (es or self.es).enter_context(self.nc.sbuf_tensor(name, list(shape), dt))
        return Tile(t, [Region(f"{name}.{i}") for i in range(nreg)])

    def barrier(self):
        qs = [self.pe, self.act, self.dve, self.pool, self.sp]
        for q in qs:
            for q2 in qs:
                if q2 is not q and q2.cnt > 0:
                    q.wait(Ev(q2.sem, q2.cnt))
            for r in self.dregs:
                q.wait(Ev(r.dsem, r.dcnt))

    def ps(self, name, shape=(128, 512), dt=F32):
        t = self.es.enter_context(self.nc.psum_tensor(name, list(shape), dt))
        return Tile(t, [Region(name, excl=True)])

    def dram(self, name, shape, dt, nreg=1, kind="Internal"):
        t = self.nc.dram_tensor(name, list(shape), dt, kind=kind)
        return Tile(t.ap(), [Region(f"{name}.{i}") for i in range(nreg)])

    def _deps(self, q, reads, writes):
        for r in reads:
            q.wait(r.last_w)
            if r.excl:
                for ev in r.readers.values():
                    if ev.sem is not q.sem:
                        q.wait(ev)
        for w in writes:
            q.wait(w.last_w)
            for ev in w.readers.values():
                q.wait(ev)

    def _record(self, ev, reads, writes):
        for r in reads:
            r.readers[id(ev.sem)] = ev
        for w in writes:
            w.last_w = ev
            w.readers = {}

    def op(self, q, fn, reads=(), writes=(), inc=True):
        self._deps(q, reads, writes)
        inst = fn(q.eng)
        self.ninst += 1
        ev = Ev(q.sem, q.cnt + 1)
        if inc:
            inst.then_inc(q.sem, 1)
            q.cnt += 1
        self._record(ev, reads, writes)
        return ev

    def dma(self, q, out, in_, reads, writes, semreg, **kw):
        self._deps(q, reads, writes)
        if semreg.dsem is None:
            semreg.dsem = self.newsem("d_" + semreg.name.replace(".", "_"))
            self.dregs.append(semreg)
        inst = q.eng.dma_start(out=out, in_=in_, **kw)
        semreg.dcnt += 16
        inst.then_inc(semreg.dsem, 16)
        self.ninst += 1
        ev = Ev(semreg.dsem, semreg.dcnt)
        self._record(ev, reads, writes)
        return ev


def _bucket(rel):
    rel = np.asarray(rel, dtype=np.int64)
    n = np.abs(rel)
    side = np.where(rel > 0, 16, 0)
    nf = np.maximum(n, 1).astype(np.float32)
    large = 8 + (np.log(nf / np.float32(8)) / np.float32(np.log(16.0)) * np.float32(8)).astype(np.int32)
    large = np.minimum(large, 15)
    return (side + np.where(n < 8, n, large)).astype(np.int64)


def _host_consts(rel_bias):
    rel_bias = np.asarray(rel_bias, dtype=np.float32)
    s = np.arange(128)[:, None]
    q = np.arange(128)[None, :]
    ip = _bucket(s - 128 - q)
    idg = _bucket(s - q)
    g = np.stack([rel_bias[ip], rel_bias[idg]])
    cA = np.ascontiguousarray(g[..., 0:4].transpose(0, 1, 3, 2)).reshape(2, 128, 512)
    cC = np.ascontiguousarray(g[..., 4:8].transpose(0, 1, 3, 2)).reshape(2, 128, 512)
    bfar = np.ascontiguousarray(rel_bias[15:16, 0:4])
    mp = np.where((q >= 64) & (s < 64), NEG, 0.0).astype(np.float32)
    md = np.where((q < 64) & (s >= 64), NEG, 0.0).astype(np.float32)
    mC = np.stack([np.broadcast_to(mp[:, None, :], (128, 4, 128)),
                   np.broadcast_to(md[:, None, :], (128, 4, 128))]).reshape(2, 128, 512)
    return cA, cC, np.ascontiguousarray(mC, dtype=np.float32), bfar


_FCH = [
    ("qa0", [(0, 128)]), ("qa1", [(128, 128)]),
    ("qi0", [(384, 128)]), ("qi1", [(512, 128)]),
    ("ki", [(640, 64), (640, 64)]),
    ("glr", [(1732, 16)]),
    ("qb0", [(708, 128)]), ("qb1", [(836, 128)]),
    ("kb0", [(964, 128)]), ("kb1", [(1092, 128)]),
    ("rb0", [(1748, 128)]), ("rb1", [(1876, 128)]), ("rb2", [(2004, 128)]), ("rb3", [(2132, 128)]),
    ("qc0", [(2260, 64), (2388, 64)]), ("qc1", [(2324, 64), (2452, 64)]),
    ("kc", [(2516, 128)]),
]
_TCH = [("A", [(256, 128), (704, 4)]), ("B", [(964, 256), (2644, 128)]), ("C", [(1220, 512)])]


def build(S=4096, NSEQ=2, DEPTH=2, do_mixer=True, do_ffn=True, xbuf=1, parts=("gla", "swa", "dsa"), stage=99):
    nc = bass.Bass("TRN2", target_bir_lowering=False)
    NT = NSEQ * S
    TT = 512
    NTILE = NT // TT
    TPS = S // TT
    NB = S // 128

    def din(name, shape):
        return nc.dram_tensor(name, list(shape), F32, kind="ExternalInput").ap()

    x_in = din("x", [NT, D])
    w_in = din("w_in", [DEPTH, D, 2772])
    w_out = din("w_out", [DEPTH, D, D])
    n_mix = din("norm_mix", [DEPTH, D])
    n_ffn = din("norm_ffn", [DEPTH, D])
    kv_norm = din("kv_norm", [DEPTH, 128])
    w_uk = din("w_uk", [DEPTH, 128, 256])
    w_uv = din("w_uv", [DEPTH, 128, 256])
    w_g2 = din("w_gate2", [DEPTH, 16, 256])
    b_g = din("b_gate", [DEPTH, 256])
    gla_n = din("gla_norm", [DEPTH, 512])
    sinks = din("sinks", [DEPTH, 4])
    w_g = din("w_ffn_gate", [DEPTH, D, DFF])
    w_u = din("w_ffn_up", [DEPTH, D, DFF])
    w_d = din("w_ffn_down", [DEPTH, DFF, D])
    n_fin = din("final_norm", [1, D])
    cA_d = din("cA", [2, 128, 512])
    cC_d = din("cC", [2, 128, 512])
    mC_d = din("mC", [2, 128, 512])
    bfar_d = din("bfar", [1, 4])
    y_out = nc.dram_tensor("y", [NT, D], F32, kind="ExternalOutput").ap()

    with ExitStack() as es:
        kb = KB(nc, es)
        pe, act, dve, pool, sp = kb.pe, kb.act, kb.dve, kb.pool, kb.sp

        xs = kb.dram("xs", [NT, D], F32, nreg=NTILE)
        xin_regs = [Region(f"xin.{i}") for i in range(NTILE)]
        yout_regs = [Region(f"yout.{i}") for i in range(NTILE)]

        def MM(out, lhsT, rhs, reads, wr, start=True, stop=True, inc=True):
            return kb.op(pe, lambda e: e.matmul(out, lhsT=lhsT, rhs=rhs, start=start, stop=stop),
                         reads=reads, writes=wr, inc=inc)

        def ACT(out, in_, func, reads, wr, **kw):
            return kb.op(act, lambda e: e.activation(out=out, in_=in_, func=func, **kw), reads=reads, writes=wr)

        identf = kb.sb("identf", [128, 128], F32)
        ident = kb.sb("ident", [128, 128], BF16)
        ident4 = kb.sb("ident4", [128, 4, 128], BF16)
        ones_bf = kb.sb("ones_bf", [128, 128], BF16)
        tribd = kb.sb("tribd", [128, 128], F32)
        ones_f = kb.sb("ones_f", [128, 128], F32)
        ramp512 = kb.sb("ramp512", [128, 512], F32)
        cmask = kb.sb("cmask", [128, 2], F32)
        subd = kb.sb("subd", [128, 128], F32)
        epsc = kb.sb("epsc", [128, 1], F32)
        onec = kb.sb("onec", [128, 1], F32)
        thr_all = kb.sb("thr_all", [128, 1], F32)
        kb.op(pool, lambda e: e.memset(identf[:], 0.0), writes=identf.r)
        kb.op(pool, lambda e: e.affine_select(out=identf[:], in_=identf[:], pattern=[[-1, 128]],
                                              compare_op=ALU.not_equal, fill=1.0, base=0, channel_multiplier=1),
              reads=identf.r, writes=identf.r)
        kb.op(dve, lambda e: e.tensor_copy(out=ident[:], in_=identf[:]), reads=identf.r, writes=ident.r)
        for h in range(4):
            kb.op(dve, lambda e: e.tensor_copy(out=ident4[:, h, :], in_=identf[:]), reads=identf.r, writes=ident4.r)
        kb.op(pool, lambda e: e.memset(ones_bf[:], 1.0), writes=ones_bf.r)
        kb.op(pool, lambda e: e.memset(epsc[:], EPS), writes=epsc.r)
        kb.op(pool, lambda e: e.memset(onec[:], 1.0), writes=onec.r)
        kb.op(pool, lambda e: e.memset(thr_all[:], -1e29), writes=thr_all.r)
        kb.op(pool, lambda e: e.memset(tribd[:], 1.0), writes=tribd.r)
        kb.op(pool, lambda e: e.affine_select(out=tribd[:], in_=tribd[:], pattern=[[1, 128]],
                                              compare_op=ALU.is_ge, fill=0.0, base=0, channel_multiplier=-1),
              reads=tribd.r, writes=tribd.r)
        kb.op(pool, lambda e: e.memset(tribd[0:64, 64:128], 0.0), reads=tribd.r, writes=tribd.r)
        kb.op(pool, lambda e: e.memset(cmask[:], 0.0), writes=cmask.r)
        kb.op(pool, lambda e: e.memset(cmask[0:64, 0:1], 1.0), reads=cmask.r, writes=cmask.r)
        kb.op(pool, lambda e: e.memset(cmask[64:128, 1:2], 1.0), reads=cmask.r, writes=cmask.r)
        kb.op(pool, lambda e: e.memset(subd[:], 1.0), writes=subd.r)
        kb.op(pool, lambda e: e.affine_select(out=subd[:], in_=subd[:], pattern=[[-1, 128]],
                                              compare_op=ALU.is_gt, fill=0.0, base=0, channel_multiplier=1),
              reads=subd.r, writes=subd.r)
        kb.op(pool, lambda e: e.memset(subd[64:128, 0:64], 0.0), reads=subd.r, writes=subd.r)

        TIE = 1e-9
        KBIS = 24
        pw = kb.sb("pw", [128, KBIS + 1], F32)
        for k in range(KBIS + 1):
            kb.op(pool, lambda e: e.memset(pw[:, k:k + 1], 2.0 ** -(k + 1)), reads=pw.r, writes=pw.r)
        c256 = kb.sb("c256", [128, 1], F32)
        kb.op(pool, lambda e: e.memset(c256[:], 256.0), writes=c256.r)
        kb.op(pool, lambda e: e.memset(ones_f[:], 1.0), writes=ones_f.r)
        gfin = kb.sb("gfin", [128, D], F32)
        kb.dma(sp, gfin[:], n_fin[0].partition_broadcast(128), reads=(), writes=gfin.r, semreg=gfin.r[0])
        gffn = kb.sb("gffn", [128, DEPTH * 8], F32)
        gmix = kb.sb("gmix", [128, DEPTH * 8], F32)
        with nc.allow_non_contiguous_dma(reason="tiny gain vectors"):
            kb.dma(sp, gffn[:], n_ffn.rearrange("l (k p) -> p (l k)", p=128), reads=(), writes=gffn.r,
                   semreg=gffn.r[0])
            kb.dma(sp, gmix[:], n_mix.rearrange("l (k p) -> p (l k)", p=128), reads=(), writes=gmix.r,
                   semreg=gmix.r[0])

        X = [kb.sb(f"X{i}", [128, 4, D], F32) for i in range(xbuf)]
        ss = kb.sb("ss", [128, 4], F32)
        rstd = kb.sb("rstd", [128, 4], F32)
        hT = kb.sb("hT", [128, 8, TT], BF16)
        PS = [kb.ps(f"ps{i}") for i in range(8)]
        gstate = {"i": 0}

        def gp():
            gstate["i"] += 1
            return PS[gstate["i"] % 4]

        pr = PS[0]
        MM(pr[:, 0:128], ones_f[:], tribd[:], ones_f.r + tribd.r, pr.r)
        for c8 in range(8):
            kb.op(dve, lambda e: e.tensor_scalar(out=ramp512[:, c8 * 64:(c8 + 1) * 64],
                                                 in0=pr[:, 0:64], scalar1=-TIE,
                                                 scalar2=-TIE * 64 * c8, op0=ALU.mult, op1=ALU.add),
                  reads=pr.r, writes=ramp512.r)

        def rstd_from_ss():
            ACT(rstd[:], ss[:], AF.Sqrt, ss.r + epsc.r, rstd.r, bias=epsc[:], scale=1.0 / D)
            kb.op(dve, lambda e: e.reciprocal(out=rstd[:], in_=rstd[:]), reads=rstd.r, writes=rstd.r)

        def rmsnorm_T(Xt, gain_cols, xn, junk):
            for s in range(4):
                ACT(junk[:], Xt[:, s, :], AF.Square, Xt.r, junk.r + ss.r, accum_out=ss[:, s:s + 1])
            rstd_from_ss()
            for s in range(4):
                ACT(xn[:, s, :], Xt[:, s, :], AF.Copy, Xt.r + rstd.r, xn.r, scale=rstd[:, s:s + 1])
            for c in range(8):
                p = gp()
                for s in range(4):
                    MM(p[:, s * 128:(s + 1) * 128], xn[:, s, c * 128:(c + 1) * 128], ident[:],
                       xn.r + ident.r, p.r, inc=(s == 3))
                kb.op(dve, lambda e: e.tensor_scalar_mul(out=hT[:, c, :], in0=p[:], scalar1=gain_cols[c]),
                      reads=p.r, writes=hT.r)

        def ffn_phase(l, src, src_regs, last, pes):
            Wg = kb.sb("Wg", [128, 8, DFF], BF16, es=pes)
            Wu = kb.sb("Wu", [128, 8, DFF], BF16, es=pes)
            Wd = kb.sb("Wd", [128, NFF, D], BF16, es=pes)
            aT = kb.sb("aT", [128, NFF, TT], BF16, es=pes)
            xn = Tile(aT[:, 0:8, :].rearrange("p (s a) t -> p s (a t)", s=4), aT.r)
            sg = [kb.sb(f"sg{i}", [128, TT], BF16, es=pes) for i in range(2)]
            junk = Tile(hT[:, 0:2, :].rearrange("p a t -> p (a t)"), hT.r)
            Xf = [X[0], kb.sb("X2", [128, 4, D], F32, es=pes)]
            HC = DFF // 2
            for hh in range(2):
                kb.dma(pool, Wg[:, :, hh * HC:(hh + 1) * HC],
                       w_g[l].rearrange("(k p) n -> p k n", p=128)[:, :, hh * HC:(hh + 1) * HC],
                       reads=(), writes=Wg.r, semreg=Wg.r[0])
                kb.dma(pool, Wu[:, :, hh * HC:(hh + 1) * HC],
                       w_u[l].rearrange("(k p) n -> p k n", p=128)[:, :, hh * HC:(hh + 1) * HC],
                       reads=(), writes=Wu.r, semreg=Wu.r[0])
            for hh in range(2):
                kb.dma(pool, Wd[:, hh * 11:(hh + 1) * 11, :],
                       w_d[l].rearrange("(k p) n -> p k n", p=128)[:, hh * 11:(hh + 1) * 11, :],
                       reads=(), writes=Wd.r, semreg=Wd.r[0])
            gcols = [gffn[:, l * 8 + c:l * 8 + c + 1] for c in range(8)]
            for t in range(NTILE):
                Xt = Xf[t % 2]
                kb.dma(sp, Xt[:], src[t * TT:(t + 1) * TT, :].rearrange("(s p) d -> p s d", p=128),
                       reads=[src_regs[t]], writes=Xt.r, semreg=Xt.r[0])
                rmsnorm_T(Xt, gcols, xn, junk)
                for f in range(NFF):
                    pg = PS[4 + (f % 2)]
                    pu = PS[6 + (f % 2)]
                    for k in range(8):
                        MM(pg[:], Wg[:, k, f * 128:(f + 1) * 128], hT[:, k, :], Wg.r + hT.r, pg.r,
                           start=(k == 0), stop=(k == 7), inc=(k == 7))
                    for k in range(8):
                        MM(pu[:], Wu[:, k, f * 128:(f + 1) * 128], hT[:, k, :], Wu.r + hT.r, pu.r,
                           start=(k == 0), stop=(k == 7), inc=(k == 7))
                    sgt = sg[f % 2]
                    ACT(sgt[:], pg[:], AF.Silu, pg.r, sgt.r)
                    kb.op(dve, lambda e: e.tensor_tensor(out=aT[:, f, :], in0=sgt[:], in1=pu[:], op=ALU.mult),
                          reads=sgt.r + pu.r, writes=aT.r)
                for s in range(4):
                    for hh in range(2):
                        po = gp()
                        for f in range(NFF):
                            MM(po[:], aT[:, f, s * 128:(s + 1) * 128], Wd[:, f, hh * 512:(hh + 1) * 512],
                               aT.r + Wd.r, po.r, start=(f == 0), stop=(f == NFF - 1), inc=(f == NFF - 1))
                        kb.op(dve, lambda e: e.tensor_tensor(
                            out=Xt[:, s, hh * 512:(hh + 1) * 512], in0=Xt[:, s, hh * 512:(hh + 1) * 512],
                            in1=po[:], op=ALU.add), reads=Xt.r + po.r, writes=Xt.r)
                if last:
                    for s in range(4):
                        ACT(junk[:], Xt[:, s, :], AF.Square, Xt.r, junk.r + ss.r, accum_out=ss[:, s:s + 1])
                    rstd_from_ss()
                    for s in range(4):
                        kb.op(dve, lambda e: e.scalar_tensor_tensor(
                            out=Xt[:, s, :], in0=Xt[:, s, :], scalar=rstd[:, s:s + 1], in1=gfin[:],
                            op0=ALU.mult, op1=ALU.mult), reads=Xt.r + rstd.r + gfin.r, writes=Xt.r)
                    kb.dma(sp, y_out[t * TT:(t + 1) * TT, :].rearrange("(s p) d -> p s d", p=128), Xt[:],
                           reads=Xt.r, writes=[yout_regs[t]], semreg=Xt.r[0])
                else:
                    kb.dma(sp, xs[t * TT:(t + 1) * TT, :].rearrange("(s p) d -> p s d", p=128), Xt[:],
                           reads=Xt.r, writes=[xs.r[t]], semreg=Xt.r[0])

        def mixer_phase(l, src, src_regs, pes):
            def sb(name, shape, dt, nreg=1):
                return kb.sb(name, shape, dt, nreg=nreg, es=pes)

            foff, fw, off = {}, {}, 0
            for name, pieces in _FCH:
                foff[name] = off
                fw[name] = sum(n for _, n in pieces)
                off += fw[name]
            NFC = off
            toff, tw, off = {}, {}, 0
            for name, pieces in _TCH:
                toff[name] = off
                tw[name] = sum(n for _, n in pieces)
                off += tw[name]
            NTC = off
            Wf = sb("Wf", [128, 8, NFC], BF16)
            Wt = sb("Wt", [128, 8, NTC], BF16)
            Wo = sb("Wo", [128, 8, D], BF16)
            wsrc = w_in[l].rearrange("(k p) n -> p k n", p=128)
            for name, pieces in _FCH:
                o = foff[name]
                for c0, n in pieces:
                    kb.dma(pool, Wf[:, :, o:o + n], wsrc[:, :, c0:c0 + n], reads=(), writes=Wf.r, semreg=Wf.r[0])
                    o += n
            for name, pieces in _TCH:
                o = toff[name]
                for c0, n in pieces:
                    kb.dma(pool, Wt[:, :, o:o + n], wsrc[:, :, c0:c0 + n], reads=(), writes=Wt.r, semreg=Wt.r[0])
                    o += n
            for hh in range(2):
                kb.dma(pool, Wo[:, :, hh * 512:(hh + 1) * 512],
                       w_out[l].rearrange("(k p) n -> p k n", p=128)[:, :, hh * 512:(hh + 1) * 512],
                       reads=(), writes=Wo.r, semreg=Wo.r[0])

            kvn = sb("kvn", [128, 128], F32)
            wukn = sb("wukn", [128, 256], F32)
            wuvn = sb("wuvn", [128, 256], F32)
            wg2a = sb("wg2a", [33, 256], F32)
            gn = sb("gn", [128, 4], F32)
            sinkb = sb("sinkb", [128, 4], F32)
            bfar = sb("bfar", [128, 4], F32)
            cst = sb("cst", [128, 512], F32)
            cst2 = sb("cst2", [128, 512], F32)
            kb.dma(sp, kvn[:], kv_norm[l].partition_broadcast(128), reads=(), writes=kvn.r, semreg=kvn.r[0])
            kb.dma(sp, wukn[:], w_uk[l], reads=(), writes=wukn.r, semreg=wukn.r[0])
            kb.dma(sp, wuvn[:], w_uv[l], reads=(), writes=wuvn.r, semreg=wuvn.r[0])
            kb.op(pool, lambda e: e.memset(wg2a[:], 0.0), writes=wg2a.r)
            kb.dma(sp, wg2a[0:16, :], w_g2[l], reads=(), writes=wg2a.r, semreg=wg2a.r[0])
            kb.dma(sp, wg2a[32:33, :], b_g[l:l + 1, :], reads=(), writes=wg2a.r, semreg=wg2a.r[0])
            with nc.allow_non_contiguous_dma(reason="tiny gain vectors"):
                kb.dma(sp, gn[:], gla_n[l].rearrange("(h e) -> e h", e=128), reads=(), writes=gn.r, semreg=gn.r[0])
            kb.dma(sp, sinkb[:], sinks[l].partition_broadcast(128), reads=(), writes=sinkb.r, semreg=sinkb.r[0])
            kb.dma(sp, bfar[:], bfar_d[0].partition_broadcast(128), reads=(), writes=bfar.r, semreg=bfar.r[0])

            if stage < 1:
                return
            wukT = sb("wukT", [128, 2, 128], BF16)
            wuv = sb("wuv", [128, 256], BF16)
            for i in range(2):
                p = gp()
                MM(p[:, 0:128], wukn[:, i * 128:(i + 1) * 128], identf[:], wukn.r + identf.r, p.r)
                ACT(wukT[:, i, :], p[:, 0:128], AF.Copy, p.r, wukT.r, scale=0.125)
            kb.op(dve, lambda e: e.tensor_copy(out=wuv[:], in_=wuvn[:]), reads=wuvn.r, writes=wuv.r)
            sinkexp = sb("sinkexp", [128, 4, 128], F32)
            ACT(sinkb[:], sinkb[:], AF.Exp, sinkb.r, sinkb.r)
            kb.op(pool, lambda e: e.memset(sinkexp[:], 0.0), writes=sinkexp.r)
            for h in range(4):
                kb.op(dve, lambda e: e.tensor_scalar_add(out=sinkexp[:, h, :], in0=sinkexp[:, h, :],
                                                         scalar1=sinkb[:, h:h + 1]),
                      reads=sinkexp.r + sinkb.r, writes=sinkexp.r)
            biasA = [sb(f"biasA{i}", [128, 4, 128], BF16) for i in range(2)]
            biasC = [sb(f"biasC{i}", [128, 4, 128], BF16) for i in range(2)]
            for i in range(2):
                kb.dma(sp, cst[:], cA_d[i], reads=(), writes=cst.r, semreg=cst.r[0])
                for h in range(4):
                    kb.op(dve, lambda e: e.tensor_scalar_sub(out=biasA[i][:, h, :], in0=cst[:, h * 128:(h + 1) * 128],
                                                             scalar1=bfar[:, h:h + 1]),
                          reads=cst.r + bfar.r, writes=biasA[i].r)
                kb.dma(sp, cst[:], cC_d[i], reads=(), writes=cst.r, semreg=cst.r[0])
                kb.dma(sp, cst2[:], mC_d[i], reads=(), writes=cst2.r, semreg=cst2.r[0])
                kb.op(dve, lambda e: e.tensor_tensor(out=biasC[i][:].rearrange("p h q -> p (h q)"), in0=cst[:],
                                                     in1=cst2[:], op=ALU.add),
                      reads=cst.r + cst2.r, writes=biasC[i].r)

            if stage < 2:
                return
            kiT = sb("kiT", [128, S], BF16, nreg=NB)
            cT = sb("cT", [128, S], BF16, nreg=NB)
            ctok = sb("ctok", [128, NB, 128], BF16, nreg=NB)
            kcT = sb("kcT", [128, 8, 128], BF16, nreg=8)
            vctok = sb("vctok", [128, 8, 128], BF16, nreg=8)
            St = sb("St", [128, 2, 128], F32)
            Sbf = [sb(f"Sbf{i}", [128, 2, 128], BF16) for i in range(2)]
            qaT = sb("qaT", [128, 2, TT], BF16)
            qlT = sb("qlT", [128, 4, TT], BF16)
            qiT = sb("qiT", [128, 2, TT], BF16)
            glra = sb("glra", [33, TT], F32)
            sptok1 = sb("sptok", [128, 256], F32)
            eDk = sb("eDk", [128, 256], F32)
            EbT = sb("EbT", [128, 2, TT], F32)
            EnT = sb("EnT", [128, 2, TT], BF16)
            qeT = sb("qeT", [128, 2, TT], BF16)
            keT = sb("keT", [128, 2, TT], BF16)
            SrgT = sb("SrgT", [128, 4, TT], BF16)
            qcT = sb("qcT", [128, 2, TT], BF16)
            kdtok = sb("kdtok", [128, 4, 256], BF16, nreg=4)
            vbtok = sb("vbtok", [128, 4, 512], BF16, nreg=4)
            widx = sb("widx", [128, 4, 4], F32, nreg=4)
            ssc = sb("ssc", [128, 1], F32)
            rc = sb("rc", [128, 1], F32)
            mixT = hT
            attm = sb("attm", [128, 4, 128], BF16)
            kdm = sb("kdm", [128, 2, 256], BF16)
            sqb = sb("sqb", [128, 512], BF16)
            PT = [sb(f"PT{i}", [128, 512], BF16) for i in range(2)]
            assert xbuf == 1
            score = Tile(X[0][:].rearrange("p s d -> p (s d)"), X[0].r)
            negm0 = sb("negm0", [128, 4096], BF16)
            negm1 = sb("negm1", [128, S], BF16)
            negmB = [negm0, negm1]
            xn = Tile(negm0[:, 0:4096].rearrange("p (s d) -> p s d", s=4), negm0.r)
            junk = Tile(negm0[:, 0:1024], negm0.r)
            rsbB = sb("rsbB", [128, 512], F32)
            tmpR = [cst, cst2]
            t1, rsb = cst, cst2
            m8 = sb("m8", [128, 8], F32)
            bs_lo = sb("bs_lo", [128, 1], F32)
            bs_R = sb("bs_R", [128, 1], F32)
            bs_hh = sb("bs_hh", [128, KBIS + 1], F32)
            bs_mid = sb("bs_mid", [128, 1], F32)
            bs_cnt = sb("bs_cnt", [128, 1], F32)
            bs_fh = sb("bs_fh", [128, 1], F32)
            olT = sb("olT", [128, 4, 128], BF16)
            pstate = {"i": 0}

            def nextPT():
                pstate["i"] += 1
                return PT[pstate["i"] % 2]

            kb.op(pool, lambda e: e.memset(glra[:], 0.0), writes=glra.r)
            kb.op(pool, lambda e: e.memset(glra[32:33, :], 1.0), reads=glra.r, writes=glra.r)
            gcols = [gmix[:, l * 8 + c:l * 8 + c + 1] for c in range(8)]

            def fproj(name):
                o, n = foff[name], fw[name]
                p = gp()
                for k in range(8):
                    MM(p[0:n, :], Wf[:, k, o:o + n], hT[:, k, :], Wf.r + hT.r, p.r,
                       start=(k == 0), stop=(k == 7), inc=(k == 7))
                return p

            def tproj(name, blk):
                o, n = toff[name], tw[name]
                p = gp()
                for k in range(8):
                    MM(p[:, 0:n], hT[:, k, blk * 128:(blk + 1) * 128], Wt[:, k, o:o + n], Wt.r + hT.r, p.r,
                       start=(k == 0), stop=(k == 7), inc=(k == 7))
                return p

            for b in range(NSEQ):
                kb.op(pool, lambda e: e.memset(St[:], 0.0), reads=St.r, writes=St.r)
                for t in range(TPS):
                    gt = b * TPS + t
                    Xt = X[gt % xbuf]
                    kb.dma(sp, Xt[:], src[gt * TT:(gt + 1) * TT, :].rearrange("(s p) d -> p s d", p=128),
                           reads=[src_regs[gt]], writes=Xt.r, semreg=Xt.r[0])
                    rmsnorm_T(Xt, gcols, xn, junk)
                    if stage < 3:
                        continue

                    for i in range(2):
                        p = fproj(f"qa{i}")
                        ACT(qaT[:, i, :], p[:], AF.Copy, p.r, qaT.r)
                    for h in range(4):
                        p = gp()
                        hp = (h % 2) * 64
                        MM(p[:], wukT[hp:hp + 64, h // 2, :], qaT[hp:hp + 64, h // 2, :], wukT.r + qaT.r, p.r)
                        kb.op(dve, lambda e: e.tensor_copy(out=qlT[:, h, :], in_=p[:]), reads=p.r, writes=qlT.r)
                    if stage == 31:
                        continue
                    for i in range(2):
                        p = fproj(f"qi{i}")
                        ACT(qiT[:, i, :], p[:], AF.Copy, p.r, qiT.r)
                    p = fproj("ki")
                    kb.op(dve, lambda e: e.tensor_copy(out=kiT[:, t * TT:(t + 1) * TT], in_=p[:]), reads=p.r,
                          writes=kiT.r[t * 4:t * 4 + 4])
                    p = fproj("glr")
                    kb.op(dve, lambda e: e.tensor_copy(out=glra[0:16, :], in_=p[0:16, :]), reads=p.r, writes=glra.r)
                    if stage == 32:
                        continue
                    for i in range(2):
                        p = fproj(f"qc{i}")
                        ACT(qcT[:, i, :], p[:], AF.Copy, p.r, qcT.r, scale=0.125)
                    p = fproj("kc")
                    for blk in range(4):
                        sl = (t * 4 + blk) % 8
                        kb.op(dve, lambda e: e.tensor_copy(out=kcT[:, sl, :], in_=p[:, blk * 128:(blk + 1) * 128]),
                              reads=p.r, writes=[kcT.r[sl]])
                    if stage == 33:
                        continue
                    for i in range(4):
                        p = fproj(f"rb{i}")
                        ACT(SrgT[:, i, :], p[:], AF.Silu, p.r, SrgT.r)
                        kb.op(dve, lambda e: e.tensor_scalar_mul(out=SrgT[:, i, :], in0=SrgT[:, i, :],
                                                                   scalar1=gn[:, i:i + 1]),
                              reads=SrgT.r + gn.r, writes=SrgT.r)

                    if stage < 4 or stage == 41:
                        continue
                    pG = [PS[4], PS[5]]
                    for blk in range(4):
                        jb = t * 4 + blk
                        sl = jb % 8
                        p = tproj("A", blk)
                        ACT(junk[:, 0:128], p[:, 0:128], AF.Square, p.r, junk.r + ssc.r, accum_out=ssc[:])
                        ACT(rc[:], ssc[:], AF.Sqrt, ssc.r + epsc.r, rc.r, bias=epsc[:], scale=1.0 / 128)
                        kb.op(dve, lambda e: e.reciprocal(out=rc[:], in_=rc[:]), reads=rc.r, writes=rc.r)
                        kb.op(dve, lambda e: e.scalar_tensor_tensor(out=ctok[:, jb, :], in0=p[:, 0:128], scalar=rc[:],
                                                                    in1=kvn[:], op0=ALU.mult, op1=ALU.mult),
                              reads=p.r + rc.r + kvn.r, writes=[ctok.r[jb]])
                        kb.op(dve, lambda e: e.tensor_copy(out=widx[:, blk, :], in_=p[:, 128:132]), reads=p.r,
                              writes=[widx.r[blk]])
                        p2 = gp()
                        MM(p2[:, 0:128], ctok[:, jb, :], ident[:], [ctok.r[jb]] + ident.r, p2.r)
                        ACT(cT[:, jb * 128:(jb + 1) * 128], p2[:, 0:128], AF.Copy, p2.r, [cT.r[jb]])
                        if stage == 42:
                            continue
                        pz = gp()
                        MM(pz[:, 0:256], glra[0:33, blk * 128:(blk + 1) * 128], wg2a[0:33, :], glra.r + wg2a.r, pz.r)
                        ACT(sptok1[:], pz[:, 0:256], AF.Exp, pz.r, sptok1.r, scale=-1.0)
                        ACT(sptok1[:], sptok1[:], AF.Ln, sptok1.r + onec.r, sptok1.r,
                            bias=onec[:], scale=1.0)
                        if stage == 43:
                            continue
                        for i in range(2):
                            MM(pG[i][:, blk * 128:(blk + 1) * 128], sptok1[:, i * 128:(i + 1) * 128], tribd[:],
                               sptok1.r + tribd.r, pG[i].r)
                        pd = gp()
                        MM(pd[:, 0:256], subd[:], sptok1[:], sptok1.r + subd.r, pd.r)
                        ACT(eDk[:], pd[:, 0:256], AF.Exp, pd.r, eDk.r, scale=-1.0 / 16)
                        if stage == 44:
                            continue
                        p = tproj("B", blk)
                        if stage == 46:
                            continue
                        if stage != 49:
                            kb.op(dve, lambda e: e.tensor_tensor(out=kdtok[:, blk, :], in0=p[:, 0:256], in1=eDk[:],
                                                                 op=ALU.mult),
                                  reads=p.r + eDk.r, writes=[kdtok.r[blk]])
                        if stage == 48:
                            continue
                        ACT(vctok[:, sl, :], p[:, 256:384], AF.Copy, p.r, [vctok.r[sl]])
                        if stage in (47, 49):
                            continue
                        p = tproj("C", blk)
                        ACT(vbtok[:, blk, :], p[:], AF.Copy, p.r, [vbtok.r[blk]])
                    if stage in (42, 43, 44, 45, 46, 47, 48, 49):
                        continue
                    for i in range(2):
                        ACT(EbT[:, i, :], pG[i][:], AF.Exp, pG[i].r, EbT.r, scale=-1.0 / 16)
                        ACT(EnT[:, i, :], pG[i][:], AF.Exp, pG[i].r, EnT.r, scale=1.0 / 16)
                    for i in range(2):
                        p = fproj(f"qb{i}")
                        kb.op(dve, lambda e: e.scalar_tensor_tensor(out=qeT[:, i, :], in0=p[:], scalar=0.125,
                                                                    in1=EbT[:, i, :], op0=ALU.mult, op1=ALU.mult),
                              reads=p.r + EbT.r, writes=qeT.r)
                        p = fproj(f"kb{i}")
                        kb.op(dve, lambda e: e.tensor_tensor(out=keT[:, i, :], in0=p[:], in1=EnT[:, i, :], op=ALU.mult),
                              reads=p.r + EnT.r, writes=keT.r)

                    if stage < 5:
                        continue
                    if len(parts) < 3:
                        kb.op(pool, lambda e: e.memset(mixT[:], 0.0), reads=mixT.r, writes=mixT.r)
                    for blk in range(4 if "gla" in parts else 0):
                        c0 = blk * 128
                        pA2 = [gp(), gp()]
                        for h in range(4):
                            hp = (h % 2) * 64
                            MM(pA2[h % 2][:, (h // 2) * 128:(h // 2 + 1) * 128], keT[hp:hp + 64, h // 2, c0:c0 + 128],
                               qeT[hp:hp + 64, h // 2, c0:c0 + 128], keT.r + qeT.r, pA2[h % 2].r)
                        for h in range(4):
                            kb.op(dve, lambda e: e.tensor_tensor(out=attm[:, h, :],
                                                                 in0=pA2[h % 2][:, (h // 2) * 128:(h // 2 + 1) * 128],
                                                                 in1=tribd[:], op=ALU.mult),
                                  reads=pA2[h % 2].r + tribd.r, writes=attm.r)
                        for cc in range(2):
                            kb.op(dve, lambda e: e.tensor_copy(out=Sbf[cc][:], in_=St[:]), reads=St.r, writes=Sbf[cc].r)
                            kb.op(dve, lambda e: e.tensor_scalar_mul(out=kdm[:, cc, :], in0=kdtok[:, blk, :],
                                                                     scalar1=cmask[:, cc:cc + 1]),
                                  reads=[kdtok.r[blk]] + cmask.r, writes=kdm.r)
                            pK = gp()
                            for h in range(4):
                                hp = (h % 2) * 64
                                MM(pK[hp:hp + 64, (h // 2) * 128:(h // 2 + 1) * 128],
                                   kdm[:, cc, h * 64:(h + 1) * 64], vbtok[:, blk, h * 128:(h + 1) * 128],
                                   kdm.r + [vbtok.r[blk]], pK.r, inc=(h == 3))
                            col = c0 + cc * 64 + 63
                            for i in range(2):
                                kb.op(dve, lambda e: e.scalar_tensor_tensor(
                                    out=St[:, i, :], in0=St[:, i, :], scalar=EbT[:, i, col:col + 1],
                                    in1=pK[:, i * 128:(i + 1) * 128], op0=ALU.mult, op1=ALU.add),
                                    reads=St.r + EbT.r + pK.r, writes=St.r)
                        import os as _os
                        if _os.environ.get("GSKIP") == "1":
                            continue
                        pO = gp()
                        for h in range(4):
                            hp = (h % 2) * 64
                            for cc in range(2):
                                oc = h * 128 + cc * 64
                                MM(pO[:, oc:oc + 64], vbtok[:, blk, h * 128:(h + 1) * 128],
                                   attm[:, h, cc * 64:cc * 64 + 64], [vbtok.r[blk]] + attm.r, pO.r,
                                   start=True, stop=False, inc=False)
                                MM(pO[:, oc:oc + 64], Sbf[cc][hp:hp + 64, h // 2, :],
                                   qeT[hp:hp + 64, h // 2, c0 + cc * 64:c0 + cc * 64 + 64], Sbf[cc].r + qeT.r, pO.r,
                                   start=False, stop=True, inc=(h == 3 and cc == 1))
                        if _os.environ.get("GSKIP") == "2":
                            continue
                        ACT(sqb[:], pO[:], AF.Square, pO.r, sqb.r)
                        pN = gp()
                        MM(pN[:], ones_bf[:], sqb[:], ones_bf.r + sqb.r, pN.r)
                        ACT(rsb[:], pN[:], AF.Sqrt, pN.r + epsc.r, rsb.r, bias=epsc[:], scale=1.0 / 128)
                        kb.op(dve, lambda e: e.reciprocal(out=rsb[:], in_=rsb[:]), reads=rsb.r, writes=rsb.r)
                        kb.op(dve, lambda e: e.tensor_tensor(out=t1[:], in0=pO[:], in1=rsb[:], op=ALU.mult),
                              reads=pO.r + rsb.r, writes=t1.r)
                        kb.op(dve, lambda e: e.tensor_tensor(out=mixT[:, 2:6, c0:c0 + 128],
                                                              in0=t1[:].rearrange("p (h q) -> p h q", h=4),
                                                              in1=SrgT[:, :, c0:c0 + 128], op=ALU.mult),
                              reads=t1.r + SrgT.r, writes=mixT.r)

                    for blk in range(4 if "swa" in parts else 0):
                        jb = t * 4 + blk
                        c0 = blk * 128
                        pO, pD = PS[6], PS[7]
                        kbs = [jb - 1, jb] if jb > 0 else [jb]
                        for n_i, kbk in enumerate(kbs):
                            sl = kbk % 8
                            which = 1 if kbk == jb else 0
                            pL = PS[4 + (n_i % 2)]
                            for kv in range(2):
                                MM(pL[:, kv * 256:(kv + 1) * 256], kcT[kv * 64:kv * 64 + 64, sl, :],
                                   qcT[kv * 64:kv * 64 + 64, :, c0:c0 + 128], [kcT.r[sl]] + qcT.r, pL.r,
                                   start=True, stop=False, inc=False)
                                MM(pL[:, kv * 256:(kv + 1) * 256], ident[:],
                                   biasC[which][:, kv * 2:kv * 2 + 2, :].rearrange("p h q -> p (h q)"),
                                   ident.r + biasC[which].r, pL.r, start=False, stop=True, inc=(kv == 1))
                            P = PT[n_i]
                            ACT(P[:], pL[:], AF.Exp, pL.r, P.r)
                            first, lastk = (n_i == 0), (n_i == len(kbs) - 1)
                            MM(pD[:], ones_bf[:], P[:], ones_bf.r + P.r, pD.r, start=first, stop=lastk)
                        for kv in range(2):
                            for g in range(2):
                                hd = kv * 2 + g
                                for n_i, kbk in enumerate(kbs):
                                    sl = kbk % 8
                                    P = PT[n_i]
                                    MM(pO[g * 64:g * 64 + 64, kv * 128:(kv + 1) * 128], vctok[:, sl, kv * 64:kv * 64 + 64],
                                       P[:, hd * 128:(hd + 1) * 128], [vctok.r[sl]] + P.r, pO.r,
                                       start=(n_i == 0), stop=(n_i == len(kbs) - 1))
                        kb.op(dve, lambda e: e.tensor_tensor(out=rsb[:], in0=pD[:],
                                                             in1=sinkexp[:].rearrange("p h q -> p (h q)"), op=ALU.add),
                              reads=pD.r + sinkexp.r, writes=rsb.r)
                        kb.op(dve, lambda e: e.reciprocal(out=rsb[:], in_=rsb[:]), reads=rsb.r, writes=rsb.r)
                        for kv in range(2):
                            for g in range(2):
                                hd = kv * 2 + g
                                kb.op(dve, lambda e: e.tensor_tensor(
                                    out=mixT[g * 64:g * 64 + 64, 6 + kv, c0:c0 + 128],
                                    in0=pO[g * 64:g * 64 + 64, kv * 128:(kv + 1) * 128],
                                    in1=rsb[g * 64:g * 64 + 64, hd * 128:(hd + 1) * 128], op=ALU.mult),
                                    reads=pO.r + rsb.r, writes=mixT.r)

                    def dsa_A(blk):
                        jb = t * 4 + blk
                        c0 = blk * 128
                        N = (jb + 1) * 128
                        negm = negmB[blk % 2]
                        for kt in range((N + 511) // 512):
                            k0 = kt * 512
                            n = min(512, N - k0)
                            kregs = kiT.r[k0 // 128:(k0 + n) // 128]
                            for h in range(4):
                                hp = (h % 2) * 64
                                p = gp()
                                MM(p[:, 0:n], qiT[hp:hp + 64, h // 2, c0:c0 + 128], kiT[hp:hp + 64, k0:k0 + n],
                                   qiT.r + kregs, p.r)
                                if h == 0:
                                    kb.op(dve, lambda e: e.tensor_scalar(
                                        out=score[:, k0:k0 + n], in0=p[:, 0:n], scalar1=0.0,
                                        scalar2=widx[:, blk, 0:1], op0=ALU.max, op1=ALU.mult),
                                        reads=p.r + [widx.r[blk]], writes=score.r)
                                else:
                                    tr = tmpR[h % 2]
                                    ACT(tr[:, 0:n], p[:, 0:n], AF.Relu, p.r, tr.r)
                                    kb.op(dve, lambda e: e.scalar_tensor_tensor(
                                        out=score[:, k0:k0 + n], in0=tr[:, 0:n], scalar=widx[:, blk, h:h + 1],
                                        in1=score[:, k0:k0 + n], op0=ALU.mult, op1=ALU.add),
                                        reads=tr.r + [widx.r[blk]] + score.r, writes=score.r)
                            kb.op(dve, lambda e: e.scalar_tensor_tensor(
                                out=score[:, k0:k0 + n], in0=ramp512[:, 0:n], scalar=-TIE * k0,
                                in1=score[:, k0:k0 + n], op0=ALU.add, op1=ALU.add),
                                reads=ramp512.r + score.r, writes=score.r)
                        if N > 256:
                            kb.op(dve, lambda e: e.max(out=m8[:], in_=score[:, 0:N]), reads=score.r, writes=m8.r)
                            kb.op(dve, lambda e: e.tensor_reduce(out=bs_lo[:], in_=score[:, 0:N], axis=AX.X, op=ALU.min),
                                  reads=score.r, writes=bs_lo.r)
                            kb.op(dve, lambda e: e.tensor_scalar_add(out=bs_lo[:], in0=bs_lo[:], scalar1=-1.0),
                                  reads=bs_lo.r, writes=bs_lo.r)
                            kb.op(dve, lambda e: e.tensor_tensor(out=bs_R[:], in0=m8[:, 0:1], in1=bs_lo[:], op=ALU.subtract),
                                  reads=m8.r + bs_lo.r, writes=bs_R.r)
                            kb.op(dve, lambda e: e.tensor_scalar_mul(out=bs_hh[:], in0=pw[:], scalar1=bs_R[:, 0:1]),
                                  reads=pw.r + bs_R.r, writes=bs_hh.r)
                            kb.op(dve, lambda e: e.tensor_tensor(out=bs_mid[:], in0=bs_lo[:], in1=bs_hh[:, 0:1], op=ALU.add),
                                  reads=bs_lo.r + bs_hh.r, writes=bs_mid.r)
                        kb.op(pool, lambda e: e.memset(score[0:64, N - 64:N], -3e38), reads=score.r, writes=score.r)
                        if N > 256:
                            for k in range(KBIS):
                                kb.op(dve, lambda e: e.tensor_scalar(out=negm[:, 0:N], in0=score[:, 0:N],
                                                                     scalar1=bs_mid[:, 0:1], scalar2=None, op0=ALU.is_gt,
                                                                     op1=ALU.add, accum_out=bs_cnt[:]),
                                      reads=score.r + bs_mid.r, writes=negm.r + bs_cnt.r)
                                kb.op(dve, lambda e: e.tensor_scalar(out=bs_fh[:], in0=bs_cnt[:], scalar1=c256[:, 0:1],
                                                                     scalar2=bs_hh[:, k:k + 1], op0=ALU.is_ge, op1=ALU.mult),
                                      reads=bs_cnt.r + c256.r + bs_hh.r, writes=bs_fh.r)
                                kb.op(dve, lambda e: e.scalar_tensor_tensor(out=bs_mid[:], in0=bs_mid[:],
                                                                            scalar=bs_hh[:, k + 1:k + 2], in1=bs_fh[:],
                                                                            op0=ALU.subtract, op1=ALU.add),
                                      reads=bs_mid.r + bs_hh.r + bs_fh.r, writes=bs_mid.r)
                            kb.op(dve, lambda e: e.tensor_tensor(out=bs_lo[:], in0=bs_mid[:], in1=bs_hh[:, KBIS:KBIS + 1],
                                                                 op=ALU.subtract),
                                  reads=bs_mid.r + bs_hh.r, writes=bs_lo.r)
                            kb.op(dve, lambda e: e.tensor_scalar(out=negm[:, 0:N], in0=score[:, 0:N], scalar1=bs_lo[:, 0:1],
                                                                 scalar2=NEG, op0=ALU.is_le, op1=ALU.mult),
                                  reads=score.r + bs_lo.r, writes=negm.r)
                        else:
                            kb.op(dve, lambda e: e.tensor_scalar(out=negm[:, 0:N], in0=score[:, 0:N], scalar1=-1e29,
                                                                 scalar2=NEG, op0=ALU.is_lt, op1=ALU.mult),
                                  reads=score.r, writes=negm.r)

                    def dsa_B(blk):
                        jb = t * 4 + blk
                        c0 = blk * 128
                        negm = negmB[blk % 2]
                        pO, pD = PS[6], PS[7]
                        for kbk in range(jb + 1):
                            pL = PS[4 + (kbk % 2)]
                            near = kbk >= jb - 1
                            MM(pL[:], cT[:, kbk * 128:(kbk + 1) * 128], qlT[:, :, c0:c0 + 128], [cT.r[kbk]] + qlT.r, pL.r,
                               start=True, stop=False, inc=False)
                            MM(pL[:], negm[:, kbk * 128:(kbk + 1) * 128], ident4[:].rearrange("p h q -> p (h q)"),
                               negm.r + ident4.r, pL.r, start=False, stop=not near, inc=not near)
                            if near:
                                which = 1 if kbk == jb else 0
                                MM(pL[:], ident[:], biasA[which][:].rearrange("p h q -> p (h q)"),
                                   ident.r + biasA[which].r, pL.r, start=False, stop=True)
                            P = nextPT()
                            ACT(P[:], pL[:], AF.Exp, pL.r, P.r)
                            MM(pO[:], ctok[:, kbk, :], P[:], [ctok.r[kbk]] + P.r, pO.r, start=(kbk == 0), stop=(kbk == jb),
                               inc=False)
                            MM(pD[:], ones_bf[:], P[:], ones_bf.r + P.r, pD.r + pO.r, start=(kbk == 0), stop=(kbk == jb))
                        kb.op(dve, lambda e: e.reciprocal(out=rsbB[:], in_=pD[:]), reads=pD.r, writes=rsbB.r)
                        kb.op(dve, lambda e: e.tensor_tensor(out=olT[:].rearrange("p h q -> p (h q)"), in0=pO[:],
                                                             in1=rsbB[:], op=ALU.mult),
                              reads=pO.r + rsbB.r, writes=olT.r)
                        pF = gp()
                        for h in range(4):
                            hp = (h % 2) * 64
                            MM(pF[hp:hp + 64, (h // 2) * 128:(h // 2 + 1) * 128], wuv[:, h * 64:(h + 1) * 64], olT[:, h, :],
                               wuv.r + olT.r, pF.r, inc=(h == 3))
                        ACT(mixT[:, 0:2, c0:c0 + 128], pF[:, 0:256].rearrange("p (i q) -> p i q", i=2), AF.Copy, pF.r,
                            mixT.r)

                    if "dsa" in parts:
                        dsa_A(0)
                        for blk in range(4):
                            if blk + 1 < 4:
                                dsa_A(blk + 1)
                            dsa_B(blk)

                    kb.dma(sp, Xt[:], src[gt * TT:(gt + 1) * TT, :].rearrange("(s p) d -> p s d", p=128),
                           reads=[src_regs[gt]], writes=Xt.r, semreg=Xt.r[0])
                    for s in range(4):
                        for hh in range(2):
                            po = gp()
                            for c in range(8):
                                MM(po[:], mixT[:, c, s * 128:(s + 1) * 128], Wo[:, c, hh * 512:(hh + 1) * 512],
                                   mixT.r + Wo.r, po.r, start=(c == 0), stop=(c == 7), inc=(c == 7))
                            kb.op(dve, lambda e: e.tensor_tensor(
                                out=Xt[:, s, hh * 512:(hh + 1) * 512], in0=Xt[:, s, hh * 512:(hh + 1) * 512],
                                in1=po[:], op=ALU.add), reads=Xt.r + po.r, writes=Xt.r)
                    kb.dma(sp, xs[gt * TT:(gt + 1) * TT, :].rearrange("(s p) d -> p s d", p=128), Xt[:],
                           reads=Xt.r, writes=[xs.r[gt]], semreg=Xt.r[0])

        cur, cur_regs = x_in, xin_regs
        for l in range(DEPTH):
            if do_mixer:
                with ExitStack() as pes:
                    mixer_phase(l, cur, cur_regs, pes)
                    kb.barrier()
                cur, cur_regs = xs.t, xs.r
            if do_ffn:
                with ExitStack() as pes:
                    ffn_phase(l, cur, cur_regs, last=(l == DEPTH - 1), pes=pes)
                    kb.barrier()
                cur, cur_regs = xs.t, xs.r

        for r in yout_regs:
            sp.wait(r.last_w)
        print(f"[build] instructions={kb.ninst} sems={kb.nsem}", flush=True)
    return nc


_W_NAMES = ["w_in", "w_out", "norm_mix", "norm_ffn", "kv_norm", "w_uk", "w_uv", "w_gate2", "b_gate", "gla_norm",
            "sinks", "w_ffn_gate", "w_ffn_up", "w_ffn_down", "final_norm"]


def make_in_maps(inputs, n_cores, nseq, S):
    x = np.ascontiguousarray(inputs["x"], dtype=np.float32)
    L = inputs["w_in"].shape[0]
    shared = {}
    for k in _W_NAMES:
        a = np.ascontiguousarray(inputs[k], dtype=np.float32)
        if k == "final_norm":
            a = a.reshape(1, D)
        if k in ("w_uk", "w_uv"):
            a = a.reshape(L, 128, 256)
        shared[k] = a
    cA, cC, mC, bfar = _host_consts(inputs["rel_bias"])
    shared.update(cA=cA, cC=cC, mC=mC, bfar=bfar)
    in_maps = []
    for c in range(n_cores):
        m = dict(shared)
        m["x"] = x[c * nseq:(c + 1) * nseq].reshape(nseq * S, D)
        in_maps.append(m)
    return in_maps


def kernel(**inputs):
    n = 8
    B, S, _ = inputs["x"].shape
    nseq = B // n
    nc = build(S=S, NSEQ=nseq, DEPTH=inputs["w_in"].shape[0])
    in_maps = make_in_maps(inputs, n, nseq, S)
    res = run_bass_kernel_spmd(nc, in_maps, core_ids=list(range(n)))
    out = np.concatenate([r["y"].reshape(nseq, S, D) for r in res.results], axis=0)
    return out.astype(np.float32)
```
